# Optimizing a Trainium2 kernel written in Bass

```python
import math
import jax, jax.numpy as jnp
from jax import lax
import numpy as np

D_MODEL = 1024
BATCH = 16
SEQ = 2048
DEPTH = 2

CTX_LEN = 256
GRID_W = 64
D_MIX = D_MODEL
DN_HEAD_DIM = 128
DN_WIDTH = D_MIX // 2
DN_HEADS = DN_WIDTH // DN_HEAD_DIM
DN_CHUNK = 64
RG_WIDTH = D_MIX - DN_WIDTH
RG_BLOCKS = 8
RG_BLOCK_DIM = RG_WIDTH // RG_BLOCKS
RG_C = 8.0
CONV_K = 4
CONV_PAD_L = 2
N_GROUPS = 4
EXPERTS_PER_GROUP = 8
N_EXPERTS = N_GROUPS * EXPERTS_PER_GROUP
TOP_K = 2
D_EXPERT = D_MODEL // 4
DEEP_ALPHA = (2 * DEPTH) ** 0.25
DEEP_BETA = (8 * DEPTH) ** -0.25
LN_EPS = 1e-5
NORM_EPS = 1e-6
SPLITS = (3 * DN_WIDTH, 4 * DN_WIDTH, 4 * DN_WIDTH + 2 * DN_HEADS, 4 * DN_WIDTH + 4 * DN_HEADS,
          4 * DN_WIDTH + 4 * DN_HEADS + RG_WIDTH)
D_IN = 4 * DN_WIDTH + 4 * DN_HEADS + 2 * RG_WIDTH

kernel_name = "hybrid_deltanet_rglru_hmoe_dit"


def layer_norm(x):
    xf = x.astype(jnp.float32)
    mu = jnp.mean(xf, axis=-1, keepdims=True)
    var = jnp.mean(jnp.square(xf - mu), axis=-1, keepdims=True)
    return (xf - mu) * lax.rsqrt(var + LN_EPS)


def modulate(x, shift, scale):
    return (layer_norm(x) * (1.0 + scale) + shift).astype(x.dtype)


def post_norm(r, g, b):
    return (layer_norm(r) * g + b).astype(r.dtype)


def l2norm(t):
    return t * lax.rsqrt(jnp.sum(t * t, axis=-1, keepdims=True) + NORM_EPS)


def _rev(t, d):
    return jnp.flip(t, axis=1) if d == 1 else t


def dwconv_centred(x, w):
    L = x.shape[1]
    xp = jnp.pad(x, ((0, 0), (CONV_PAD_L, CONV_K - 1 - CONV_PAD_L), (0, 0)))
    y = xp[:, 0:L] * w[0]
    for j in range(1, CONV_K):
        y = y + xp[:, j:j + L] * w[j]
    return y


def conv_latent_rows(x, w):
    B, L, C = x.shape
    rows = L // GRID_W
    return dwconv_centred(x.reshape(B * rows, GRID_W, C), w).reshape(B, L, C)


def gated_delta_chunked(q, k, v, g, beta, s0):
    B, T, H, DK = q.shape
    DV = v.shape[-1]
    C = DN_CHUNK
    N = T // C
    to_chunks = lambda t: t.reshape(B, N, C, H, -1).transpose(0, 3, 1, 2, 4)
    q, k, v = to_chunks(q), to_chunks(k), to_chunks(v)
    beta = to_chunks(beta[..., None])
    gc = jnp.cumsum(to_chunks(g[..., None])[..., 0], axis=-1)
    incl = jnp.tril(jnp.ones((C, C), dtype=bool))
    strict = jnp.tril(jnp.ones((C, C), dtype=bool), -1)
    decay = jnp.exp(jnp.where(incl, gc[..., :, None] - gc[..., None, :], -jnp.inf))
    kb = k * beta
    vb = v * beta
    a_strict = jnp.where(strict, jnp.einsum('bhnid,bhnjd->bhnij', kb, k) * decay, 0.0)
    eye = jnp.eye(C, dtype=q.dtype)
    rhs = jnp.concatenate([vb, kb * jnp.exp(gc)[..., None]], axis=-1)
    sol = lax.linalg.triangular_solve(eye + a_strict, rhs, left_side=True, lower=True, unit_diagonal=True)
    u, w = sol[..., :DV], sol[..., DV:]
    intra = jnp.einsum('bhnid,bhnjd->bhnij', q, k) * decay
    q_dec = q * jnp.exp(gc)[..., None]
    k_dec = k * jnp.exp(gc[..., -1:] - gc)[..., None]
    g_last = jnp.exp(gc[..., -1])

    def step(S, xs):
        qd, kd, u_i, w_i, at, gl = xs
        v_new = u_i - jnp.einsum('bhck,bhkv->bhcv', w_i, S)
        o = jnp.einsum('bhck,bhkv->bhcv', qd, S) + jnp.einsum('bhij,bhjv->bhiv', at, v_new)
        S = S * gl[..., None, None] + jnp.einsum('bhck,bhcv->bhkv', kd, v_new)
        return S, o

    xs = tuple(jnp.moveaxis(t, 2, 0) for t in (q_dec, k_dec, u, w, intra, g_last))
    s_final, o = lax.scan(step, s0, xs)
    o = o.transpose(1, 0, 3, 2, 4).reshape(B, T, H, DV)
    return o, s_final


def dn_prep(qkv, beta_raw, decay_raw, a_log, dt_bias):
    B, L, _ = qkv.shape
    qkv = jax.nn.silu(qkv).astype(jnp.float32)
    q, k, v = jnp.split(qkv, 3, axis=-1)
    q = l2norm(q.reshape(B, L, DN_HEADS, DN_HEAD_DIM)) * (DN_HEAD_DIM ** -0.5)
    k = l2norm(k.reshape(B, L, DN_HEADS, DN_HEAD_DIM))
    v = v.reshape(B, L, DN_HEADS, DN_HEAD_DIM)
    beta = jax.nn.sigmoid(beta_raw.astype(jnp.float32)).reshape(B, L, 2, DN_HEADS)
    g = -jnp.exp(a_log.astype(jnp.float32)) * jax.nn.softplus(
        decay_raw.astype(jnp.float32).reshape(B, L, 2, DN_HEADS) + dt_bias.astype(jnp.float32))
    return q, k, v, beta, g


def bidir_deltanet(dn_c, dn_l):
    qc, kc, vc, bc, gcx = dn_c
    ql, kl, vl, bl, glx = dn_l
    B = qc.shape[0]
    outs_c, outs_l = [], []
    for d in range(2):
        s0 = jnp.zeros((B, DN_HEADS, DN_HEAD_DIM, DN_HEAD_DIM), jnp.float32)
        oc, sc = gated_delta_chunked(_rev(qc, d), _rev(kc, d), _rev(vc, d), _rev(gcx[:, :, d], d),
                                     _rev(bc[:, :, d], d), s0)
        ol, _ = gated_delta_chunked(_rev(ql, d), _rev(kl, d), _rev(vl, d), _rev(glx[:, :, d], d),
                                    _rev(bl[:, :, d], d), sc)
        outs_c.append(_rev(oc, d))
        outs_l.append(_rev(ol, d))
    return outs_c[0] + outs_c[1], outs_l[0] + outs_l[1]


def gated_rmsnorm(o, z, w):
    B, L = o.shape[:2]
    o = o * lax.rsqrt(jnp.mean(o * o, axis=-1, keepdims=True) + NORM_EPS) * w.astype(jnp.float32)
    return o.reshape(B, L, DN_WIDTH) * jax.nn.silu(z.astype(jnp.float32))


def rglru_coeffs(xc, w_a, b_a, w_i, b_i, lam):
    B, L, _ = xc.shape
    xb = xc.reshape(B, L, RG_BLOCKS, RG_BLOCK_DIM)
    r = jax.nn.sigmoid(jnp.einsum('blnc,ncd->blnd', xb, w_a.astype(jnp.float32)).reshape(B, L, RG_WIDTH)
                       + b_a.astype(jnp.float32))
    i = jax.nn.sigmoid(jnp.einsum('blnc,ncd->blnd', xb, w_i.astype(jnp.float32)).reshape(B, L, RG_WIDTH)
                       + b_i.astype(jnp.float32))
    log_a = -RG_C * r * jax.nn.softplus(-lam.astype(jnp.float32))
    a = jnp.exp(log_a)
    b = jnp.sqrt(-jnp.expm1(2.0 * log_a)) * (i * xc)
    return a, b


def linear_scan(a, b, h0):
    b = b.at[:, 0].add(a[:, 0] * h0)

    def combine(left, right):
        a_l, b_l = left
        a_r, b_r = right
        return a_l * a_r, a_r * b_l + b_r

    _, h = lax.associative_scan(combine, (a, b), axis=1)
    return h


def bidir_rglru(xc_c, xc_l, wa, ba, wi, bi, lam):
    B = xc_c.shape[0]
    outs_c, outs_l = [], []
    for d in range(2):
        a_c, b_c = rglru_coeffs(_rev(xc_c, d), wa[d], ba[d], wi[d], bi[d], lam[d])
        h_c = linear_scan(a_c, b_c, jnp.zeros((B, RG_WIDTH), jnp.float32))
        a_l, b_l = rglru_coeffs(_rev(xc_l, d), wa[d], ba[d], wi[d], bi[d], lam[d])
        h_l = linear_scan(a_l, b_l, h_c[:, -1])
        outs_c.append(_rev(h_c, d))
        outs_l.append(_rev(h_l, d))
    return outs_c[0] + outs_c[1], outs_l[0] + outs_l[1]


def hybrid_mixer(h_c, h_l, w_in, conv_qkv_w, dn_a_log, dn_dt_bias, dn_onorm_w, rg_conv_w, rg_conv_b,
                 rg_wa, rg_ba, rg_wi, rg_bi, rg_lambda, w_out):
    n_ctx = h_c.shape[1]
    proj = jnp.concatenate([h_c, h_l], axis=1) @ w_in
    qkv_c, z_c, br_c, dr_c, rx_c, ry_c = jnp.split(proj[:, :n_ctx], SPLITS, axis=-1)
    qkv_l, z_l, br_l, dr_l, rx_l, ry_l = jnp.split(proj[:, n_ctx:], SPLITS, axis=-1)
    dn_c = dn_prep(dwconv_centred(qkv_c, conv_qkv_w), br_c, dr_c, dn_a_log, dn_dt_bias)
    dn_l = dn_prep(conv_latent_rows(qkv_l, conv_qkv_w), br_l, dr_l, dn_a_log, dn_dt_bias)
    o_c, o_l = bidir_deltanet(dn_c, dn_l)
    y_dn_c = gated_rmsnorm(o_c, z_c, dn_onorm_w)
    y_dn_l = gated_rmsnorm(o_l, z_l, dn_onorm_w)
    xc_c = (dwconv_centred(rx_c, rg_conv_w) + rg_conv_b).astype(jnp.float32)
    xc_l = (conv_latent_rows(rx_l, rg_conv_w) + rg_conv_b).astype(jnp.float32)
    hr_c, hr_l = bidir_rglru(xc_c, xc_l, rg_wa, rg_ba, rg_wi, rg_bi, rg_lambda)
    y_rg_c = hr_c * jax.nn.gelu(ry_c.astype(jnp.float32))
    y_rg_l = hr_l * jax.nn.gelu(ry_l.astype(jnp.float32))
    y = jnp.concatenate([jnp.concatenate([y_dn_c, y_rg_c], axis=-1),
                         jnp.concatenate([y_dn_l, y_rg_l], axis=-1)], axis=1).astype(h_l.dtype) @ w_out
    return y[:, :n_ctx], y[:, n_ctx:]


def hierarchical_moe(h, wg, bg, we, be, w_gate, w_up, w_down):
    T = h.shape[0]
    hf = h.astype(jnp.float32)
    rows = jnp.arange(T)
    group_logits = hf @ wg.astype(jnp.float32) + bg.astype(jnp.float32)
    g_sel = jnp.argmax(group_logits, axis=-1)
    p_group = jax.nn.softmax(group_logits, axis=-1)[rows, g_sel]
    expert_logits = (hf @ we.astype(jnp.float32) + be.astype(jnp.float32)).reshape(
        T, N_GROUPS, EXPERTS_PER_GROUP)[rows, g_sel]
    top_val, top_idx = lax.top_k(expert_logits, TOP_K)
    top_w = jax.nn.softmax(top_val, axis=-1) * p_group[:, None]
    expert_id = g_sel[:, None] * EXPERTS_PER_GROUP + top_idx
    combine = jnp.einsum('tk,tke->te', top_w,
                         jax.nn.one_hot(expert_id, N_EXPERTS, dtype=jnp.float32)).astype(h.dtype)
    y = None
    for gi in range(N_GROUPS):
        sl = slice(gi * EXPERTS_PER_GROUP, (gi + 1) * EXPERTS_PER_GROUP)
        a = jnp.einsum('td,edf->tef', h, w_gate[sl])
        b = jnp.einsum('td,edf->tef', h, w_up[sl])
        part = jnp.einsum('tef,efd->td', jax.nn.silu(a) * b * combine[:, sl, None], w_down[sl])
        y = part if y is None else y + part
    return y


def setup_inputs(seed: int = 0) -> dict:
    key = jax.random.key(seed)
    ks = jax.random.split(key, 28)
    f32 = jnp.float32
    nrm = lambda k, shape, s: jax.random.normal(k, shape, f32) * s
    x = nrm(ks[0], (BATCH, SEQ, D_MODEL), 1.0)
    c = nrm(ks[1], (BATCH, D_MODEL), 1.0)
    ctx = nrm(ks[2], (BATCH, CTX_LEN, D_MODEL), 1.0)
    c_ctx = nrm(ks[3], (D_MODEL,), 1.0)
    w_ada = nrm(ks[4], (DEPTH, D_MODEL, 6 * D_MODEL), 0.5 * D_MODEL ** -0.5)
    b_ada = nrm(ks[5], (DEPTH, 6 * D_MODEL), 0.02)
    w_in = nrm(ks[6], (DEPTH, D_MODEL, D_IN), D_MODEL ** -0.5)
    conv_qkv_w = nrm(ks[7], (DEPTH, CONV_K, 3 * DN_WIDTH), CONV_K ** -0.5)
    dn_a_log = jnp.log(jax.random.uniform(ks[8], (DEPTH, 2, DN_HEADS), f32, 1.0, 16.0))
    dt = jnp.exp(jax.random.uniform(ks[9], (DEPTH, 2, DN_HEADS), f32, math.log(1e-3), math.log(1e-1)))
    dn_dt_bias = dt + jnp.log(-jnp.expm1(-dt))
    dn_onorm_w = 1.0 + nrm(ks[10], (DEPTH, DN_HEAD_DIM), 0.02)
    rg_conv_w = nrm(ks[11], (DEPTH, CONV_K, RG_WIDTH), CONV_K ** -0.5)
    rg_conv_b = nrm(ks[12], (DEPTH, RG_WIDTH), 0.02)
    rg_wa = nrm(ks[13], (DEPTH, 2, RG_BLOCKS, RG_BLOCK_DIM, RG_BLOCK_DIM), RG_BLOCK_DIM ** -0.5)
    rg_ba = nrm(ks[14], (DEPTH, 2, RG_WIDTH), 0.02)
    rg_wi = nrm(ks[15], (DEPTH, 2, RG_BLOCKS, RG_BLOCK_DIM, RG_BLOCK_DIM), RG_BLOCK_DIM ** -0.5)
    rg_bi = nrm(ks[16], (DEPTH, 2, RG_WIDTH), 0.02)
    a_pow = jax.random.uniform(ks[17], (DEPTH, 2, RG_WIDTH), f32, 0.9, 0.999)
    a0 = a_pow ** (1.0 / RG_C)
    rg_lambda = jnp.log(a0) - jnp.log1p(-a0)
    w_out = nrm(ks[18], (DEPTH, D_MIX, D_MODEL), DEEP_BETA * D_MIX ** -0.5)
    ln_g = 1.0 + nrm(ks[19], (DEPTH, 2, D_MODEL), 0.02)
    ln_b = nrm(ks[20], (DEPTH, 2, D_MODEL), 0.02)
    router_wg = nrm(ks[21], (DEPTH, D_MODEL, N_GROUPS), D_MODEL ** -0.5)
    router_bg = nrm(ks[22], (DEPTH, N_GROUPS), 0.01)
    router_we = nrm(ks[23], (DEPTH, D_MODEL, N_EXPERTS), D_MODEL ** -0.5)
    router_be = nrm(ks[24], (DEPTH, N_EXPERTS), 0.01)
    w_e_gate = nrm(ks[25], (DEPTH, N_EXPERTS, D_MODEL, D_EXPERT), D_MODEL ** -0.5)
    w_e_up = nrm(ks[26], (DEPTH, N_EXPERTS, D_MODEL, D_EXPERT), D_MODEL ** -0.5)
    w_e_down = nrm(ks[27], (DEPTH, N_EXPERTS, D_EXPERT, D_MODEL), DEEP_BETA * D_EXPERT ** -0.5)
    return {"x": x, "c": c, "ctx": ctx, "c_ctx": c_ctx, "w_ada": w_ada, "b_ada": b_ada, "w_in": w_in,
            "conv_qkv_w": conv_qkv_w, "dn_a_log": dn_a_log, "dn_dt_bias": dn_dt_bias,
            "dn_onorm_w": dn_onorm_w, "rg_conv_w": rg_conv_w, "rg_conv_b": rg_conv_b, "rg_wa": rg_wa,
            "rg_ba": rg_ba, "rg_wi": rg_wi, "rg_bi": rg_bi, "rg_lambda": rg_lambda, "w_out": w_out,
            "ln_g": ln_g, "ln_b": ln_b, "router_wg": router_wg, "router_bg": router_bg,
            "router_we": router_we, "router_be": router_be, "w_e_gate": w_e_gate, "w_e_up": w_e_up,
            "w_e_down": w_e_down}


def reference(x, c, ctx, c_ctx, w_ada, b_ada, w_in, conv_qkv_w, dn_a_log, dn_dt_bias, dn_onorm_w,
              rg_conv_w, rg_conv_b, rg_wa, rg_ba, rg_wi, rg_bi, rg_lambda, w_out, ln_g, ln_b,
              router_wg, router_bg, router_we, router_be, w_e_gate, w_e_up, w_e_down):
    B, L, D = x.shape
    n_ctx = ctx.shape[1]
    for l in range(DEPTH):
        last = l == DEPTH - 1
        mod_l = (jax.nn.silu(c) @ w_ada[l] + b_ada[l])[:, None, :]
        mod_c = (jax.nn.silu(c_ctx) @ w_ada[l] + b_ada[l])[None, None, :]
        sh1_l, sc1_l, gt1_l, sh2_l, sc2_l, gt2_l = jnp.split(mod_l, 6, axis=-1)
        sh1_c, sc1_c, gt1_c, sh2_c, sc2_c, gt2_c = jnp.split(mod_c, 6, axis=-1)
        u_c, u_l = hybrid_mixer(modulate(ctx, sh1_c, sc1_c), modulate(x, sh1_l, sc1_l), w_in[l],
                                conv_qkv_w[l], dn_a_log[l], dn_dt_bias[l], dn_onorm_w[l], rg_conv_w[l],
                                rg_conv_b[l], rg_wa[l], rg_ba[l], rg_wi[l], rg_bi[l], rg_lambda[l], w_out[l])
        x = post_norm(DEEP_ALPHA * x + gt1_l * u_l, ln_g[l, 0], ln_b[l, 0])
        moe_args = (router_wg[l], router_bg[l], router_we[l], router_be[l], w_e_gate[l], w_e_up[l], w_e_down[l])
        if last:
            f_l = hierarchical_moe(modulate(x, sh2_l, sc2_l).reshape(B * L, D), *moe_args).reshape(B, L, D)
        else:
            ctx = post_norm(DEEP_ALPHA * ctx + gt1_c * u_c, ln_g[l, 0], ln_b[l, 0])
            h = jnp.concatenate([modulate(ctx, sh2_c, sc2_c), modulate(x, sh2_l, sc2_l)], axis=1)
            f = hierarchical_moe(h.reshape(B * (n_ctx + L), D), *moe_args).reshape(B, n_ctx + L, D)
            f_c, f_l = f[:, :n_ctx], f[:, n_ctx:]
            ctx = post_norm(DEEP_ALPHA * ctx + gt2_c * f_c, ln_g[l, 1], ln_b[l, 1])
        x = post_norm(DEEP_ALPHA * x + gt2_l * f_l, ln_g[l, 1], ln_b[l, 1])
    return x
```

```python
import numpy as np
from contextlib import ExitStack
import concourse.bass as bass
import concourse.mybir as mybir
from concourse.bass_utils import run_bass_kernel_spmd

F32 = mybir.dt.float32
BF16 = mybir.dt.bfloat16
AF = mybir.ActivationFunctionType
ALU = mybir.AluOpType

T = 2304
NT = 18
D = 1024
DIN = 3088
NCTX = 256
ALPHA = float((2 * 2) ** 0.25)
LN_EPS = 1e-5
NORM_EPS = 1e-6
NEG = -30000.0
ENGS = ["pe", "act", "dve", "pool", "sp"]
(C_ID, C_TRIF, C_TRIB, C_NTRIF, C_NTRIB, C_MSF, C_MSB, C_MITF, C_MITB, C_ONES) = range(10)


class Sched:
    def __init__(self, nc, stack):
        self.nc = nc
        self.stack = stack
        self.q = {e: [] for e in ENGS}
        self.sems = {}
        self.semval = {}
        self.waited = {}
        self.last_w = {}
        self.readers = {}
        self.ninstr = 0
        for e in ENGS:
            if e != "sp":
                self._sem("eng_" + e)

    def _sem(self, name):
        if name not in self.sems:
            self.sems[name] = self.stack.enter_context(self.nc.semaphore(name))
            self.semval[name] = 0
        return self.sems[name]

    @staticmethod
    def _norm(reads, writes):
        r2 = []
        w2 = []
        for k in reads:
            if k.startswith("ps"):
                w2.append(k.split(":")[0])
            else:
                r2.append(k)
        for k in writes:
            w2.append(k.split(":")[0] if k.startswith("ps") else k)
        return r2, w2

    def _deps(self, eng, reads, writes):
        toks = []
        for k in reads:
            t = self.last_w.get(k)
            if t is not None:
                toks.append(t)
        for k in writes:
            t = self.last_w.get(k)
            if t is not None:
                toks.append(t)
            toks.extend(self.readers.get(k, ()))
        best = {}
        for (s, v) in toks:
            if eng == "pe" and s == "eng_pe":
                continue
            if self.waited.get((eng, s), 0) < v and best.get(s, 0) < v:
                best[s] = v
        for s, v in best.items():
            self.waited[(eng, s)] = v
        return list(best.items())

    def _commit(self, tok, reads, writes):
        for k in reads:
            self.readers.setdefault(k, []).append(tok)
        for k in writes:
            self.last_w[k] = tok
            self.readers[k] = []

    def op(self, eng, fn, reads=(), writes=()):
        reads, writes = self._norm(reads, writes)
        waits = self._deps(eng, reads, writes)
        s = "eng_" + eng
        self.semval[s] += 1
        tok = (s, self.semval[s])
        self.q[eng].append((waits, fn, (s, 1)))
        self._commit(tok, reads, writes)
        self.ninstr += 1
        return tok

    def dma(self, eng, fn, slot, reads=(), writes=()):
        s = "dma_" + slot
        self._sem(s)
        reads, writes = self._norm(reads, writes)
        waits = self._deps(eng, reads, writes)
        self.semval[s] += 16
        tok = (s, self.semval[s])
        self.q[eng].append((waits, fn, (s, 16)))
        self._commit(tok, reads, writes)
        self.ninstr += 1
        return tok

    def barrier(self):
        allt = [(s, v) for s, v in self.semval.items() if v > 0]
        for e in ENGS:
            waits = []
            for (s, v) in allt:
                if self.waited.get((e, s), 0) < v:
                    self.waited[(e, s)] = v
                    waits.append((s, v))
            if waits:
                self.q[e].append((waits, None, None))

    def emit(self):
        self.barrier()
        nc = self.nc
        sems = self.sems
        q = self.q
        self.q = {e: [] for e in ENGS}

        def run(engobj, lst):
            for waits, fn, inc in lst:
                for (s, v) in waits:
                    engobj.wait_ge(sems[s], v)
                if fn is not None:
                    ins = fn(engobj)
                    ins.then_inc(sems[inc[0]], inc[1])

        with nc.Block() as block:
            @block.sync
            def _(e):
                run(e, q["sp"])

            @block.tensor
            def _(e):
                run(e, q["pe"])

            @block.scalar
            def _(e):
                run(e, q["act"])

            @block.vector
            def _(e):
                run(e, q["dve"])

            @block.gpsimd
            def _(e):
                run(e, q["pool"])


def rr(gens):
    gens = list(gens)
    while gens:
        nxt = []
        for g in gens:
            try:
                next(g)
                nxt.append(g)
            except StopIteration:
                pass
        gens = nxt


class KB:
    def __init__(self, nc, cfg):
        self.nc = nc
        self.cfg = cfg
        self.dbg = set(cfg.get("dbg", ()))
        self.d = {}
        self.dbg_out = {}

    def sbt(self, st, name, shape, dt=F32):
        self.uid = getattr(self, "uid", 0) + 1
        return st.enter_context(self.nc.sbuf_tensor(f"{name}_u{self.uid}", shape, dt))

    def mm(self, out, lhsT, rhs, start=True, stop=True, r=(), w=()):
        return self.S.op("pe", lambda e: e.matmul(out, lhsT=lhsT, rhs=rhs, start=start, stop=stop), r, w)

    def tr(self, out, in_, r=(), w=()):
        ident = self.cm[C_ID]
        return self.S.op("pe", lambda e: e.transpose(out, in_, ident), list(r), w)

    def act(self, out, in_, func, r=(), w=(), **kw):
        return self.S.op("act", lambda e: e.activation(out=out, in_=in_, func=func, **kw), r, w)

    def ts(self, eng, out, in0, s1, s2, op0, op1=None, r=(), w=()):
        if op1 is None:
            return self.S.op(eng, lambda e: e.tensor_scalar(out=out, in0=in0, scalar1=s1, scalar2=None, op0=op0), r, w)
        return self.S.op(eng, lambda e: e.tensor_scalar(out=out, in0=in0, scalar1=s1, scalar2=s2, op0=op0, op1=op1), r, w)

    def tt(self, eng, out, in0, in1, op, r=(), w=()):
        return self.S.op(eng, lambda e: e.tensor_tensor(out=out, in0=in0, in1=in1, op=op), r, w)

    def stt(self, eng, out, in0, scalar, in1, op0, op1, r=(), w=()):
        return self.S.op(eng, lambda e: e.scalar_tensor_tensor(out=out, in0=in0, scalar=scalar, in1=in1, op0=op0, op1=op1), r, w)

    def cp(self, eng, out, in_, r=(), w=()):
        if eng == "act":
            return self.S.op("act", lambda e: e.activation(out=out, in_=in_, func=AF.Copy), r, w)
        return self.S.op(eng, lambda e: e.tensor_copy(out=out, in_=in_), r, w)

    def memset(self, eng, ap, val, w=()):
        return self.S.op(eng, lambda e: e.memset(ap, val), (), w)

    def ld(self, q, out, in_, slot, r=(), w=()):
        return self.S.dma(q, lambda e: e.dma_start(out=out, in_=in_), slot, r, w)

    def dump(self, name, ap, key, shape, dt=F32):
        if name not in self.dbg:
            return
        o = self.nc.dram_tensor("d_" + name, shape, dt, kind="ExternalOutput").ap()
        self.dbg_out[name] = o
        self.S.dma("sp", lambda e: e.dma_start(out=o, in_=ap), "dbg_" + name, [key], ["dbgdram_" + name])

    @staticmethod
    def pipe(makers, ni=2, skew=4):
        makers = list(makers)
        active = []
        nxt = 0
        since = skew
        while nxt < len(makers) or active:
            if nxt < len(makers) and len(active) < ni and (since >= skew or not active):
                active.append(makers[nxt]())
                nxt += 1
                since = 0
            for g in list(active):
                try:
                    next(g)
                except StopIteration:
                    active.remove(g)
            since += 1

    def ln_stats_g(self, x_ap, xkey, out):
        i = self.stat_i % 8
        self.stat_i += 1
        st6 = self.stat6[:, i, :, :]
        mv = self.statmv[:, i, :]
        k6 = f"st6_{i}"
        kmv = f"stmv_{i}"
        for g in range(2):
            self.S.op("dve", lambda e, g=g: e.bn_stats(out=st6[:, g, :], in_=x_ap[:, g * 512:(g + 1) * 512]), [xkey], [k6])
        self.S.op("dve", lambda e: e.bn_aggr(out=mv[:, 0:2], in_=st6.rearrange("p a b -> p (a b)")), [k6], [kmv])
        yield
        self.act(mv[:, 2:3], mv[:, 1:2], AF.Sqrt, [kmv, "epsc"], [kmv], bias=self.epsc[:, 0:1], scale=1.0)
        yield
        self.S.op("dve", lambda e: e.reciprocal(out=mv[:, 3:4], in_=mv[:, 2:3]), [kmv], [kmv])
        out["mean"], out["rstd"], out["k"] = mv[:, 0:1], mv[:, 3:4], kmv

    def ln_stats(self, x_ap, xkey, tag, eps=LN_EPS):
        i = self.stat_i % 8
        self.stat_i += 1
        st6 = self.stat6[:, i, :, :]
        mv = self.statmv[:, i, :]
        k6 = f"st6_{i}"
        kmv = f"stmv_{i}"
        for g in range(2):
            self.S.op("dve", lambda e, g=g: e.bn_stats(out=st6[:, g, :], in_=x_ap[:, g * 512:(g + 1) * 512]), [xkey], [k6])
        self.S.op("dve", lambda e: e.bn_aggr(out=mv[:, 0:2], in_=st6.rearrange("p a b -> p (a b)")), [k6], [kmv])
        self.act(mv[:, 2:3], mv[:, 1:2], AF.Sqrt, [kmv], [kmv], bias=self.epsc[:, 0:1] if eps == LN_EPS else self.epsc[:, 1:2], scale=1.0)
        self.S.op("dve", lambda e: e.reciprocal(out=mv[:, 3:4], in_=mv[:, 2:3]), [kmv], [kmv])
        return mv[:, 0:1], mv[:, 3:4], kmv

    def conv(self, eng, out_t, in_t, w4, bias, rk, wk, wv=None):
        wv = wv or (lambda a: a)
        if bias is not None:
            self.ts(eng, wv(out_t[:, :]), in_t[:, :], w4[:, 2:3], bias, ALU.mult, ALU.add, r=rk, w=[wk])
        else:
            self.ts(eng, wv(out_t[:, :]), in_t[:, :], w4[:, 2:3], None, ALU.mult, r=rk, w=[wk])
        o3 = out_t[:, NCTX:].rearrange("p (r c) -> p r c", c=64)
        i3 = in_t[:, NCTX:].rearrange("p (r c) -> p r c", c=64)
        for (j, o) in ((0, -2), (1, -1), (3, 1)):
            lo = max(0, -o)
            hi = NCTX - max(0, o)
            self.stt(eng, wv(out_t[:, lo:hi]), in_t[:, lo + o:hi + o], w4[:, j:j + 1], out_t[:, lo:hi], ALU.mult, ALU.add,
                     r=list(rk) + [wk], w=[wk])
            hi = 64 - max(0, o)
            self.stt(eng, wv(o3[:, :, lo:hi]), i3[:, :, lo + o:hi + o], w4[:, j:j + 1], o3[:, :, lo:hi], ALU.mult, ALU.add,
                     r=list(rk) + [wk], w=[wk])

    def conv_g(self, eng, out_t, in_t, w4, bias, rk, wk, wv=None):
        wv = wv or (lambda a: a)
        if bias is not None:
            self.ts(eng, wv(out_t[:, :]), in_t[:, :], w4[:, 2:3], bias, ALU.mult, ALU.add, r=rk, w=[wk])
        else:
            self.ts(eng, wv(out_t[:, :]), in_t[:, :], w4[:, 2:3], None, ALU.mult, r=rk, w=[wk])
        yield
        o3 = out_t[:, NCTX:].rearrange("p (r c) -> p r c", c=64)
        i3 = in_t[:, NCTX:].rearrange("p (r c) -> p r c", c=64)
        for (j, o) in ((0, -2), (1, -1), (3, 1)):
            lo = max(0, -o)
            hi = NCTX - max(0, o)
            self.stt(eng, wv(out_t[:, lo:hi]), in_t[:, lo + o:hi + o], w4[:, j:j + 1], out_t[:, lo:hi], ALU.mult, ALU.add,
                     r=list(rk) + [wk], w=[wk])
            hi = 64 - max(0, o)
            self.stt(eng, wv(o3[:, :, lo:hi]), i3[:, :, lo + o:hi + o], w4[:, j:j + 1], o3[:, :, lo:hi], ALU.mult, ALU.add,
                     r=list(rk) + [wk], w=[wk])
            yield

    def proj_fm_g(self, dst, dkey, wt, wkey, c0, wv=None):
        wv_ = wv or (lambda a: a)
        for blk in range(5):
            b0 = blk * 512
            n = min(512, T - b0)
            pb = self.PS[self.ps_rot % 4]
            pk = f"ps{self.ps_rot % 4}"
            self.ps_rot += 1
            for kc in range(8):
                self.mm(pb[:, 0:n], wt[:, kc, c0:c0 + 128], self.hT[:, kc, b0:b0 + n], kc == 0, kc == 7,
                        r=[wkey] + [f"hT:{t}" for t in range(b0 // 128, (b0 + n) // 128)], w=[pk])
            self.cp("act", wv_(dst[:, b0:b0 + n]), pb[:, 0:n], r=[pk], w=[dkey])
            yield

    def proj_fm(self, dst, dkey, wt, wkey, c0, r_extra=(), evac="act", func=None, wv=None):
        for blk in range(5):
            b0 = blk * 512
            n = min(512, T - b0)
            pb = self.PS[self.ps_rot % 4]
            pk = f"ps{self.ps_rot % 4}"
            self.ps_rot += 1
            for kc in range(8):
                self.mm(pb[:, 0:n], wt[:, kc, c0:c0 + 128], self.hT[:, kc, b0:b0 + n], kc == 0, kc == 7,
                        r=[wkey] + [f"hT:{t}" for t in range(b0 // 128, (b0 + n) // 128)] + list(r_extra), w=[pk])
            wv_ = wv or (lambda a: a)
            if func is None:
                self.cp("act", wv_(dst[:, b0:b0 + n]), pb[:, 0:n], r=[pk], w=[dkey])
            else:
                self.act(wv_(dst[:, b0:b0 + n]), pb[:, 0:n], func, r=[pk], w=[dkey])

    def build(self):
        nc = self.nc
        cfg = self.cfg
        d = self.d

        def din(name, shape):
            d[name] = nc.dram_tensor(name, shape, F32, kind="ExternalInput").ap()

        din("xin", [2, T, D]); din("cT", [128, 8, 3]); din("w_ada", [2, 1024, 6144]); din("b_ada", [2, 6144])
        din("b_adaT", [2, 128, 48]); din("w_in", [2, 1024, DIN]); din("cqw", [2, 128, 12, 4]); din("rgcw", [2, 128, 4, 4])
        din("rgcb", [2, 128, 4]); din("alog", [2, 8]); din("dtb", [2, 8]); din("onw", [2, 128])
        din("wbd", [2, 128, 16, 128]); din("rgb", [2, 128, 16]); din("lam", [2, 128, 8])
        din("w_out", [2, 1024, 1024]); din("ln_g", [2, 2, 1024]); din("ln_b", [2, 2, 1024]); din("wr", [2, 1024, 36])
        din("br", [2, 36]); din("weg", [2, 32, 1024, 256]); din("weu", [2, 32, 1024, 256]); din("wed", [2, 32, 256, 1024])
        din("cmat", [128, 10, 128])
        d["out"] = nc.dram_tensor("out", [2, 2048, D], F32, kind="ExternalOutput").ap()
        d["xs1"] = nc.dram_tensor("xs1", [2, T, D], F32, kind="Internal").ap()
        d["xs2"] = nc.dram_tensor("xs2", [2, T, D], F32, kind="Internal").ap()
        d["modrow"] = nc.dram_tensor("modrow", [2, 3, 6144], F32, kind="Internal").ap()
        d["combD"] = nc.dram_tensor("combD", [32, T], BF16, kind="Internal").ap()

        with ExitStack() as outer:
            self.S = Sched(nc, outer)
            S = self.S
            self.cmat = self.sbt(outer, "cmat_sb", [128, 10, 128])
            self.cm = [self.cmat[:, i, :] for i in range(10)]
            self.modT = self.sbt(outer, "modT", [128, 2, 48, 3])
            self.onep = self.sbt(outer, "onep", [128, 2, 48, 3])
            self.stat6 = self.sbt(outer, "stat6", [128, 8, 2, 6])
            self.statmv = self.sbt(outer, "statmv", [128, 8, 4])
            self.epsc = self.sbt(outer, "epsc", [128, 2])
            self.stat_i = 0
            self.ps_rot = 0
            self.PS = [outer.enter_context(nc.psum_tensor(f"psb{i}", [128, 512], F32)) for i in range(8)]
            self.ld("sp", self.cmat[:], d["cmat"][:, :, :], "cmat", w=["ident", "cmat"])
            self.cmatr = self.sbt(outer, "cmatr_sb", [128, 10, 128])
            self.S.op("dve", lambda e: e.tensor_copy(out=self.cmatr[:].bitcast(mybir.dt.float32r), in_=self.cmat[:]), ["cmat"], ["cmatr"])
            self.cmr = [self.cmatr[:, i, :].bitcast(mybir.dt.float32r) for i in range(10)]
            self.memset("pool", self.epsc[:, 0:1], LN_EPS, w=["epsc"])
            self.memset("pool", self.epsc[:, 1:2], NORM_EPS, w=["epsc"])
            self.stage0()
            S.emit()
            if cfg.get("stop") == "stage0":
                return
            for bi in range(cfg.get("nb", 2)):
                for l in range(cfg.get("nl", 2)):
                    self.layer(bi, l)
            S.emit()

    def stage0(self):
        nc, S, d = self.nc, self.S, self.d
        with ExitStack() as ph:
            cT = self.sbt(ph, "cT_sb", [128, 8, 3])
            scT = self.sbt(ph, "scT", [128, 8, 3])
            brow = self.sbt(ph, "brow", [3, 6144])
            bT = self.sbt(ph, "bT", [128, 48])
            wa = [self.sbt(ph, f"wa{i}", [128, 8, 512]) for i in range(2)]
            rowsb = [self.sbt(ph, f"rowsb{i}", [3, 512]) for i in range(2)]
            self.ld("sp", cT[:], d["cT"][:, :, :], "cT", w=["cT"])
            self.act(scT[:], cT[:], AF.Silu, r=["cT"], w=["scT"])
            for l in range(2):
                self.ld("sp", brow[:], d["b_ada"][l, :].partition_broadcast(3), "brow", w=["brow"])
                self.ld("sp", bT[:], d["b_adaT"][l, :, :], "bT", w=["bT"])
                psM = self.PS[l]
                for piece in range(12):
                    s = piece % 2
                    self.ld("sp", wa[s][:], d["w_ada"][l, :, piece * 512:(piece + 1) * 512].rearrange("(kc p) n -> p kc n", p=128),
                            f"wa{s}", w=[f"wa{s}"])
                    psR = self.PS[2 + s]
                    for kc in range(8):
                        self.mm(psR[0:3, 0:512], scT[:, kc, :], wa[s][:, kc, :], kc == 0, kc == 7, r=[f"wa{s}", "scT"], w=[f"psR{s}"])
                    self.tt("dve", rowsb[s][:], psR[0:3, 0:512], brow[:, piece * 512:(piece + 1) * 512], ALU.add,
                            r=[f"psR{s}", "brow"], w=[f"rowsb{s}"])
                    self.ld("sp", d["modrow"][l, :, piece * 512:(piece + 1) * 512], rowsb[s][:], f"rowst{s}", r=[f"rowsb{s}"], w=["modrow"])
                    for fc in range(4):
                        ch = piece * 4 + fc
                        self.S.op("pe", lambda e, ch=ch, fc=fc, s=s, psM=psM: e.transpose(psM[:, ch * 3:(ch + 1) * 3], rowsb[s][0:3, fc * 128:(fc + 1) * 128],
                                                                              self.cm[C_ID][0:3, 0:3]),
                                  [f"rowsb{s}", "ident"], [f"psM{l}"])
                self.cp("dve", self.modT[:, l, :, :], psM[:, 0:144].rearrange("p (c j) -> p c j", j=3), r=[f"psM{l}"], w=["modT"])
                self.ts("dve", self.onep[:, l, :, :], self.modT[:, l, :, :], 1.0, None, ALU.add, r=["modT"], w=["onep"])
            self.dump("modT", self.modT[:], "modT", [128, 2, 48, 3])
            S.barrier()

    def layer(self, bi, l):
        nc, S, d, cfg = self.nc, self.S, self.d, self.cfg
        last = (l == 1)
        src = d["xin"] if l == 0 else d["xs2"]
        stop = cfg.get("stop")
        with ExitStack() as bl:
            self.hT = self.sbt(bl, "hT", [128, 8, T], BF16)
            self.combT = self.sbt(bl, "combT", [32, T], BF16)
            with ExitStack() as ml:
                self.yT = self.sbt(ml, "yT", [128, 8, T], BF16)
                self.sm = {nm: self.sbt(ml, "sm_" + nm, [128, NT, 8]) for nm in ("BETA", "NBETA", "EGC", "NEGC", "EDEC", "GLB", "G")}
                self.cqw = self.sbt(ml, "cqw_sb", [128, 12, 4])
                self.rgcw = self.sbt(ml, "rgcw_sb", [128, 4, 4])
                self.rgcb = self.sbt(ml, "rgcb_sb", [128, 4])
                self.dtb = self.sbt(ml, "dtb_bc", [128, 8])
                self.nA = self.sbt(ml, "nA_bc", [128, 8])
                self.onw = self.sbt(ml, "onw_bc", [128, 128])
                self.rgb = self.sbt(ml, "rgb_sb", [128, 16])
                self.c1 = self.sbt(ml, "c1_sb", [128, 8])
                self.wbd = self.sbt(ml, "wbd_bf", [128, 16, 128], BF16)
                self.wsm = self.sbt(ml, "wsm_bf", [128, 8, 16], BF16)
                self.phaseA(bi, l, src)
                S.emit()
                self.dump_seq("hT", self.hT, BF16)
                if stop == "A":
                    self.dump_sm(); S.emit(); return
                self.phaseB(bi, l)
                S.emit()
                if stop == "B":
                    self.dump_seq("yT", self.yT, BF16); S.emit(); return
                for h in cfg.get("heads", range(4)):
                    self.phaseC(bi, l, h)
                    S.emit()
                if stop == "C":
                    self.dump_seq("yT", self.yT, BF16); S.emit(); return
                self.phaseD(bi, l, src)
                S.emit()
            if stop == "D":
                self.dump_seq("hT", self.hT, BF16)
                if "combT" in self.dbg:
                    self.dump("combT", self.combT[:], "combT", [32, T], BF16)
                S.emit(); return
            self.phaseE(bi, l)
            S.emit()

    def dump_seq(self, name, tile, dt):
        if name in self.dbg:
            o = self.nc.dram_tensor("d_" + name, [128, 8, T], dt, kind="ExternalOutput").ap()
            self.dbg_out[name] = o
            self.dbg.discard(name)
            self.S.dma("sp", lambda e: e.dma_start(out=o, in_=tile[:]), "dbg_" + name, [], ["dbgdram_" + name])
            self.S.barrier()

    def dump_sm(self):
        for nm, t in self.sm.items():
            self.dump("sm_" + nm, t[:], "sm", [128, NT, 8])

    def phaseA(self, bi, l, src):
        nc, S, d = self.nc, self.S, self.d
        sm = self.sm
        with ExitStack() as ph:
            xt = [self.sbt(ph, f"xt{i}", [128, D]) for i in range(4)]
            xn = [self.sbt(ph, f"xn{i}", [128, D]) for i in range(4)]
            tmp = self.sbt(ph, "smtmp", [128, 4, 4, 8])
            alog = self.sbt(ph, "alog_bc", [128, 8])
            lam = self.sbt(ph, "lam_sb", [128, 8])
            self.ld("sp", self.cqw[:], d["cqw"][l, :, :, :], "cqw", w=["cqw"])
            self.ld("sp", self.rgcw[:], d["rgcw"][l, :, :, :], "rgcw", w=["rgcw"])
            self.ld("sp", self.rgcb[:], d["rgcb"][l, :, :], "rgcb", w=["rgcb"])
            self.ld("sp", self.dtb[:], d["dtb"][l, :].partition_broadcast(128), "dtb", w=["dtb"])
            self.ld("sp", alog[:], d["alog"][l, :].partition_broadcast(128), "alog", w=["alog"])
            self.ld("sp", self.onw[:], d["onw"][l, :].partition_broadcast(128), "onw", w=["onw"])
            self.ld("sp", self.rgb[:], d["rgb"][l, :, :], "rgb", w=["rgb"])
            self.ld("sp", lam[:], d["lam"][l, :, :], "lam", w=["lam"])
            self.ld("pool", self.wbd[:], d["wbd"][l, :, :, :], "wbd", w=["wbd"])
            self.ld("pool", self.wsm[:], d["w_in"][l, :, 2048:2064].rearrange("(kc p) n -> p kc n", p=128), "wsm", w=["wsm"])
            self.act(self.nA[:], alog[:], AF.Exp, r=["alog"], w=["nA"])
            self.ts("dve", self.nA[:], self.nA[:], -1.0, None, ALU.mult, r=["nA"], w=["nA"])
            self.act(self.c1[:], lam[:], AF.Exp, r=["lam"], w=["c1"], scale=-1.0)
            self.act(self.c1[:], self.c1[:], AF.Ln, r=["c1"], w=["c1"], bias=1.0)
            self.ts("dve", self.c1[:], self.c1[:], -8.0, None, ALU.mult, r=["c1"], w=["c1"])
            def tileA(t):
                j = 2 if t < 2 else bi
                s3 = t % 4
                s2 = t % 4
                self.ld("sp", xt[s3][:], src[bi, t * 128:(t + 1) * 128, :], f"xt{s3}", w=[f"xt{s3}"])
                st = {}
                yield from self.ln_stats_g(xt[s3], f"xt{s3}", st)
                self.ts("dve", xn[s2][:], xt[s3][:], st["mean"], st["rstd"], ALU.subtract, ALU.mult, r=[f"xt{s3}", st["k"]], w=[f"xn{s2}"])
                yield
                for kc in range(8):
                    pb = self.PS[s2 * 2 + kc // 4]
                    pk = f"ps{s2 * 2 + kc // 4}:{kc % 4}"
                    self.tr(pb[:, (kc % 4) * 128:(kc % 4 + 1) * 128], xn[s2][:, kc * 128:(kc + 1) * 128], r=[f"xn{s2}", "ident"], w=[pk])
                yield
                for kc in range(8):
                    pb = self.PS[s2 * 2 + kc // 4]
                    pk = f"ps{s2 * 2 + kc // 4}:{kc % 4}"
                    self.act(self.hT[:, kc, t * 128:(t + 1) * 128], pb[:, (kc % 4) * 128:(kc % 4 + 1) * 128], AF.Identity,
                             r=[pk, "onep", "modT"], w=[f"hT:{t}"], scale=self.onep[:, l, 8 + kc, j:j + 1], bias=self.modT[:, l, kc, j:j + 1])
                yield
                p16 = self.PS[s2 * 2]
                k16 = f"ps{s2 * 2}"
                for kc in range(8):
                    self.mm(p16[:, 0:16], self.hT[:, kc, t * 128:(t + 1) * 128], self.wsm[:, kc, :], kc == 0, kc == 7,
                            r=[f"hT:{t}", "wsm"], w=[k16])
                yield
                smk = f"sm:{t}"
                tk = f"smtmp{s2}"
                self.act(sm["BETA"][:, t, :], p16[:, 0:8], AF.Sigmoid, r=[k16], w=[smk])
                self.tt("dve", tmp[:, s2, 0, :], p16[:, 8:16], self.dtb[:], ALU.add, r=[k16, "dtb"], w=[tk])
                yield
                self.ts("dve", sm["NBETA"][:, t, :], sm["BETA"][:, t, :], -1.0, None, ALU.mult, r=[smk], w=[smk])
                self.act(tmp[:, s2, 1, :], tmp[:, s2, 0, :], AF.Exp, r=[tk], w=[tk])
                yield
                self.act(tmp[:, s2, 2, :], tmp[:, s2, 1, :], AF.Ln, r=[tk], w=[tk], bias=1.0)
                yield
                self.tt("dve", sm["G"][:, t, :], tmp[:, s2, 2, :], self.nA[:], ALU.mult, r=[tk, "nA"], w=[smk])
                yield
                self.mm(p16[:, 16:20], self.cm[C_TRIF], sm["G"][:, t, 0:4], r=[smk, "cmat"], w=[k16])
                self.mm(p16[:, 20:24], self.cm[C_TRIB], sm["G"][:, t, 4:8], r=[smk, "cmat"], w=[k16])
                self.mm(p16[:, 32:40], self.cm[C_ONES], sm["G"][:, t, :], r=[smk, "cmat"], w=[k16])
                yield
                self.act(sm["EGC"][:, t, :], p16[:, 16:24], AF.Exp, r=[k16], w=[smk])
                self.act(sm["GLB"][:, t, :], p16[:, 32:40], AF.Exp, r=[k16], w=[smk])
                self.cp("act", tmp[:, s2, 3, :], p16[:, 16:24], r=[k16], w=[tk])
                yield
                self.ts("dve", sm["NEGC"][:, t, :], sm["EGC"][:, t, :], -1.0, None, ALU.mult, r=[smk], w=[smk])
                self.tt("dve", tmp[:, s2, 3, :], p16[:, 32:40], tmp[:, s2, 3, :], ALU.subtract, r=[k16, tk], w=[tk])
                yield
                self.act(sm["EDEC"][:, t, :], tmp[:, s2, 3, :], AF.Exp, r=[tk], w=[smk])
                yield

            self.pipe([(lambda t=t: tileA(t)) for t in range(NT)], ni=4, skew=4)
            S.barrier()

    def phaseB(self, bi, l):
        nc, S, d = self.nc, self.S, self.d
        with ExitStack() as ph:
            wrg = [self.sbt(ph, f"wrg{i}", [128, 8, 256], BF16) for i in range(2)]
            RAW = self.sbt(ph, "RAW", [128, T])
            XC = self.sbt(ph, "XC", [128, T])
            XCB = self.sbt(ph, "XCB", [128, T], BF16)
            Rt = self.sbt(ph, "Rt", [128, T])
            It = self.sbt(ph, "It", [128, T])
            A2 = self.sbt(ph, "A2", [128, T])
            H = [self.sbt(ph, f"H{i}", [128, T]) for i in range(2)]
            for ch in range(4):
                s = ch % 2
                wk = f"wrg{s}"
                for part, c0 in ((0, 2064 + ch * 128), (1, 2576 + ch * 128)):
                    self.ld("pool", wrg[s][:, :, part * 128:(part + 1) * 128],
                            d["w_in"][l, :, c0:c0 + 128].rearrange("(kc p) n -> p kc n", p=128), f"wrg{s}_{part}", w=[f"{wk}:{part}"])
                self.proj_fm(RAW, "RAW", wrg[s], f"{wk}:0", 0)
                self.conv("dve", XC, RAW, self.rgcw[:, ch, :], self.rgcb[:, ch:ch + 1], ["RAW", "rgcw", "rgcb"], "XC")
                self.cp("act", XCB[:], XC[:], r=["XC"], w=["XCB"])
                if ch == 0:
                    self.dump("xc0", XC[:], "XC", [128, T])
                for dr in range(2):
                    for gate, dst, dk in ((0, Rt, "Rt"), (1, It, "It")):
                        idx = (dr * 2 + gate) * 4 + ch
                        for blk in range(5):
                            b0 = blk * 512
                            n = min(512, T - b0)
                            pb = self.PS[self.ps_rot % 4]
                            pk = f"ps{self.ps_rot % 4}"
                            self.ps_rot += 1
                            self.mm(pb[:, 0:n], self.wbd[:, idx, :], XCB[:, b0:b0 + n], r=["wbd", "XCB"], w=[pk])
                            self.act(dst[:, b0:b0 + n], pb[:, 0:n], AF.Sigmoid, r=[pk, "rgb"], w=[dk], bias=self.rgb[:, idx:idx + 1])
                    self.act(Rt[:], Rt[:], AF.Exp, r=["Rt", "c1"], w=["Rt"], scale=self.c1[:, dr * 4 + ch:dr * 4 + ch + 1])
                    self.act(A2[:], Rt[:], AF.Square, r=["Rt"], w=["A2"])
                    self.act(A2[:], A2[:], AF.Sqrt, r=["A2"], w=["A2"], scale=-1.0, bias=1.0)
                    self.tt("dve", It[:], It[:], XC[:], ALU.mult, r=["It", "XC"], w=["It"])
                    self.tt("dve", It[:], It[:], A2[:], ALU.mult, r=["It", "A2"], w=["It"])
                    hk = f"H{dr}"
                    if dr == 0:
                        S.op("dve", lambda e: e.tensor_tensor_scan(out=H[0][:, :], data0=Rt[:, :], data1=It[:, :], initial=0.0,
                                                                   op0=ALU.mult, op1=ALU.add), ["Rt", "It"], [hk])
                    else:
                        S.op("dve", lambda e: e.tensor_tensor_scan(out=H[1][:, 0:NCTX][:, ::-1], data0=Rt[:, 0:NCTX][:, ::-1],
                                                                   data1=It[:, 0:NCTX][:, ::-1], initial=0.0,
                                                                   op0=ALU.mult, op1=ALU.add), ["Rt", "It"], [hk])
                        S.op("dve", lambda e: e.tensor_tensor_scan(out=H[1][:, NCTX:T][:, ::-1], data0=Rt[:, NCTX:T][:, ::-1],
                                                                   data1=It[:, NCTX:T][:, ::-1], initial=H[1][:, 0:1],
                                                                   op0=ALU.mult, op1=ALU.add), ["Rt", "It", hk], [hk])
                self.tt("dve", H[0][:], H[0][:], H[1][:], ALU.add, r=["H0", "H1"], w=["H0"])
                if ch == 0:
                    self.dump("hr0", H[0][:], "H0", [128, T])
                self.proj_fm(RAW, "RAW", wrg[s], f"{wk}:1", 128, func=AF.Gelu_apprx_tanh)
                self.tt("dve", self.yT[:, 4 + ch, :], RAW[:], H[0][:], ALU.mult, r=["RAW", "H0"], w=[f"yT:{4 + ch}"])
            S.barrier()

    def phaseC(self, bi, l, h):
        nc, S, d = self.nc, self.S, self.d
        sm = self.sm
        PS = self.PS
        cm = self.cm
        cmr = self.cmr
        r_ = lambda ap: ap
        cmr = cm
        F32R = mybir.dt.float32r
        rr_ = lambda ap: ap.bitcast(F32R)
        with ExitStack() as ph:
            wq = self.sbt(ph, "wq", [128, 8, 512], BF16)
            RAW = self.sbt(ph, "RAWc", [128, T])
            CV = self.sbt(ph, "CVc", [128, T])
            QT = self.sbt(ph, "QT", [128, T])
            KT = self.sbt(ph, "KT", [128, T])
            KTOK = self.sbt(ph, "KTOK", [128, NT, 128])
            VTOK = self.sbt(ph, "VTOK", [128, NT, 128])
            QTOK = self.sbt(ph, "QTOK", [128, NT, 128])
            OACC = self.sbt(ph, "OACC", [128, NT, 128])
            Sst = [self.sbt(ph, f"Sst{i}", [128, 128]) for i in range(2)]
            RING = [[self.sbt(ph, f"RING_{dr}_{i}", [128, 640]) for i in range(3)] for dr in range(2)]
            for part, c0 in enumerate((h * 128, 512 + h * 128, 1024 + h * 128, 1536 + h * 128)):
                self.ld("pool", wq[:, :, part * 128:(part + 1) * 128],
                        d["w_in"][l, :, c0:c0 + 128].rearrange("(kc p) n -> p kc n", p=128), f"wq_{part}", w=[f"wq:{part}"])
            flat = lambda tl: tl[:].rearrange("p a b -> p (a b)")
            KTOKf, VTOKf, QTOKf, OACCf = flat(KTOK), flat(VTOK), flat(QTOK), flat(OACC)

            def prep_chain(which, raw, kraw, cvt, kcv, dst, kdst, wv_s, wv_d):
                yield from self.proj_fm_g(raw, kraw, wq, f"wq:{which}", which * 128, wv=wv_s)
                yield from self.conv_g("dve", cvt, raw, self.cqw[:, which * 4 + h, :], None, [kraw, "cqw"], kcv, wv=wv_s)
                self.act(wv_d(dst[:, :]), cvt[:, :], AF.Silu, r=[kcv], w=[kdst])
                yield
                if which < 2:
                    self.tt("pool", wv_s(cvt[:, :]), dst[:, :], dst[:, :], ALU.mult, r=[kdst], w=[kcv])
                    yield
                    for blk in range(5):
                        b0 = blk * 512
                        n = min(512, T - b0)
                        pb = PS[self.ps_rot % 4]
                        pk = f"ps{self.ps_rot % 4}"
                        self.ps_rot += 1
                        self.mm(pb[:, 0:n], cm[C_ONES], cvt[:, b0:b0 + n], r=["cmat", kcv], w=[pk])
                        self.act(wv_s(raw[:, b0:b0 + n]), pb[:, 0:n], AF.Sqrt, r=[pk, "epsc"], w=[kraw], bias=self.epsc[:, 1:2], scale=1.0)
                        yield

                    def recip(e):
                        with self.nc.allow_low_precision("fp32r-rounded rsqrt scratch"):
                            return e.reciprocal(out=wv_s(raw[:, :]), in_=raw[:, :])
                    S.op("dve", recip, [kraw], [kraw])
                    yield
                    self.stt("dve", wv_d(dst[:, :]), dst[:, :], (128.0 ** -0.5) if which == 0 else 1.0, raw[:, :], ALU.mult, ALU.mult,
                             r=[kdst, kraw], w=[kdst])
                    yield

            self.pipe([
                lambda: prep_chain(1, RAW, "RAWc", CV, "CVc", KT, "KT", r_, rr_),
                lambda: prep_chain(0, KTOKf, "KTOKs", VTOKf, "VTOKs", QT, "QT", rr_, rr_),
                lambda: prep_chain(2, QTOKf, "QTOKs", OACCf, "OACCs", OACCf, "OACCs", r_, r_),
            ], ni=3, skew=1)
            S.barrier()
            if h == 0:
                self.dump("q0", QT[:], "QT", [128, T]); self.dump("k0", KT[:], "KT", [128, T]); self.dump("v0", OACCf, "OACCs", [128, T])
            cnt = 0
            for t in range(NT):
                for srcT, sk, dstT, dk2 in ((KT, "KT", KTOK, "KTOK"), (OACCf, "OACCs", VTOK, "VTOK"), (QT, "QT", QTOK, "QTOK")):
                    bank = 4 + cnt % 4
                    pb = PS[bank]
                    pk = f"ps{bank}"
                    self.tr(pb[:, 0:128], srcT[:, t * 128:(t + 1) * 128], r=[sk, "ident"], w=[pk])
                    self.cp("act" if cnt % 2 == 0 else "dve", (rr_ if dk2 != "QTOK" else r_)(dstT[:, t, :]), pb[:, 0:128], r=[pk], w=[f"{dk2}:{t}"])
                    cnt += 1
            S.barrier()
            carve_state = {"i": 0}

            def carve(n):
                i = carve_state["i"]
                if i < T and i + n > T:
                    i = T
                assert i + n <= 2 * T
                carve_state["i"] = i + n
                return (RAW if i < T else CV)[:, (i % T):(i % T) + n]

            NI = 4
            GB = [carve(128) for j in range(NI)]
            DD = [carve(256) for j in range(NI)]
            WA = [carve(384) for j in range(NI)]
            WB = [carve(384) for j in range(NI)]
            smallt = self.sbt(ph, "csmall", [128, 8, 128])
            vnt = self.sbt(ph, "cvn", [128, 2, 128])
            ttft = self.sbt(ph, "cttf", [128, NI, 128])
            ket = self.sbt(ph, "cke", [128, NI, 128])
            VN = [vnt[:, dr, :] for dr in range(2)]
            O1 = [smallt[:, 2 + dr, :] for dr in range(2)]
            zs = [smallt[:, 4 + i, :] for i in range(2)]
            y1 = [smallt[:, 6 + i, :] for i in range(2)]
            self.ts("dve", rr_(Sst[0][:]), cm[C_ONES], 0.0, None, ALU.mult, r=["cmat"], w=["S0"])
            self.ts("dve", rr_(Sst[1][:]), cm[C_ONES], 0.0, None, ALU.mult, r=["cmat"], w=["S1"])
            order = [list(range(NT)), [1, 0] + list(range(NT - 1, 1, -1))]
            oacc_written = set()

            def evac_eng(n, k):
                return "act" if (n + k) % 2 == 0 else "dve"

            def pre(n):
                i, dr = n // 2, n % 2
                t = order[dr][i]
                col = dr * 4 + h
                j = n % NI
                sl = i % 3
                bank = PS[4 + n % 4]
                bk = f"ps{4 + n % 4}"
                gb, dd, wa, wb = GB[j], DD[j], WA[j], WB[j]
                kgb, kdd, kwa, kwb = f"GB{j}", f"DD{j}", f"WA{j}", f"WB{j}"
                ring = RING[dr][sl]
                rk = f"RING_{dr}_{sl}"
                tri, ntri, ms, mit = (cmr[C_TRIF], cmr[C_NTRIF], cmr[C_MSF], cmr[C_MITF]) if dr == 0 else (cmr[C_TRIB], cmr[C_NTRIB], cmr[C_MSB], cmr[C_MITB])
                idr = cmr[C_ID]
                tok = slice(t * 128, (t + 1) * 128)
                smk = f"sm:{t}"
                self.act(r_(gb), cm[C_ONES], AF.Identity, r=["cmat", smk], w=[kgb], scale=sm["G"][:, t, col:col + 1])
                self.mm(bank[:, 0:128], rr_(KT[:, tok]), rr_(KT[:, tok]), r=["KT"], w=[bk])
                self.mm(bank[:, 128:256], rr_(KT[:, tok]), rr_(QT[:, tok]), r=["KT", "QT"], w=[bk])
                self.mm(bank[:, 256:384], tri, r_(gb), True, False, r=["cmatr", kgb], w=[bk])
                self.mm(bank[:, 256:384], r_(gb), ntri, False, False, r=["cmatr", kgb], w=[bk])
                self.mm(bank[:, 256:384], idr, ms, False, True, r=["cmatr"], w=[bk])
                self.mm(bank[:, 384:512], r_(gb), tri, True, False, r=["cmatr", kgb], w=[bk])
                self.mm(bank[:, 384:512], ntri, r_(gb), False, False, r=["cmatr", kgb], w=[bk])
                self.mm(bank[:, 384:512], idr, mit, False, True, r=["cmatr"], w=[bk])
                yield
                self.act(r_(dd[:, 0:256]), bank[:, 256:512], AF.Exp, r=[bk], w=[kdd])
                self.stt("dve", r_(wa[:, 256:384]), bank[:, 0:128], sm["NBETA"][:, t, col:col + 1], dd[:, 0:128], ALU.mult, ALU.mult,
                         r=[bk, smk, kdd], w=[kwa])
                self.tt("dve", rr_(ring[:, 384:512]), bank[:, 128:256], dd[:, 128:256], ALU.mult, r=[bk, kdd], w=[rk])
                yield
                self.tr(bank[:, 0:128], wa[:, 256:384], r=[kwa, "ident"], w=[bk])
                yield
                self.cp(evac_eng(n, 0), r_(wa[:, 0:128]), bank[:, 0:128], r=[bk], w=[kwa])
                yield
                self.mm(bank[:, 0:128], r_(wa[:, 256:384]), r_(wa[:, 0:128]), r=[kwa], w=[bk])
                self.mm(bank[:, 256:384], r_(wa[:, 0:128]), r_(wa[:, 256:384]), r=[kwa], w=[bk])
                yield
                self.cp("act", wb[:, 0:384].rearrange("p (a b) -> p a b", b=128)[:, ::2, :],
                        bank[:, 0:384].rearrange("p (a b) -> p a b", b=128)[:, ::2, :], r=[bk], w=[kwb])
                self.tt("dve", wb[:, 128:256], wa[:, 0:128], cm[C_ID], ALU.add, r=[kwa, "cmat"], w=[kwb])
                yield
                cur, kcur, nxt, knxt = wb, kwb, wa, kwa
                for k in range(1, 6):
                    self.mm(bank[:, 0:256], r_(cur[:, 256:384]), r_(cur[:, 0:256]), r=[kcur], w=[bk])
                    self.mm(bank[:, 256:384], r_(cur[:, 0:128]), r_(cur[:, 256:384]), r=[kcur], w=[bk])
                    yield
                    self.cp("act", nxt[:, 0:384].rearrange("p (a b) -> p a b", b=128)[:, ::2, :],
                            bank[:, 0:384].rearrange("p (a b) -> p a b", b=128)[:, ::2, :], r=[bk], w=[knxt])
                    self.tt("dve", nxt[:, 128:256], bank[:, 128:256], cur[:, 128:256], ALU.add, r=[bk, kcur], w=[knxt])
                    cur, kcur, nxt, knxt = nxt, knxt, cur, kcur
                    yield
                self.mm(bank[:, 0:128], r_(cur[:, 256:384]), r_(cur[:, 128:256]), True, False, r=[kcur], w=[bk])
                self.mm(bank[:, 0:128], idr, r_(cur[:, 128:256]), False, True, r=[kcur, "cmatr"], w=[bk])
                yield
                self.act(rr_(ttft[:, j, :]), bank[:, 0:128], AF.Identity, r=[bk, smk], w=[f"TTF{j}"], scale=sm["BETA"][:, t, col:col + 1])
                self.act(rr_(ket[:, j, :]), KTOK[:, t, :], AF.Identity, r=[f"KTOK:{t}", smk], w=[f"KE{j}"], scale=sm["EGC"][:, t, col:col + 1])
                self.ts("dve", r_(nxt[:, 256:384]), QTOK[:, t, :], sm["EGC"][:, t, col:col + 1], None, ALU.mult, r=[f"QTOK:{t}", smk], w=[knxt])
                self.ts("dve", rr_(ring[:, 512:640]), KTOK[:, t, :], sm["EDEC"][:, t, col:col + 1], None, ALU.mult, r=[f"KTOK:{t}", smk], w=[rk])
                yield
                self.mm(bank[:, 0:128], rr_(ttft[:, j, :]), rr_(VTOK[:, t, :]), r=[f"TTF{j}", f"VTOK:{t}"], w=[bk])
                self.mm(bank[:, 128:256], rr_(ket[:, j, :]), rr_(ttft[:, j, :]), r=[f"KE{j}", f"TTF{j}"], w=[bk])
                self.tr(bank[:, 256:384], nxt[:, 256:384], r=[knxt, "ident"], w=[bk])
                yield
                self.cp(evac_eng(n, 0), rr_(ring[:, 0:384]), bank[:, 0:384], r=[bk], w=[rk])
                yield

            def step(i, dr):
                t = order[dr][i]
                col = dr * 4 + h
                sl = i % 3
                ring = RING[dr][sl]
                rk = f"RING_{dr}_{sl}"
                A = PS[dr * 2]
                B = PS[dr * 2 + 1]
                ka = f"ps{dr * 2}"
                kb = f"ps{dr * 2 + 1}"
                Sd = Sst[dr]
                skey = f"S{dr}"
                vn, o1 = VN[dr], O1[dr]
                kvn, ko1 = f"VN{dr}", f"O1{dr}"
                smk = f"sm:{t}"
                self.mm(A[:, 0:128], rr_(ring[:, 128:256]), rr_(Sd[:]), r=[rk, skey], w=[ka])
                yield
                self.tt("dve", rr_(vn), ring[:, 0:128], A[:, 0:128], ALU.subtract, r=[rk, ka], w=[kvn])
                yield
                self.mm(B[:, 0:128], rr_(ring[:, 256:384]), rr_(Sd[:]), True, False, r=[rk, skey], w=[kb])
                self.mm(B[:, 0:128], rr_(ring[:, 384:512]), rr_(vn), False, True, r=[rk, kvn], w=[kb])
                self.mm(A[:, 128:256], rr_(ring[:, 512:640]), rr_(vn), r=[rk, kvn], w=[ka])
                yield
                self.stt("dve", rr_(Sd[:]), Sd[:], sm["GLB"][:, t, col:col + 1], A[:, 128:256], ALU.mult, ALU.add,
                         r=[skey, smk, ka], w=[skey])
                if t not in oacc_written:
                    oacc_written.add(t)
                    self.cp("act", OACC[:, t, :], B[:, 0:128], r=[kb], w=[f"OACC:{t}"])
                else:
                    self.cp("act", r_(o1), B[:, 0:128], r=[kb], w=[ko1])
                    self.tt("pool", OACC[:, t, :], OACC[:, t, :], o1, ALU.add, r=[f"OACC:{t}", ko1], w=[f"OACC:{t}"])
                yield

            nitems = 2 * NT
            next_item = 0
            active = []
            pre_done = set()
            steps_emitted = [0, 0]
            chain = [None, None]
            chain_i = [0, 0]
            while True:
                progressed = False
                while len(active) < NI and next_item < nitems:
                    n = next_item
                    i, dr = n // 2, n % 2
                    if i >= 3 and steps_emitted[dr] < i - 2:
                        break
                    assert all(a[0] % NI != n % NI for a in active)
                    active.append((n, pre(n)))
                    next_item += 1
                for (n, g) in list(active):
                    try:
                        next(g)
                    except StopIteration:
                        active.remove((n, g))
                        pre_done.add(n)
                    progressed = True
                for dr in range(2):
                    if chain[dr] is None and chain_i[dr] < NT and (2 * chain_i[dr] + dr) in pre_done:
                        chain[dr] = step(chain_i[dr], dr)
                    if chain[dr] is not None:
                        try:
                            next(chain[dr])
                        except StopIteration:
                            chain[dr] = None
                            steps_emitted[dr] += 1
                            chain_i[dr] += 1
                        progressed = True
                if not progressed:
                    break
            assert steps_emitted == [NT, NT], steps_emitted
            S.barrier()
            if h == 0:
                self.dump("oacc0", OACC[:], "OACC:0", [128, NT, 128])
            self.dump(f"oacch{h}", OACC[:], "OACC:0", [128, NT, 128])
            st6 = self.sbt(ph, "cst6", [128, 4, 6])
            mv = self.sbt(ph, "cmv", [128, 4, 6])
            zs4 = [smallt[:, i, :] for i in range(4)]
            y14 = [smallt[:, 4 + i, :] for i in range(4)]

            def tileY(t):
                s2 = t % 4
                kst, kmv, kz, ky = f"cst6{s2}", f"cmv{s2}", f"zs{s2}", f"y1{s2}"
                pz = PS[s2]
                pk = f"ps{s2}"
                S.op("dve", lambda e: e.bn_stats(out=st6[:, s2, :], in_=OACC[:, t, :]), [f"OACC:{t}"], [kst])
                for kc in range(8):
                    self.mm(pz[:, 0:128], self.hT[:, kc, t * 128:(t + 1) * 128], wq[:, kc, 384:512], kc == 0, kc == 7,
                            r=[f"hT:{t}", "wq:3"], w=[pk])
                yield
                S.op("dve", lambda e: e.bn_aggr(out=mv[:, s2, 0:2], in_=st6[:, s2, :]), [kst], [kmv])
                self.act(r_(zs4[s2]), pz[:, 0:128], AF.Silu, r=[pk], w=[kz])
                yield
                self.tt("dve", mv[:, s2, 2:3], mv[:, s2, 0:1], mv[:, s2, 0:1], ALU.mult, r=[kmv], w=[kmv])
                yield
                self.tt("dve", mv[:, s2, 2:3], mv[:, s2, 2:3], mv[:, s2, 1:2], ALU.add, r=[kmv], w=[kmv])
                yield
                self.act(mv[:, s2, 3:4], mv[:, s2, 2:3], AF.Sqrt, r=[kmv, "epsc"], w=[kmv], bias=self.epsc[:, 1:2], scale=1.0)
                yield
                S.op("dve", lambda e: e.reciprocal(out=mv[:, s2, 4:5], in_=mv[:, s2, 3:4]), [kmv], [kmv])
                yield
                self.stt("dve", r_(y14[s2]), OACC[:, t, :], mv[:, s2, 4:5], self.onw[:], ALU.mult, ALU.mult,
                         r=[f"OACC:{t}", kmv, "onw"], w=[ky])
                yield
                self.tt("dve", r_(y14[s2]), y14[s2], zs4[s2], ALU.mult, r=[ky, kz], w=[ky])
                yield
                self.tr(pz[:, 128:256], y14[s2], r=[ky, "ident"], w=[pk])
                yield
                self.cp("act", self.yT[:, h, t * 128:(t + 1) * 128], pz[:, 128:256], r=[pk], w=[f"yT:{h}"])
                yield

            self.pipe([(lambda t=t: tileY(t)) for t in range(NT)], ni=4, skew=3)
            S.barrier()

    def phaseD(self, bi, l, src):
        nc, S, d = self.nc, self.S, self.d
        PS = self.PS
        last = (l == 1)
        NI = 4
        with ExitStack() as ph:
            wo = self.sbt(ph, "wo_bf", [128, 8, 1024], BF16)
            wr = self.sbt(ph, "wr_bf", [128, 8, 36], BF16)
            rb = self.sbt(ph, "rb_bc", [128, 36])
            gt = [self.sbt(ph, f"gt1_{i}", [128, D]) for i in range(2)]
            lng = self.sbt(ph, "lng_bc", [128, D])
            lnb = self.sbt(ph, "lnb_bc", [128, D])
            xt = [self.sbt(ph, f"xtD{i}", [128, D]) for i in range(NI)]
            rt = [self.sbt(ph, f"rtD{i}", [128, D]) for i in range(NI)]
            xn = [self.sbt(ph, f"xnD{i}", [128, D]) for i in range(NI)]
            rs = [self.sbt(ph, f"rsD{i}", [128, 128]) for i in range(NI)]
            for half in range(2):
                self.ld("pool", wo[:, :, half * 512:(half + 1) * 512],
                        d["w_out"][l, :, half * 512:(half + 1) * 512].rearrange("(kc p) n -> p kc n", p=128), f"wo{half}", w=[f"wo:{half}"])
            self.ld("pool", wr[:], d["wr"][l, :, :].rearrange("(kc p) n -> p kc n", p=128), "wr", w=["wr"])
            self.ld("sp", rb[:], d["br"][l, :].partition_broadcast(128), "rb", w=["rb"])
            self.ld("sp", gt[0][:], d["modrow"][l, bi, 2048:3072].partition_broadcast(128), "gt0", r=["modrow"], w=["gt0"])
            self.ld("sp", gt[1][:], d["modrow"][l, 2, 2048:3072].partition_broadcast(128), "gt1", r=["modrow"], w=["gt1"])
            self.ld("sp", lng[:], d["ln_g"][l, 0, :].partition_broadcast(128), "lng", w=["lng"])
            self.ld("sp", lnb[:], d["ln_b"][l, 0, :].partition_broadcast(128), "lnb", w=["lnb"])
            tiles = list(range(2 if last else 0, NT))

            def tileD(idx, t):
                j = 2 if t < 2 else bi
                g = 1 if t < 2 else 0
                s = idx % NI
                tok = slice(t * 128, (t + 1) * 128)
                bank = [PS[s * 2], PS[s * 2 + 1]]
                bkey = [f"ps{s * 2}", f"ps{s * 2 + 1}"]
                kx, kr, kn, krs = f"xtD{s}", f"rtD{s}", f"xnD{s}", f"rsD{s}"
                self.ld("sp", xt[s][:], src[bi, tok, :], kx, w=[kx])
                for half in range(2):
                    for c in range(8):
                        self.mm(bank[half][:, 0:512], self.yT[:, c, tok], wo[:, c, half * 512:(half + 1) * 512], c == 0, c == 7,
                                r=[f"yT:{c}", f"wo:{half}"], w=[bkey[half]])
                yield
                for half in range(2):
                    self.tt("dve", rt[s][:, half * 512:(half + 1) * 512], bank[half][:, 0:512], gt[g][:, half * 512:(half + 1) * 512], ALU.mult,
                            r=[bkey[half], f"gt{g}"], w=[kr])
                yield
                self.stt("dve", rt[s][:], xt[s][:], ALPHA, rt[s][:], ALU.mult, ALU.add, r=[kx, kr], w=[kr])
                yield
                st = {}
                yield from self.ln_stats_g(rt[s], kr, st)
                self.ts("dve", rt[s][:], rt[s][:], st["mean"], st["rstd"], ALU.subtract, ALU.mult, r=[kr, st["k"]], w=[kr])
                yield
                self.tt("pool", rt[s][:], rt[s][:], lng[:], ALU.mult, r=[kr, "lng"], w=[kr])
                yield
                self.tt("pool", rt[s][:], rt[s][:], lnb[:], ALU.add, r=[kr, "lnb"], w=[kr])
                yield
                self.ld("sp", d["xs1"][bi, tok, :], rt[s][:], f"x1st{s}", r=[kr], w=[f"xs1:{t}"])
                if t == 2:
                    self.dump("x1_t2", rt[s][:], kr, [128, D])
                st2 = {}
                yield from self.ln_stats_g(rt[s], kr, st2)
                self.ts("dve", xn[s][:], rt[s][:], st2["mean"], st2["rstd"], ALU.subtract, ALU.mult, r=[kr, st2["k"]], w=[kn])
                yield
                for kc in range(8):
                    q = kc % 4
                    self.tr(bank[kc // 4][:, q * 128:(q + 1) * 128], xn[s][:, kc * 128:(kc + 1) * 128], r=[kn, "ident"], w=[bkey[kc // 4]])
                yield
                for kc in range(8):
                    q = kc % 4
                    self.act(self.hT[:, kc, tok], bank[kc // 4][:, q * 128:(q + 1) * 128], AF.Identity, r=[bkey[kc // 4], "onep", "modT"], w=[f"hT:{t}"],
                             scale=self.onep[:, l, 32 + kc, j:j + 1], bias=self.modT[:, l, 24 + kc, j:j + 1])
                yield
                pr = bank[0]
                prk = bkey[0]
                for kc in range(8):
                    self.mm(pr[:, 0:36], self.hT[:, kc, tok], wr[:, kc, :], kc == 0, kc == 7, r=[f"hT:{t}", "wr"], w=[prk])
                yield
                R = rs[s]
                rk = krs
                self.tt("dve", R[:, 0:36], pr[:, 0:36], rb[:], ALU.add, r=[prk, "rb"], w=[rk])
                yield
                sc = lambda i: R[:, 120 + i:121 + i]
                S.op("dve", lambda e: e.reduce_max(out=R[:, 120:121], in_=R[:, 0:4], axis=mybir.AxisListType.X), [rk], [rk])
                yield
                self.ts("dve", R[:, 36:40], R[:, 0:4], sc(0), None, ALU.subtract, r=[rk], w=[rk])
                self.ts("dve", R[:, 40:44], R[:, 0:4], sc(0), None, ALU.is_ge, r=[rk], w=[rk])
                yield
                self.act(R[:, 36:40], R[:, 36:40], AF.Exp, r=[rk], w=[rk])
                self.ts("dve", R[:, 40:44], R[:, 40:44], 1.0, 1e30, ALU.subtract, ALU.mult, r=[rk], w=[rk])
                yield
                S.op("dve", lambda e: e.reduce_sum(out=R[:, 121:122], in_=R[:, 36:40], axis=mybir.AxisListType.X), [rk], [rk])
                for g4 in range(4):
                    self.ts("dve", R[:, 44 + g4 * 8:52 + g4 * 8], R[:, 4 + g4 * 8:12 + g4 * 8], R[:, 40 + g4:41 + g4], None, ALU.add, r=[rk], w=[rk])
                yield
                S.op("dve", lambda e: e.reciprocal(out=R[:, 122:123], in_=R[:, 121:122]), [rk], [rk])
                S.op("dve", lambda e: e.reduce_max(out=R[:, 123:124], in_=R[:, 44:76], axis=mybir.AxisListType.X), [rk], [rk])
                yield
                self.ts("dve", R[:, 76:108], R[:, 44:76], sc(3), None, ALU.is_ge, r=[rk], w=[rk])
                yield
                self.stt("dve", R[:, 44:76], R[:, 76:108], -1e30, R[:, 44:76], ALU.mult, ALU.add, r=[rk], w=[rk])
                yield
                S.op("dve", lambda e: e.reduce_max(out=R[:, 124:125], in_=R[:, 44:76], axis=mybir.AxisListType.X), [rk], [rk])
                yield
                self.ts("dve", R[:, 44:76], R[:, 44:76], sc(4), None, ALU.is_ge, r=[rk], w=[rk])
                self.tt("dve", R[:, 125:126], R[:, 124:125], R[:, 123:124], ALU.subtract, r=[rk], w=[rk])
                yield
                self.act(R[:, 125:126], R[:, 125:126], AF.Exp, r=[rk], w=[rk])
                yield
                self.ts("dve", R[:, 125:126], R[:, 125:126], 1.0, None, ALU.add, r=[rk], w=[rk])
                yield
                S.op("dve", lambda e: e.reciprocal(out=R[:, 126:127], in_=R[:, 125:126]), [rk], [rk])
                yield
                self.tt("dve", R[:, 126:127], R[:, 126:127], R[:, 122:123], ALU.mult, r=[rk], w=[rk])
                yield
                self.tt("dve", R[:, 127:128], R[:, 122:123], R[:, 126:127], ALU.subtract, r=[rk], w=[rk])
                self.ts("dve", R[:, 76:108], R[:, 76:108], R[:, 126:127], None, ALU.mult, r=[rk], w=[rk])
                yield
                self.stt("dve", R[:, 76:108], R[:, 44:76], R[:, 127:128], R[:, 76:108], ALU.mult, ALU.add, r=[rk], w=[rk])
                yield
                if t == 2:
                    self.dump("comb_t2", R[:, 76:108], rk, [128, 32])
                self.tr(pr[0:32, 128:256], R[:, 76:108], r=[rk, "ident"], w=[prk])
                yield
                self.cp("act", self.combT[0:32, tok], pr[0:32, 128:256], r=[prk], w=[f"combT:{t}"])
                yield

            self.pipe([(lambda i=i, t=t: tileD(i, t)) for i, t in enumerate(tiles)], ni=NI, skew=8)
            c0 = tiles[0] * 128
            self.ld("sp", d["combD"][:, c0:T], self.combT[0:32, c0:T], "combst", r=[f"combT:{t}" for t in tiles], w=["combD"])
            S.barrier()

    def phaseE(self, bi, l):
        nc, S, d = self.nc, self.S, self.d
        PS = self.PS
        last = (l == 1)
        tok0 = NCTX if last else 0
        blocks = []
        b = tok0
        while b < T:
            n = min(512, T - b)
            blocks.append((b, n))
            b += n
        ne = self.cfg.get("nexp", 32)
        with ExitStack() as ph:
            Y = self.sbt(ph, "Yacc", [128, NT, D])
            with ExitStack() as ex:
                WG = [self.sbt(ex, f"WG{i}", [128, 8, 256], BF16) for i in range(3)]
                WU = [self.sbt(ex, f"WU{i}", [128, 8, 256], BF16) for i in range(3)]
                WD = [self.sbt(ex, f"WD{i}", [128, 2, 1024], BF16) for i in range(3)]
                CB = [self.sbt(ex, f"CB{i}", [128, T], BF16) for i in range(3)]
                SA = [self.sbt(ex, f"SA{i}", [128, 512]) for i in range(2)]
                T1 = [self.sbt(ex, f"T1{i}", [128, 512], BF16) for i in range(2)]
                AT = [[self.sbt(ex, f"AT{p}{i}", [128, 512], BF16) for i in range(2)] for p in range(2)]
                ycnt = [0]
                work = []
                bcnt = 0
                for e in range(ne):
                    for (b0, n) in blocks:
                        work.append((e, e % 3, b0, n, bcnt % 2))
                        bcnt += 1

                def load_w(e):
                    s = e % 3
                    self.ld("pool", WG[s][:], d["weg"][l, e, :, :].rearrange("(kc p) f -> p kc f", p=128), f"WG{s}", w=[f"WG{s}"])
                    self.ld("pool", WU[s][:], d["weu"][l, e, :, :].rearrange("(kc p) f -> p kc f", p=128), f"WU{s}", w=[f"WU{s}"])
                    self.ld("pool", WD[s][:], d["wed"][l, e, :, :].rearrange("(fc p) n -> p fc n", p=128), f"WD{s}", w=[f"WD{s}"])
                    self.ld("sp", CB[s][:], d["combD"][e, :].partition_broadcast(128), f"CB{s}", r=["combD"], w=[f"CB{s}"])

                def gu(wi, fc):
                    e, s, b0, n, par = work[wi]
                    tiles = list(range(b0 // 128, (b0 + n) // 128))
                    hk = [f"hT:{t}" for t in tiles]
                    pa, pb = PS[fc * 2], PS[fc * 2 + 1]
                    ka, kb = f"ps{fc * 2}", f"ps{fc * 2 + 1}"
                    for kc in range(8):
                        self.mm(pa[:, 0:n], WG[s][:, kc, fc * 128:(fc + 1) * 128], self.hT[:, kc, b0:b0 + n], kc == 0, kc == 7,
                                r=[f"WG{s}"] + hk, w=[ka])
                    for kc in range(8):
                        self.mm(pb[:, 0:n], WU[s][:, kc, fc * 128:(fc + 1) * 128], self.hT[:, kc, b0:b0 + n], kc == 0, kc == 7,
                                r=[f"WU{s}"] + hk, w=[kb])
                    self.act(SA[fc][:, 0:n], pa[:, 0:n], AF.Silu, r=[ka], w=[f"SA{fc}"])
                    self.tt("dve", T1[fc][:, 0:n], pb[:, 0:n], SA[fc][:, 0:n], ALU.mult, r=[kb, f"SA{fc}"], w=[f"T1{fc}"])
                    self.tt("pool", AT[par][fc][:, 0:n], T1[fc][:, 0:n], CB[s][:, b0:b0 + n], ALU.mult, r=[f"CB{s}", f"T1{fc}"], w=[f"AT{par}{fc}"])

                def down(wi):
                    e, s, b0, n, par = work[wi]
                    tiles = list(range(b0 // 128, (b0 + n) // 128))
                    for ti, t in enumerate(tiles):
                        for half in range(2):
                            yb = 4 + ycnt[0] % 4
                            ycnt[0] += 1
                            py = PS[yb]
                            yk = f"ps{yb}"
                            for fc in range(2):
                                self.mm(py[:, 0:512], AT[par][fc][:, ti * 128:(ti + 1) * 128], WD[s][:, fc, half * 512:(half + 1) * 512],
                                        fc == 0, fc == 1, r=[f"AT{par}{fc}", f"WD{s}"], w=[yk])
                            ysl = Y[:, t, half * 512:(half + 1) * 512]
                            ykey = f"Y:{t}:{half}"
                            if e == 0:
                                self.cp("act", ysl, py[:, 0:512], r=[yk], w=[ykey])
                            else:
                                self.tt("dve", ysl, py[:, 0:512], ysl, ALU.add, r=[yk, ykey], w=[ykey])

                loaded = set()
                for e in range(min(2, ne)):
                    load_w(e); loaded.add(e)
                nw = len(work)
                gu(0, 0)
                for wi in range(nw):
                    e = work[wi][0]
                    if wi == 0 or work[wi - 1][0] != e:
                        if e + 2 < ne and (e + 2) not in loaded:
                            load_w(e + 2); loaded.add(e + 2)
                    gu(wi, 1)
                    if wi + 1 < nw:
                        gu(wi + 1, 0)
                    down(wi)
                S.barrier()
                S.emit()

            gt = [self.sbt(ph, f"gt2_{i}", [128, D]) for i in range(2)]
            lng = self.sbt(ph, "lng2_bc", [128, D])
            lnb = self.sbt(ph, "lnb2_bc", [128, D])
            NF = 3
            xt = [self.sbt(ph, f"xtF{i}", [128, D]) for i in range(NF)]
            ot = [self.sbt(ph, f"otF{i}", [128, D]) for i in range(NF)]
            self.ld("sp", gt[0][:], d["modrow"][l, bi, 5120:6144].partition_broadcast(128), "gt20", r=["modrow"], w=["gt20"])
            self.ld("sp", gt[1][:], d["modrow"][l, 2, 5120:6144].partition_broadcast(128), "gt21", r=["modrow"], w=["gt21"])
            self.ld("sp", lng[:], d["ln_g"][l, 1, :].partition_broadcast(128), "lng2", w=["lng2"])
            self.ld("sp", lnb[:], d["ln_b"][l, 1, :].partition_broadcast(128), "lnb2", w=["lnb2"])
            tiles = list(range(2 if last else 0, NT))

            def tileF(idx, t):
                g = 1 if t < 2 else 0
                s2 = idx % NF
                tok = slice(t * 128, (t + 1) * 128)
                kx, ko = f"xtF{s2}", f"otF{s2}"
                self.ld("sp", xt[s2][:], d["xs1"][bi, tok, :], kx, r=[f"xs1:{t}"], w=[kx])
                self.tt("pool", ot[s2][:], Y[:, t, :], gt[g][:], ALU.mult, r=[f"Y:{t}:0", f"Y:{t}:1", f"gt2{g}"], w=[ko])
                yield
                self.stt("dve", ot[s2][:], xt[s2][:], ALPHA, ot[s2][:], ALU.mult, ALU.add, r=[kx, ko], w=[ko])
                yield
                st = {}
                yield from self.ln_stats_g(ot[s2], ko, st)
                self.ts("dve", ot[s2][:], ot[s2][:], st["mean"], st["rstd"], ALU.subtract, ALU.mult, r=[ko, st["k"]], w=[ko])
                yield
                self.tt("pool", ot[s2][:], ot[s2][:], lng[:], ALU.mult, r=[ko, "lng2"], w=[ko])
                yield
                self.tt("dve", ot[s2][:], ot[s2][:], lnb[:], ALU.add, r=[ko, "lnb2"], w=[ko])
                yield
                if t == 2:
                    self.dump(f"x2_t2_l{l}", ot[s2][:], ko, [128, D])
                    self.dump(f"f_t2_l{l}", Y[:, t, :], f"Y:{t}:0", [128, D])
                if last:
                    self.ld("sp", d["out"][bi, (t - 2) * 128:(t - 1) * 128, :], ot[s2][:], f"ost{s2}", r=[ko], w=[f"out:{t}"])
                else:
                    self.ld("sp", d["xs2"][bi, tok, :], ot[s2][:], f"ost{s2}", r=[ko], w=[f"xs2:{t}"])
                yield

            self.pipe([(lambda i=i, t=t: tileF(i, t)) for i, t in enumerate(tiles)], ni=NF, skew=3)
            S.barrier()


def _consts():
    k = np.arange(128)[:, None]
    i = np.arange(128)[None, :]
    cm = np.zeros((10, 128, 128), np.float32)
    cm[C_ID] = np.eye(128)
    cm[C_TRIF] = (k <= i)
    cm[C_TRIB] = (k >= i)
    cm[C_NTRIF] = -cm[C_TRIF]
    cm[C_NTRIB] = -cm[C_TRIB]
    cm[C_MSF] = np.where(i < k, 0.0, NEG)
    cm[C_MSB] = np.where(i > k, 0.0, NEG)
    cm[C_MITF] = np.where(k <= i, 0.0, NEG)
    cm[C_MITB] = np.where(k >= i, 0.0, NEG)
    cm[C_ONES] = 1.0
    sel = np.zeros((32, 32, 128), np.float32)
    for e in range(32):
        sel[e, e, :] = 1.0
    return np.ascontiguousarray(cm.transpose(1, 0, 2)), sel.reshape(32, 4096)


def prep_shared(inp):
    f = lambda a: np.ascontiguousarray(np.asarray(a, dtype=np.float32))
    sh = {}
    sh["w_ada"] = f(inp["w_ada"])
    sh["b_ada"] = f(inp["b_ada"])
    sh["b_adaT"] = f(np.asarray(inp["b_ada"]).reshape(2, 48, 128).transpose(0, 2, 1))
    sh["w_in"] = f(inp["w_in"])
    sh["cqw"] = f(np.asarray(inp["conv_qkv_w"]).reshape(2, 4, 12, 128).transpose(0, 3, 2, 1))
    sh["rgcw"] = f(np.asarray(inp["rg_conv_w"]).reshape(2, 4, 4, 128).transpose(0, 3, 2, 1))
    sh["rgcb"] = f(np.asarray(inp["rg_conv_b"]).reshape(2, 4, 128).transpose(0, 2, 1))
    sh["alog"] = f(np.asarray(inp["dn_a_log"]).reshape(2, 8))
    sh["dtb"] = f(np.asarray(inp["dn_dt_bias"]).reshape(2, 8))
    sh["onw"] = f(inp["dn_onorm_w"])
    wbd = np.zeros((2, 2, 2, 4, 128, 128), np.float32)
    for gate, nm in ((0, "rg_wa"), (1, "rg_wi")):
        w = np.asarray(inp[nm])
        for ch in range(4):
            for sub in range(2):
                wbd[:, :, gate, ch, sub * 64:(sub + 1) * 64, sub * 64:(sub + 1) * 64] = w[:, :, ch * 2 + sub]
    sh["wbd"] = f(wbd.reshape(2, 16, 128, 128).transpose(0, 2, 1, 3))
    rgb = np.zeros((2, 2, 2, 4, 128), np.float32)
    rgb[:, :, 0] = np.asarray(inp["rg_ba"]).reshape(2, 2, 4, 128)
    rgb[:, :, 1] = np.asarray(inp["rg_bi"]).reshape(2, 2, 4, 128)
    sh["rgb"] = f(rgb.reshape(2, 16, 128).transpose(0, 2, 1))
    sh["lam"] = f(np.asarray(inp["rg_lambda"]).reshape(2, 8, 128).transpose(0, 2, 1))
    sh["w_out"] = f(inp["w_out"])
    sh["ln_g"] = f(inp["ln_g"])
    sh["ln_b"] = f(inp["ln_b"])
    sh["wr"] = f(np.concatenate([np.asarray(inp["router_wg"]), np.asarray(inp["router_we"])], axis=-1))
    sh["br"] = f(np.concatenate([np.asarray(inp["router_bg"]), np.asarray(inp["router_be"])], axis=-1))
    sh["weg"] = f(inp["w_e_gate"])
    sh["weu"] = f(inp["w_e_up"])
    sh["wed"] = f(inp["w_e_down"])
    cm, sel = _consts()
    sh["cmat"] = cm
    return sh


def prep_core(inp, core):
    x = np.asarray(inp["x"], dtype=np.float32)
    ctx = np.asarray(inp["ctx"], dtype=np.float32)
    c = np.asarray(inp["c"], dtype=np.float32)
    cc = np.asarray(inp["c_ctx"], dtype=np.float32)
    b0 = 2 * core
    xin = np.concatenate([ctx[b0:b0 + 2], x[b0:b0 + 2]], axis=1)
    cols = np.stack([c[b0], c[b0 + 1], cc], axis=-1)
    cT = cols.reshape(8, 128, 3).transpose(1, 0, 2)
    return {"xin": np.ascontiguousarray(xin), "cT": np.ascontiguousarray(cT)}


def build_nc(cfg=None):
    nc = bass.Bass("TRN2", target_bir_lowering=False)
    kb = KB(nc, cfg or {})
    kb.build()
    return nc, kb


def kernel(**inputs):
    nc, kb = build_nc({})
    sh = prep_shared(inputs)
    in_maps = []
    for core in range(8):
        m = dict(sh)
        m.update(prep_core(inputs, core))
        in_maps.append(m)
    res = run_bass_kernel_spmd(nc, in_maps, core_ids=list(range(8)))
    outs = [np.asarray(r["out"]) for r in res.results]
    return np.concatenate(outs, axis=0).astype(np.float32)
```

```python
import numpy as np
from contextlib import ExitStack
import concourse.bass as bass
import concourse.mybir as mybir
from concourse.bass_utils import run_bass_kernel_spmd

F32 = mybir.dt.float32
BF16 = mybir.dt.bfloat16
AF = mybir.ActivationFunctionType
ALU = mybir.AluOpType

T = 2304
NT = 18
D = 1024
DIN = 3088
NCTX = 256
ALPHA = float((2 * 2) ** 0.25)
LN_EPS = 1e-5
NORM_EPS = 1e-6
NEG = -30000.0
ENGS = ["pe", "act", "dve", "pool", "sp"]
(C_ID, C_TRIF, C_TRIB, C_NTRIF, C_NTRIB, C_MSF, C_MSB, C_MITF, C_MITB, C_ONES) = range(10)


class Sched:
    def __init__(self, nc, stack):
        self.nc = nc
        self.stack = stack
        self.q = {e: [] for e in ENGS}
        self.sems = {}
        self.semval = {}
        self.waited = {}
        self.last_w = {}
        self.readers = {}
        self.ninstr = 0
        for e in ENGS:
            if e != "sp":
                self._sem("eng_" + e)

    def _sem(self, name):
        if name not in self.sems:
            self.sems[name] = self.stack.enter_context(self.nc.semaphore(name))
            self.semval[name] = 0
        return self.sems[name]

    @staticmethod
    def _norm(reads, writes):
        r2 = []
        w2 = []
        for k in reads:
            if k.startswith("ps"):
                w2.append(k.split(":")[0])
            else:
                r2.append(k)
        for k in writes:
            w2.append(k.split(":")[0] if k.startswith("ps") else k)
        return r2, w2

    def _deps(self, eng, reads, writes):
        toks = []
        for k in reads:
            t = self.last_w.get(k)
            if t is not None:
                toks.append(t)
        for k in writes:
            t = self.last_w.get(k)
            if t is not None:
                toks.append(t)
            toks.extend(self.readers.get(k, ()))
        best = {}
        for (s, v) in toks:
            if eng == "pe" and s == "eng_pe":
                continue
            if self.waited.get((eng, s), 0) < v and best.get(s, 0) < v:
                best[s] = v
        for s, v in best.items():
            self.waited[(eng, s)] = v
        return list(best.items())

    def _commit(self, tok, reads, writes):
        for k in reads:
            self.readers.setdefault(k, []).append(tok)
        for k in writes:
            self.last_w[k] = tok
            self.readers[k] = []

    def op(self, eng, fn, reads=(), writes=()):
        reads, writes = self._norm(reads, writes)
        waits = self._deps(eng, reads, writes)
        s = "eng_" + eng
        self.semval[s] += 1
        tok = (s, self.semval[s])
        self.q[eng].append((waits, fn, (s, 1)))
        self._commit(tok, reads, writes)
        self.ninstr += 1
        return tok

    def dma(self, eng, fn, slot, reads=(), writes=()):
        s = "dma_" + slot
        self._sem(s)
        reads, writes = self._norm(reads, writes)
        waits = self._deps(eng, reads, writes)
        self.semval[s] += 16
        tok = (s, self.semval[s])
        self.q[eng].append((waits, fn, (s, 16)))
        self._commit(tok, reads, writes)
        self.ninstr += 1
        return tok

    def barrier(self):
        allt = [(s, v) for s, v in self.semval.items() if v > 0]
        for e in ENGS:
            waits = []
            for (s, v) in allt:
                if self.waited.get((e, s), 0) < v:
                    self.waited[(e, s)] = v
                    waits.append((s, v))
            if waits:
                self.q[e].append((waits, None, None))

    def emit(self):
        self.barrier()
        nc = self.nc
        sems = self.sems
        q = self.q
        self.q = {e: [] for e in ENGS}

        def run(engobj, lst):
            for waits, fn, inc in lst:
                for (s, v) in waits:
                    engobj.wait_ge(sems[s], v)
                if fn is not None:
                    ins = fn(engobj)
                    ins.then_inc(sems[inc[0]], inc[1])

        with nc.Block() as block:
            @block.sync
            def _(e):
                run(e, q["sp"])

            @block.tensor
            def _(e):
                run(e, q["pe"])

            @block.scalar
            def _(e):
                run(e, q["act"])

            @block.vector
            def _(e):
                run(e, q["dve"])

            @block.gpsimd
            def _(e):
                run(e, q["pool"])


def rr(gens):
    gens = list(gens)
    while gens:
        nxt = []
        for g in gens:
            try:
                next(g)
                nxt.append(g)
            except StopIteration:
                pass
        gens = nxt


class KB:
    def __init__(self, nc, cfg):
        self.nc = nc
        self.cfg = cfg
        self.dbg = set(cfg.get("dbg", ()))
        self.d = {}
        self.dbg_out = {}

    def sbt(self, st, name, shape, dt=F32):
        self.uid = getattr(self, "uid", 0) + 1
        return st.enter_context(self.nc.sbuf_tensor(f"{name}_u{self.uid}", shape, dt))

    def mm(self, out, lhsT, rhs, start=True, stop=True, r=(), w=()):
        return self.S.op("pe", lambda e: e.matmul(out, lhsT=lhsT, rhs=rhs, start=start, stop=stop), r, w)

    def tr(self, out, in_, r=(), w=()):
        ident = self.cm[C_ID]
        return self.S.op("pe", lambda e: e.transpose(out, in_, ident), list(r), w)

    def act(self, out, in_, func, r=(), w=(), **kw):
        return self.S.op("act", lambda e: e.activation(out=out, in_=in_, func=func, **kw), r, w)

    def ts(self, eng, out, in0, s1, s2, op0, op1=None, r=(), w=()):
        if op1 is None:
            return self.S.op(eng, lambda e: e.tensor_scalar(out=out, in0=in0, scalar1=s1, scalar2=None, op0=op0), r, w)
        return self.S.op(eng, lambda e: e.tensor_scalar(out=out, in0=in0, scalar1=s1, scalar2=s2, op0=op0, op1=op1), r, w)

    def tt(self, eng, out, in0, in1, op, r=(), w=()):
        return self.S.op(eng, lambda e: e.tensor_tensor(out=out, in0=in0, in1=in1, op=op), r, w)

    def stt(self, eng, out, in0, scalar, in1, op0, op1, r=(), w=()):
        return self.S.op(eng, lambda e: e.scalar_tensor_tensor(out=out, in0=in0, scalar=scalar, in1=in1, op0=op0, op1=op1), r, w)

    def cp(self, eng, out, in_, r=(), w=()):
        if eng == "act":
            return self.S.op("act", lambda e: e.activation(out=out, in_=in_, func=AF.Copy), r, w)
        return self.S.op(eng, lambda e: e.tensor_copy(out=out, in_=in_), r, w)

    def memset(self, eng, ap, val, w=()):
        return self.S.op(eng, lambda e: e.memset(ap, val), (), w)

    def ld(self, q, out, in_, slot, r=(), w=()):
        return self.S.dma(q, lambda e: e.dma_start(out=out, in_=in_), slot, r, w)

    def dump(self, name, ap, key, shape, dt=F32):
        if name not in self.dbg:
            return
        o = self.nc.dram_tensor("d_" + name, shape, dt, kind="ExternalOutput").ap()
        self.dbg_out[name] = o
        self.S.dma("sp", lambda e: e.dma_start(out=o, in_=ap), "dbg_" + name, [key], ["dbgdram_" + name])

    @staticmethod
    def pipe(makers, ni=2, skew=4):
        makers = list(makers)
        active = []
        nxt = 0
        since = skew
        while nxt < len(makers) or active:
            if nxt < len(makers) and len(active) < ni and (since >= skew or not active):
                active.append(makers[nxt]())
                nxt += 1
                since = 0
            for g in list(active):
                try:
                    next(g)
                except StopIteration:
                    active.remove(g)
            since += 1

    def ln_stats_g(self, x_ap, xkey, out):
        i = self.stat_i % 8
        self.stat_i += 1
        st6 = self.stat6[:, i, :, :]
        mv = self.statmv[:, i, :]
        k6 = f"st6_{i}"
        kmv = f"stmv_{i}"
        for g in range(2):
            self.S.op("dve", lambda e, g=g: e.bn_stats(out=st6[:, g, :], in_=x_ap[:, g * 512:(g + 1) * 512]), [xkey], [k6])
        self.S.op("dve", lambda e: e.bn_aggr(out=mv[:, 0:2], in_=st6.rearrange("p a b -> p (a b)")), [k6], [kmv])
        yield
        self.act(mv[:, 2:3], mv[:, 1:2], AF.Ln, [kmv, "epsc"], [kmv], bias=self.epsc[:, 0:1], scale=1.0)
        yield
        self.act(mv[:, 3:4], mv[:, 2:3], AF.Exp, [kmv], [kmv], scale=-0.5)
        out["mean"], out["rstd"], out["k"] = mv[:, 0:1], mv[:, 3:4], kmv

    def ln_stats(self, x_ap, xkey, tag, eps=LN_EPS):
        i = self.stat_i % 8
        self.stat_i += 1
        st6 = self.stat6[:, i, :, :]
        mv = self.statmv[:, i, :]
        k6 = f"st6_{i}"
        kmv = f"stmv_{i}"
        for g in range(2):
            self.S.op("dve", lambda e, g=g: e.bn_stats(out=st6[:, g, :], in_=x_ap[:, g * 512:(g + 1) * 512]), [xkey], [k6])
        self.S.op("dve", lambda e: e.bn_aggr(out=mv[:, 0:2], in_=st6.rearrange("p a b -> p (a b)")), [k6], [kmv])
        self.act(mv[:, 2:3], mv[:, 1:2], AF.Sqrt, [kmv], [kmv], bias=self.epsc[:, 0:1] if eps == LN_EPS else self.epsc[:, 1:2], scale=1.0)
        self.S.op("dve", lambda e: e.reciprocal(out=mv[:, 3:4], in_=mv[:, 2:3]), [kmv], [kmv])
        return mv[:, 0:1], mv[:, 3:4], kmv

    def conv(self, eng, out_t, in_t, w4, bias, rk, wk, wv=None):
        wv = wv or (lambda a: a)
        if bias is not None:
            self.ts(eng, wv(out_t[:, :]), in_t[:, :], w4[:, 2:3], bias, ALU.mult, ALU.add, r=rk, w=[wk])
        else:
            self.ts(eng, wv(out_t[:, :]), in_t[:, :], w4[:, 2:3], None, ALU.mult, r=rk, w=[wk])
        o3 = out_t[:, NCTX:].rearrange("p (r c) -> p r c", c=64)
        i3 = in_t[:, NCTX:].rearrange("p (r c) -> p r c", c=64)
        for (j, o) in ((0, -2), (1, -1), (3, 1)):
            lo = max(0, -o)
            hi = NCTX - max(0, o)
            self.stt(eng, wv(out_t[:, lo:hi]), in_t[:, lo + o:hi + o], w4[:, j:j + 1], out_t[:, lo:hi], ALU.mult, ALU.add,
                     r=list(rk) + [wk], w=[wk])
            hi = 64 - max(0, o)
            self.stt(eng, wv(o3[:, :, lo:hi]), i3[:, :, lo + o:hi + o], w4[:, j:j + 1], o3[:, :, lo:hi], ALU.mult, ALU.add,
                     r=list(rk) + [wk], w=[wk])

    def conv_g(self, eng, out_t, in_t, w4, bias, rk, wk, wv=None):
        wv = wv or (lambda a: a)
        if bias is not None:
            self.ts(eng, wv(out_t[:, :]), in_t[:, :], w4[:, 2:3], bias, ALU.mult, ALU.add, r=rk, w=[wk])
        else:
            self.ts(eng, wv(out_t[:, :]), in_t[:, :], w4[:, 2:3], None, ALU.mult, r=rk, w=[wk])
        yield
        o3 = out_t[:, NCTX:].rearrange("p (r c) -> p r c", c=64)
        i3 = in_t[:, NCTX:].rearrange("p (r c) -> p r c", c=64)
        for (j, o) in ((0, -2), (1, -1), (3, 1)):
            lo = max(0, -o)
            hi = NCTX - max(0, o)
            self.stt(eng, wv(out_t[:, lo:hi]), in_t[:, lo + o:hi + o], w4[:, j:j + 1], out_t[:, lo:hi], ALU.mult, ALU.add,
                     r=list(rk) + [wk], w=[wk])
            hi = 64 - max(0, o)
            self.stt(eng, wv(o3[:, :, lo:hi]), i3[:, :, lo + o:hi + o], w4[:, j:j + 1], o3[:, :, lo:hi], ALU.mult, ALU.add,
                     r=list(rk) + [wk], w=[wk])
            yield

    def proj_fm_g(self, dst, dkey, wt, wkey, c0, wv=None):
        wv_ = wv or (lambda a: a)
        for blk in range(5):
            b0 = blk * 512
            n = min(512, T - b0)
            pb = self.PS[self.ps_rot % 4]
            pk = f"ps{self.ps_rot % 4}"
            self.ps_rot += 1
            for kc in range(8):
                self.mm(pb[:, 0:n], wt[:, kc, c0:c0 + 128], self.hT[:, kc, b0:b0 + n], kc == 0, kc == 7,
                        r=[wkey] + [f"hT:{t}" for t in range(b0 // 128, (b0 + n) // 128)], w=[pk])
            self.cp("act", wv_(dst[:, b0:b0 + n]), pb[:, 0:n], r=[pk], w=[dkey])
            yield

    def proj_fm(self, dst, dkey, wt, wkey, c0, r_extra=(), evac="act", func=None, wv=None):
        for blk in range(5):
            b0 = blk * 512
            n = min(512, T - b0)
            pb = self.PS[self.ps_rot % 4]
            pk = f"ps{self.ps_rot % 4}"
            self.ps_rot += 1
            for kc in range(8):
                self.mm(pb[:, 0:n], wt[:, kc, c0:c0 + 128], self.hT[:, kc, b0:b0 + n], kc == 0, kc == 7,
                        r=[wkey] + [f"hT:{t}" for t in range(b0 // 128, (b0 + n) // 128)] + list(r_extra), w=[pk])
            wv_ = wv or (lambda a: a)
            if func is None:
                self.cp("act", wv_(dst[:, b0:b0 + n]), pb[:, 0:n], r=[pk], w=[dkey])
            else:
                self.act(wv_(dst[:, b0:b0 + n]), pb[:, 0:n], func, r=[pk], w=[dkey])

    def build(self):
        nc = self.nc
        cfg = self.cfg
        d = self.d

        def din(name, shape):
            d[name] = nc.dram_tensor(name, shape, F32, kind="ExternalInput").ap()

        din("xin", [2, T, D]); din("cT", [128, 8, 3]); din("w_ada", [2, 1024, 6144]); din("b_ada", [2, 6144])
        din("b_adaT", [2, 128, 48]); din("w_in", [2, 1024, DIN]); din("cqw", [2, 128, 12, 4]); din("rgcw", [2, 128, 4, 4])
        din("rgcb", [2, 128, 4]); din("alog", [2, 8]); din("dtb", [2, 8]); din("onw", [2, 128])
        din("wbd", [2, 128, 16, 128]); din("rgb", [2, 128, 16]); din("lam", [2, 128, 8])
        din("w_out", [2, 1024, 1024]); din("ln_g", [2, 2, 1024]); din("ln_b", [2, 2, 1024]); din("wr", [2, 1024, 36])
        din("br", [2, 36]); din("weg", [2, 32, 1024, 256]); din("weu", [2, 32, 1024, 256]); din("wed", [2, 32, 256, 1024])
        din("cmat", [128, 10, 128])
        d["out"] = nc.dram_tensor("out", [2, 2048, D], F32, kind="ExternalOutput").ap()
        d["xs1"] = nc.dram_tensor("xs1", [2, T, D], F32, kind="Internal").ap()
        d["xs2"] = nc.dram_tensor("xs2", [2, T, D], F32, kind="Internal").ap()
        d["modrow"] = nc.dram_tensor("modrow", [2, 3, 6144], F32, kind="Internal").ap()
        d["combD"] = nc.dram_tensor("combD", [32, T], BF16, kind="Internal").ap()

        with ExitStack() as outer:
            self.S = Sched(nc, outer)
            S = self.S
            self.cmat = self.sbt(outer, "cmat_sb", [128, 10, 128])
            self.cm = [self.cmat[:, i, :] for i in range(10)]
            self.modT = self.sbt(outer, "modT", [128, 2, 48, 3])
            self.onep = self.sbt(outer, "onep", [128, 2, 48, 3])
            self.stat6 = self.sbt(outer, "stat6", [128, 8, 2, 6])
            self.statmv = self.sbt(outer, "statmv", [128, 8, 4])
            self.epsc = self.sbt(outer, "epsc", [128, 2])
            self.stat_i = 0
            self.ps_rot = 0
            self.PS = [outer.enter_context(nc.psum_tensor(f"psb{i}", [128, 512], F32)) for i in range(8)]
            self.ld("sp", self.cmat[:], d["cmat"][:, :, :], "cmat", w=["ident", "cmat"])
            self.cmatr = self.sbt(outer, "cmatr_sb", [128, 10, 128])
            self.S.op("dve", lambda e: e.tensor_copy(out=self.cmatr[:].bitcast(mybir.dt.float32r), in_=self.cmat[:]), ["cmat"], ["cmatr"])
            self.cmr = [self.cmatr[:, i, :].bitcast(mybir.dt.float32r) for i in range(10)]
            self.memset("pool", self.epsc[:, 0:1], LN_EPS, w=["epsc"])
            self.memset("pool", self.epsc[:, 1:2], NORM_EPS, w=["epsc"])
            self.stage0()
            S.emit()
            if cfg.get("stop") == "stage0":
                return
            for bi in range(cfg.get("nb", 2)):
                for l in range(cfg.get("nl", 2)):
                    self.layer(bi, l)
            S.emit()

    def stage0(self):
        nc, S, d = self.nc, self.S, self.d
        with ExitStack() as ph:
            cT = self.sbt(ph, "cT_sb", [128, 8, 3])
            scT = self.sbt(ph, "scT", [128, 8, 3])
            brow = self.sbt(ph, "brow", [3, 6144])
            bT = self.sbt(ph, "bT", [128, 48])
            wa = [self.sbt(ph, f"wa{i}", [128, 8, 512]) for i in range(2)]
            rowsb = [self.sbt(ph, f"rowsb{i}", [3, 512]) for i in range(2)]
            self.ld("sp", cT[:], d["cT"][:, :, :], "cT", w=["cT"])
            self.act(scT[:], cT[:], AF.Silu, r=["cT"], w=["scT"])
            for l in range(2):
                self.ld("sp", brow[:], d["b_ada"][l, :].partition_broadcast(3), "brow", w=["brow"])
                self.ld("sp", bT[:], d["b_adaT"][l, :, :], "bT", w=["bT"])
                psM = self.PS[l]
                for piece in range(12):
                    s = piece % 2
                    self.ld("sp", wa[s][:], d["w_ada"][l, :, piece * 512:(piece + 1) * 512].rearrange("(kc p) n -> p kc n", p=128),
                            f"wa{s}", w=[f"wa{s}"])
                    psR = self.PS[2 + s]
                    for kc in range(8):
                        self.mm(psR[0:3, 0:512], scT[:, kc, :], wa[s][:, kc, :], kc == 0, kc == 7, r=[f"wa{s}", "scT"], w=[f"psR{s}"])
                    self.tt("dve", rowsb[s][:], psR[0:3, 0:512], brow[:, piece * 512:(piece + 1) * 512], ALU.add,
                            r=[f"psR{s}", "brow"], w=[f"rowsb{s}"])
                    self.ld("sp", d["modrow"][l, :, piece * 512:(piece + 1) * 512], rowsb[s][:], f"rowst{s}", r=[f"rowsb{s}"], w=["modrow"])
                    for fc in range(4):
                        ch = piece * 4 + fc
                        self.S.op("pe", lambda e, ch=ch, fc=fc, s=s, psM=psM: e.transpose(psM[:, ch * 3:(ch + 1) * 3], rowsb[s][0:3, fc * 128:(fc + 1) * 128],
                                                                              self.cm[C_ID][0:3, 0:3]),
                                  [f"rowsb{s}", "ident"], [f"psM{l}"])
                self.cp("dve", self.modT[:, l, :, :], psM[:, 0:144].rearrange("p (c j) -> p c j", j=3), r=[f"psM{l}"], w=["modT"])
                self.ts("dve", self.onep[:, l, :, :], self.modT[:, l, :, :], 1.0, None, ALU.add, r=["modT"], w=["onep"])
            self.dump("modT", self.modT[:], "modT", [128, 2, 48, 3])
            S.barrier()

    def layer(self, bi, l):
        nc, S, d, cfg = self.nc, self.S, self.d, self.cfg
        last = (l == 1)
        src = d["xin"] if l == 0 else d["xs2"]
        stop = cfg.get("stop")
        with ExitStack() as bl:
            self.hT = self.sbt(bl, "hT", [128, 8, T], BF16)
            self.combT = self.sbt(bl, "combT", [32, T], BF16)
            with ExitStack() as ml:
                self.yT = self.sbt(ml, "yT", [128, 8, T], BF16)
                self.sm = {nm: self.sbt(ml, "sm_" + nm, [128, NT, 8]) for nm in ("BETA", "NBETA", "EGC", "NEGC", "EDEC", "GLB", "G")}
                self.cqw = self.sbt(ml, "cqw_sb", [128, 12, 4])
                self.rgcw = self.sbt(ml, "rgcw_sb", [128, 4, 4])
                self.rgcb = self.sbt(ml, "rgcb_sb", [128, 4])
                self.dtb = self.sbt(ml, "dtb_bc", [128, 8])
                self.nA = self.sbt(ml, "nA_bc", [128, 8])
                self.onw = self.sbt(ml, "onw_bc", [128, 128])
                self.rgb = self.sbt(ml, "rgb_sb", [128, 16])
                self.c1 = self.sbt(ml, "c1_sb", [128, 8])
                self.wbd = self.sbt(ml, "wbd_bf", [128, 16, 128], BF16)
                self.wsm = self.sbt(ml, "wsm_bf", [128, 8, 16], BF16)
                self.phaseA(bi, l, src)
                S.emit()
                self.dump_seq("hT", self.hT, BF16)
                if stop == "A":
                    self.dump_sm(); S.emit(); return
                self.phaseB(bi, l)
                S.emit()
                if stop == "B":
                    self.dump_seq("yT", self.yT, BF16); S.emit(); return
                for h in cfg.get("heads", range(4)):
                    self.phaseC(bi, l, h)
                    S.emit()
                if stop == "C":
                    self.dump_seq("yT", self.yT, BF16); S.emit(); return
                self.phaseD(bi, l, src)
                S.emit()
            if stop == "D":
                self.dump_seq("hT", self.hT, BF16)
                if "combT" in self.dbg:
                    self.dump("combT", self.combT[:], "combT", [32, T], BF16)
                S.emit(); return
            self.phaseE(bi, l)
            S.emit()

    def dump_seq(self, name, tile, dt):
        if name in self.dbg:
            o = self.nc.dram_tensor("d_" + name, [128, 8, T], dt, kind="ExternalOutput").ap()
            self.dbg_out[name] = o
            self.dbg.discard(name)
            self.S.dma("sp", lambda e: e.dma_start(out=o, in_=tile[:]), "dbg_" + name, [], ["dbgdram_" + name])
            self.S.barrier()

    def dump_sm(self):
        for nm, t in self.sm.items():
            self.dump("sm_" + nm, t[:], "sm", [128, NT, 8])

    def phaseA(self, bi, l, src):
        nc, S, d = self.nc, self.S, self.d
        sm = self.sm
        with ExitStack() as ph:
            xt = [self.sbt(ph, f"xt{i}", [128, D]) for i in range(4)]
            xn = [self.sbt(ph, f"xn{i}", [128, D]) for i in range(4)]
            tmp = self.sbt(ph, "smtmp", [128, 4, 5, 8])
            alog = self.sbt(ph, "alog_bc", [128, 8])
            lam = self.sbt(ph, "lam_sb", [128, 8])
            self.ld("sp", self.cqw[:], d["cqw"][l, :, :, :], "cqw", w=["cqw"])
            self.ld("sp", self.rgcw[:], d["rgcw"][l, :, :, :], "rgcw", w=["rgcw"])
            self.ld("sp", self.rgcb[:], d["rgcb"][l, :, :], "rgcb", w=["rgcb"])
            self.ld("sp", self.dtb[:], d["dtb"][l, :].partition_broadcast(128), "dtb", w=["dtb"])
            self.ld("sp", alog[:], d["alog"][l, :].partition_broadcast(128), "alog", w=["alog"])
            self.ld("sp", self.onw[:], d["onw"][l, :].partition_broadcast(128), "onw", w=["onw"])
            self.ld("sp", self.rgb[:], d["rgb"][l, :, :], "rgb", w=["rgb"])
            self.ld("sp", lam[:], d["lam"][l, :, :], "lam", w=["lam"])
            self.ld("pool", self.wbd[:], d["wbd"][l, :, :, :], "wbd", w=["wbd"])
            self.ld("pool", self.wsm[:], d["w_in"][l, :, 2048:2064].rearrange("(kc p) n -> p kc n", p=128), "wsm", w=["wsm"])
            self.act(self.nA[:], alog[:], AF.Exp, r=["alog"], w=["nA"])
            self.ts("dve", self.nA[:], self.nA[:], -1.0, None, ALU.mult, r=["nA"], w=["nA"])
            self.act(self.c1[:], lam[:], AF.Exp, r=["lam"], w=["c1"], scale=-1.0)
            self.act(self.c1[:], self.c1[:], AF.Ln, r=["c1"], w=["c1"], bias=1.0)
            self.ts("dve", self.c1[:], self.c1[:], -8.0, None, ALU.mult, r=["c1"], w=["c1"])
            def tileA(t):
                j = 2 if t < 2 else bi
                s3 = t % 4
                s2 = t % 4
                self.ld("sp", xt[s3][:], src[bi, t * 128:(t + 1) * 128, :], f"xt{s3}", w=[f"xt{s3}"])
                st = {}
                yield from self.ln_stats_g(xt[s3], f"xt{s3}", st)
                self.ts("dve", xn[s2][:], xt[s3][:], st["mean"], st["rstd"], ALU.subtract, ALU.mult, r=[f"xt{s3}", st["k"]], w=[f"xn{s2}"])
                yield
                for kc in range(8):
                    pb = self.PS[s2 * 2 + kc // 4]
                    pk = f"ps{s2 * 2 + kc // 4}:{kc % 4}"
                    self.tr(pb[:, (kc % 4) * 128:(kc % 4 + 1) * 128], xn[s2][:, kc * 128:(kc + 1) * 128], r=[f"xn{s2}", "ident"], w=[pk])
                yield
                for kc in range(8):
                    pb = self.PS[s2 * 2 + kc // 4]
                    pk = f"ps{s2 * 2 + kc // 4}:{kc % 4}"
                    self.act(self.hT[:, kc, t * 128:(t + 1) * 128], pb[:, (kc % 4) * 128:(kc % 4 + 1) * 128], AF.Identity,
                             r=[pk, "onep", "modT"], w=[f"hT:{t}"], scale=self.onep[:, l, 8 + kc, j:j + 1], bias=self.modT[:, l, kc, j:j + 1])
                yield
                p16 = self.PS[s2 * 2]
                k16 = f"ps{s2 * 2}"
                for kc in range(8):
                    self.mm(p16[:, 0:16], self.hT[:, kc, t * 128:(t + 1) * 128], self.wsm[:, kc, :], kc == 0, kc == 7,
                            r=[f"hT:{t}", "wsm"], w=[k16])
                yield
                smk = f"sm:{t}"
                tk = f"smtmp{s2}"
                self.act(tmp[:, s2, 4, :], p16[:, 0:8], AF.Exp, r=[k16], w=[tk + "b"], scale=-1.0)
                self.tt("dve", tmp[:, s2, 0, :], p16[:, 8:16], self.dtb[:], ALU.add, r=[k16, "dtb"], w=[tk])
                yield
                self.ts("dve", tmp[:, s2, 4, :], tmp[:, s2, 4, :], 1.0, None, ALU.add, r=[tk + "b"], w=[tk + "b"])
                self.act(tmp[:, s2, 1, :], tmp[:, s2, 0, :], AF.Exp, r=[tk], w=[tk])
                yield
                S.op("dve", lambda e: e.reciprocal(out=sm["BETA"][:, t, :], in_=tmp[:, s2, 4, :]), [tk + "b"], [smk])
                yield
                self.ts("dve", sm["NBETA"][:, t, :], sm["BETA"][:, t, :], -1.0, None, ALU.mult, r=[smk], w=[smk])
                yield
                self.act(tmp[:, s2, 2, :], tmp[:, s2, 1, :], AF.Ln, r=[tk], w=[tk], bias=1.0)
                yield
                self.tt("dve", sm["G"][:, t, :], tmp[:, s2, 2, :], self.nA[:], ALU.mult, r=[tk, "nA"], w=[smk])
                yield
                self.mm(p16[:, 16:20], self.cm[C_TRIF], sm["G"][:, t, 0:4], r=[smk, "cmat"], w=[k16])
                self.mm(p16[:, 20:24], self.cm[C_TRIB], sm["G"][:, t, 4:8], r=[smk, "cmat"], w=[k16])
                self.mm(p16[:, 32:40], self.cm[C_ONES], sm["G"][:, t, :], r=[smk, "cmat"], w=[k16])
                yield
                self.act(sm["EGC"][:, t, :], p16[:, 16:24], AF.Exp, r=[k16], w=[smk])
                self.act(sm["GLB"][:, t, :], p16[:, 32:40], AF.Exp, r=[k16], w=[smk])
                self.cp("act", tmp[:, s2, 3, :], p16[:, 16:24], r=[k16], w=[tk])
                yield
                self.ts("dve", sm["NEGC"][:, t, :], sm["EGC"][:, t, :], -1.0, None, ALU.mult, r=[smk], w=[smk])
                self.tt("dve", tmp[:, s2, 3, :], p16[:, 32:40], tmp[:, s2, 3, :], ALU.subtract, r=[k16, tk], w=[tk])
                yield
                self.act(sm["EDEC"][:, t, :], tmp[:, s2, 3, :], AF.Exp, r=[tk], w=[smk])
                yield

            self.pipe([(lambda t=t: tileA(t)) for t in range(NT)], ni=4, skew=4)
            S.barrier()

    def phaseB(self, bi, l):
        nc, S, d = self.nc, self.S, self.d
        with ExitStack() as ph:
            wrg = [self.sbt(ph, f"wrg{i}", [128, 8, 256], BF16) for i in range(2)]
            RAW = self.sbt(ph, "RAW", [128, T])
            XC = self.sbt(ph, "XC", [128, T])
            XCB = self.sbt(ph, "XCB", [128, T], BF16)
            Rt = self.sbt(ph, "Rt", [128, T])
            It = self.sbt(ph, "It", [128, T])
            A2 = self.sbt(ph, "A2", [128, T])
            H = [self.sbt(ph, f"H{i}", [128, T]) for i in range(2)]
            for ch in range(4):
                s = ch % 2
                wk = f"wrg{s}"
                for part, c0 in ((0, 2064 + ch * 128), (1, 2576 + ch * 128)):
                    self.ld("pool", wrg[s][:, :, part * 128:(part + 1) * 128],
                            d["w_in"][l, :, c0:c0 + 128].rearrange("(kc p) n -> p kc n", p=128), f"wrg{s}_{part}", w=[f"{wk}:{part}"])
                self.proj_fm(RAW, "RAW", wrg[s], f"{wk}:0", 0)
                self.conv("dve", XC, RAW, self.rgcw[:, ch, :], self.rgcb[:, ch:ch + 1], ["RAW", "rgcw", "rgcb"], "XC")
                self.cp("act", XCB[:], XC[:], r=["XC"], w=["XCB"])
                if ch == 0:
                    self.dump("xc0", XC[:], "XC", [128, T])
                for dr in range(2):
                    for gate, dst, dk in ((0, Rt, "Rt"), (1, It, "It")):
                        idx = (dr * 2 + gate) * 4 + ch
                        for blk in range(5):
                            b0 = blk * 512
                            n = min(512, T - b0)
                            pb = self.PS[self.ps_rot % 4]
                            pk = f"ps{self.ps_rot % 4}"
                            self.ps_rot += 1
                            self.mm(pb[:, 0:n], self.wbd[:, idx, :], XCB[:, b0:b0 + n], r=["wbd", "XCB"], w=[pk])
                            self.act(dst[:, b0:b0 + n], pb[:, 0:n], AF.Sigmoid, r=[pk, "rgb"], w=[dk], bias=self.rgb[:, idx:idx + 1])
                    self.act(Rt[:], Rt[:], AF.Exp, r=["Rt", "c1"], w=["Rt"], scale=self.c1[:, dr * 4 + ch:dr * 4 + ch + 1])
                    self.act(A2[:], Rt[:], AF.Square, r=["Rt"], w=["A2"])
                    self.act(A2[:], A2[:], AF.Sqrt, r=["A2"], w=["A2"], scale=-1.0, bias=1.0)
                    self.tt("dve", It[:], It[:], XC[:], ALU.mult, r=["It", "XC"], w=["It"])
                    self.tt("dve", It[:], It[:], A2[:], ALU.mult, r=["It", "A2"], w=["It"])
                    hk = f"H{dr}"
                    if dr == 0:
                        S.op("dve", lambda e: e.tensor_tensor_scan(out=H[0][:, :], data0=Rt[:, :], data1=It[:, :], initial=0.0,
                                                                   op0=ALU.mult, op1=ALU.add), ["Rt", "It"], [hk])
                    else:
                        S.op("dve", lambda e: e.tensor_tensor_scan(out=H[1][:, 0:NCTX][:, ::-1], data0=Rt[:, 0:NCTX][:, ::-1],
                                                                   data1=It[:, 0:NCTX][:, ::-1], initial=0.0,
                                                                   op0=ALU.mult, op1=ALU.add), ["Rt", "It"], [hk])
                        S.op("dve", lambda e: e.tensor_tensor_scan(out=H[1][:, NCTX:T][:, ::-1], data0=Rt[:, NCTX:T][:, ::-1],
                                                                   data1=It[:, NCTX:T][:, ::-1], initial=H[1][:, 0:1],
                                                                   op0=ALU.mult, op1=ALU.add), ["Rt", "It", hk], [hk])
                self.tt("dve", H[0][:], H[0][:], H[1][:], ALU.add, r=["H0", "H1"], w=["H0"])
                if ch == 0:
                    self.dump("hr0", H[0][:], "H0", [128, T])
                self.proj_fm(RAW, "RAW", wrg[s], f"{wk}:1", 128, func=AF.Gelu_apprx_tanh)
                self.tt("dve", self.yT[:, 4 + ch, :], RAW[:], H[0][:], ALU.mult, r=["RAW", "H0"], w=[f"yT:{4 + ch}"])
            S.barrier()

    def phaseC(self, bi, l, h):
        nc, S, d = self.nc, self.S, self.d
        sm = self.sm
        PS = self.PS
        cm = self.cm
        cmr = self.cmr
        r_ = lambda ap: ap
        cmr = cm
        F32R = mybir.dt.float32r
        rr_ = lambda ap: ap.bitcast(F32R)
        with ExitStack() as ph:
            wq = self.sbt(ph, "wq", [128, 8, 512], BF16)
            RAW = self.sbt(ph, "RAWc", [128, T])
            CV = self.sbt(ph, "CVc", [128, T])
            QT = self.sbt(ph, "QT", [128, T])
            KT = self.sbt(ph, "KT", [128, T])
            KTOK = self.sbt(ph, "KTOK", [128, NT, 128])
            VTOK = self.sbt(ph, "VTOK", [128, NT, 128])
            QTOK = self.sbt(ph, "QTOK", [128, NT, 128])
            OACC = self.sbt(ph, "OACC", [128, NT, 128])
            Sst = [self.sbt(ph, f"Sst{i}", [128, 128]) for i in range(2)]
            RING = [[self.sbt(ph, f"RING_{dr}_{i}", [128, 640]) for i in range(3)] for dr in range(2)]
            for part, c0 in enumerate((h * 128, 512 + h * 128, 1024 + h * 128, 1536 + h * 128)):
                self.ld("pool", wq[:, :, part * 128:(part + 1) * 128],
                        d["w_in"][l, :, c0:c0 + 128].rearrange("(kc p) n -> p kc n", p=128), f"wq_{part}", w=[f"wq:{part}"])
            flat = lambda tl: tl[:].rearrange("p a b -> p (a b)")
            KTOKf, VTOKf, QTOKf, OACCf = flat(KTOK), flat(VTOK), flat(QTOK), flat(OACC)

            def prep_chain(which, raw, kraw, cvt, kcv, dst, kdst, wv_s, wv_d):
                yield from self.proj_fm_g(raw, kraw, wq, f"wq:{which}", which * 128, wv=wv_s)
                yield from self.conv_g("dve", cvt, raw, self.cqw[:, which * 4 + h, :], None, [kraw, "cqw"], kcv, wv=wv_s)
                self.act(wv_d(dst[:, :]), cvt[:, :], AF.Silu, r=[kcv], w=[kdst])
                yield
                if which < 2:
                    self.tt("pool", wv_s(cvt[:, :]), dst[:, :], dst[:, :], ALU.mult, r=[kdst], w=[kcv])
                    yield
                    for blk in range(5):
                        b0 = blk * 512
                        n = min(512, T - b0)
                        pb = PS[self.ps_rot % 4]
                        pk = f"ps{self.ps_rot % 4}"
                        self.ps_rot += 1
                        self.mm(pb[:, 0:n], cm[C_ONES], cvt[:, b0:b0 + n], r=["cmat", kcv], w=[pk])
                        self.act(wv_s(raw[:, b0:b0 + n]), pb[:, 0:n], AF.Sqrt, r=[pk, "epsc"], w=[kraw], bias=self.epsc[:, 1:2], scale=1.0)
                        yield

                    def recip(e):
                        with self.nc.allow_low_precision("fp32r-rounded rsqrt scratch"):
                            return e.reciprocal(out=wv_s(raw[:, :]), in_=raw[:, :])
                    S.op("dve", recip, [kraw], [kraw])
                    yield
                    self.stt("dve", wv_d(dst[:, :]), dst[:, :], (128.0 ** -0.5) if which == 0 else 1.0, raw[:, :], ALU.mult, ALU.mult,
                             r=[kdst, kraw], w=[kdst])
                    yield

            self.pipe([
                lambda: prep_chain(1, RAW, "RAWc", CV, "CVc", KT, "KT", r_, rr_),
                lambda: prep_chain(0, KTOKf, "KTOKs", VTOKf, "VTOKs", QT, "QT", rr_, rr_),
                lambda: prep_chain(2, QTOKf, "QTOKs", OACCf, "OACCs", OACCf, "OACCs", r_, r_),
            ], ni=3, skew=1)
            S.barrier()
            if h == 0:
                self.dump("q0", QT[:], "QT", [128, T]); self.dump("k0", KT[:], "KT", [128, T]); self.dump("v0", OACCf, "OACCs", [128, T])
            cnt = 0
            for t in range(NT):
                for srcT, sk, dstT, dk2 in ((KT, "KT", KTOK, "KTOK"), (OACCf, "OACCs", VTOK, "VTOK"), (QT, "QT", QTOK, "QTOK")):
                    bank = 4 + cnt % 4
                    pb = PS[bank]
                    pk = f"ps{bank}"
                    self.tr(pb[:, 0:128], srcT[:, t * 128:(t + 1) * 128], r=[sk, "ident"], w=[pk])
                    self.cp("act" if cnt % 2 == 0 else "dve", (rr_ if dk2 != "QTOK" else r_)(dstT[:, t, :]), pb[:, 0:128], r=[pk], w=[f"{dk2}:{t}"])
                    cnt += 1
            S.barrier()
            carve_state = {"i": 0}

            def carve(n):
                i = carve_state["i"]
                if i < T and i + n > T:
                    i = T
                assert i + n <= 2 * T
                carve_state["i"] = i + n
                return (RAW if i < T else CV)[:, (i % T):(i % T) + n]

            NI = 4
            GB = [carve(128) for j in range(NI)]
            DD = [carve(256) for j in range(NI)]
            WA = [carve(384) for j in range(NI)]
            WB = [carve(384) for j in range(NI)]
            smallt = self.sbt(ph, "csmall", [128, 8, 128])
            vnt = self.sbt(ph, "cvn", [128, 2, 128])
            ttft = self.sbt(ph, "cttf", [128, NI, 128])
            ket = self.sbt(ph, "cke", [128, NI, 128])
            VN = [vnt[:, dr, :] for dr in range(2)]
            O1 = [smallt[:, 2 + dr, :] for dr in range(2)]
            zs = [smallt[:, 4 + i, :] for i in range(2)]
            y1 = [smallt[:, 6 + i, :] for i in range(2)]
            self.ts("dve", rr_(Sst[0][:]), cm[C_ONES], 0.0, None, ALU.mult, r=["cmat"], w=["S0"])
            self.ts("dve", rr_(Sst[1][:]), cm[C_ONES], 0.0, None, ALU.mult, r=["cmat"], w=["S1"])
            order = [list(range(NT)), [1, 0] + list(range(NT - 1, 1, -1))]
            oacc_written = set()

            def evac_eng(n, k):
                return "act" if (n + k) % 2 == 0 else "dve"

            def pre(n):
                i, dr = n // 2, n % 2
                t = order[dr][i]
                col = dr * 4 + h
                j = n % NI
                sl = i % 3
                bank = PS[4 + n % 4]
                bk = f"ps{4 + n % 4}"
                gb, dd, wa, wb = GB[j], DD[j], WA[j], WB[j]
                kgb, kdd, kwa, kwb = f"GB{j}", f"DD{j}", f"WA{j}", f"WB{j}"
                ring = RING[dr][sl]
                rk = f"RING_{dr}_{sl}"
                tri, ntri, ms, mit = (cmr[C_TRIF], cmr[C_NTRIF], cmr[C_MSF], cmr[C_MITF]) if dr == 0 else (cmr[C_TRIB], cmr[C_NTRIB], cmr[C_MSB], cmr[C_MITB])
                idr = cmr[C_ID]
                tok = slice(t * 128, (t + 1) * 128)
                smk = f"sm:{t}"
                self.act(r_(gb), cm[C_ONES], AF.Identity, r=["cmat", smk], w=[kgb], scale=sm["G"][:, t, col:col + 1])
                self.mm(bank[:, 0:128], rr_(KT[:, tok]), rr_(KT[:, tok]), r=["KT"], w=[bk])
                self.mm(bank[:, 128:256], rr_(KT[:, tok]), rr_(QT[:, tok]), r=["KT", "QT"], w=[bk])
                self.mm(bank[:, 256:384], tri, r_(gb), True, False, r=["cmatr", kgb], w=[bk])
                self.mm(bank[:, 256:384], r_(gb), ntri, False, False, r=["cmatr", kgb], w=[bk])
                self.mm(bank[:, 256:384], idr, ms, False, True, r=["cmatr"], w=[bk])
                self.mm(bank[:, 384:512], r_(gb), tri, True, False, r=["cmatr", kgb], w=[bk])
                self.mm(bank[:, 384:512], ntri, r_(gb), False, False, r=["cmatr", kgb], w=[bk])
                self.mm(bank[:, 384:512], idr, mit, False, True, r=["cmatr"], w=[bk])
                yield
                self.act(r_(dd[:, 0:256]), bank[:, 256:512], AF.Exp, r=[bk], w=[kdd])
                self.stt("dve", r_(wa[:, 256:384]), bank[:, 0:128], sm["NBETA"][:, t, col:col + 1], dd[:, 0:128], ALU.mult, ALU.mult,
                         r=[bk, smk, kdd], w=[kwa])
                self.tt("dve", rr_(ring[:, 384:512]), bank[:, 128:256], dd[:, 128:256], ALU.mult, r=[bk, kdd], w=[rk])
                yield
                self.tr(bank[:, 0:128], wa[:, 256:384], r=[kwa, "ident"], w=[bk])
                yield
                self.cp(evac_eng(n, 0), r_(wa[:, 0:128]), bank[:, 0:128], r=[bk], w=[kwa])
                yield
                self.mm(bank[:, 0:128], r_(wa[:, 256:384]), r_(wa[:, 0:128]), r=[kwa], w=[bk])
                self.mm(bank[:, 256:384], r_(wa[:, 0:128]), r_(wa[:, 256:384]), r=[kwa], w=[bk])
                yield
                self.cp("act", wb[:, 0:384].rearrange("p (a b) -> p a b", b=128)[:, ::2, :],
                        bank[:, 0:384].rearrange("p (a b) -> p a b", b=128)[:, ::2, :], r=[bk], w=[kwb])
                self.tt("dve", wb[:, 128:256], wa[:, 0:128], cm[C_ID], ALU.add, r=[kwa, "cmat"], w=[kwb])
                yield
                cur, kcur, nxt, knxt = wb, kwb, wa, kwa
                for k in range(1, 6):
                    self.mm(bank[:, 0:256], r_(cur[:, 256:384]), r_(cur[:, 0:256]), r=[kcur], w=[bk])
                    self.mm(bank[:, 256:384], r_(cur[:, 0:128]), r_(cur[:, 256:384]), r=[kcur], w=[bk])
                    yield
                    self.cp("act", nxt[:, 0:384].rearrange("p (a b) -> p a b", b=128)[:, ::2, :],
                            bank[:, 0:384].rearrange("p (a b) -> p a b", b=128)[:, ::2, :], r=[bk], w=[knxt])
                    self.tt("dve", nxt[:, 128:256], bank[:, 128:256], cur[:, 128:256], ALU.add, r=[bk, kcur], w=[knxt])
                    cur, kcur, nxt, knxt = nxt, knxt, cur, kcur
                    yield
                self.mm(bank[:, 0:128], r_(cur[:, 256:384]), r_(cur[:, 128:256]), True, False, r=[kcur], w=[bk])
                self.mm(bank[:, 0:128], idr, r_(cur[:, 128:256]), False, True, r=[kcur, "cmatr"], w=[bk])
                yield
                self.act(rr_(ttft[:, j, :]), bank[:, 0:128], AF.Identity, r=[bk, smk], w=[f"TTF{j}"], scale=sm["BETA"][:, t, col:col + 1])
                self.act(rr_(ket[:, j, :]), KTOK[:, t, :], AF.Identity, r=[f"KTOK:{t}", smk], w=[f"KE{j}"], scale=sm["EGC"][:, t, col:col + 1])
                self.ts("dve", r_(nxt[:, 256:384]), QTOK[:, t, :], sm["EGC"][:, t, col:col + 1], None, ALU.mult, r=[f"QTOK:{t}", smk], w=[knxt])
                self.ts("dve", rr_(ring[:, 512:640]), KTOK[:, t, :], sm["EDEC"][:, t, col:col + 1], None, ALU.mult, r=[f"KTOK:{t}", smk], w=[rk])
                yield
                self.mm(bank[:, 0:128], rr_(ttft[:, j, :]), rr_(VTOK[:, t, :]), r=[f"TTF{j}", f"VTOK:{t}"], w=[bk])
                self.mm(bank[:, 128:256], rr_(ket[:, j, :]), rr_(ttft[:, j, :]), r=[f"KE{j}", f"TTF{j}"], w=[bk])
                self.tr(bank[:, 256:384], nxt[:, 256:384], r=[knxt, "ident"], w=[bk])
                yield
                self.cp(evac_eng(n, 0), rr_(ring[:, 0:384]), bank[:, 0:384], r=[bk], w=[rk])
                yield

            def step(i, dr):
                t = order[dr][i]
                col = dr * 4 + h
                sl = i % 3
                ring = RING[dr][sl]
                rk = f"RING_{dr}_{sl}"
                A = PS[dr * 2]
                B = PS[dr * 2 + 1]
                ka = f"ps{dr * 2}"
                kb = f"ps{dr * 2 + 1}"
                Sd = Sst[dr]
                skey = f"S{dr}"
                vn, o1 = VN[dr], O1[dr]
                kvn, ko1 = f"VN{dr}", f"O1{dr}"
                smk = f"sm:{t}"
                self.mm(A[:, 0:128], rr_(ring[:, 128:256]), rr_(Sd[:]), r=[rk, skey], w=[ka])
                yield
                self.tt("dve", rr_(vn), ring[:, 0:128], A[:, 0:128], ALU.subtract, r=[rk, ka], w=[kvn])
                yield
                self.mm(B[:, 0:128], rr_(ring[:, 256:384]), rr_(Sd[:]), True, False, r=[rk, skey], w=[kb])
                self.mm(B[:, 0:128], rr_(ring[:, 384:512]), rr_(vn), False, True, r=[rk, kvn], w=[kb])
                self.mm(A[:, 128:256], rr_(ring[:, 512:640]), rr_(vn), r=[rk, kvn], w=[ka])
                yield
                self.stt("dve", rr_(Sd[:]), Sd[:], sm["GLB"][:, t, col:col + 1], A[:, 128:256], ALU.mult, ALU.add,
                         r=[skey, smk, ka], w=[skey])
                if t not in oacc_written:
                    oacc_written.add(t)
                    self.cp("act", OACC[:, t, :], B[:, 0:128], r=[kb], w=[f"OACC:{t}"])
                else:
                    self.cp("act", r_(o1), B[:, 0:128], r=[kb], w=[ko1])
                    self.tt("pool", OACC[:, t, :], OACC[:, t, :], o1, ALU.add, r=[f"OACC:{t}", ko1], w=[f"OACC:{t}"])
                yield

            nitems = 2 * NT
            next_item = 0
            active = []
            pre_done = set()
            steps_emitted = [0, 0]
            chain = [None, None]
            chain_i = [0, 0]
            while True:
                progressed = False
                while len(active) < NI and next_item < nitems:
                    n = next_item
                    i, dr = n // 2, n % 2
                    if i >= 3 and steps_emitted[dr] < i - 2:
                        break
                    assert all(a[0] % NI != n % NI for a in active)
                    active.append((n, pre(n)))
                    next_item += 1
                for (n, g) in list(active):
                    try:
                        next(g)
                    except StopIteration:
                        active.remove((n, g))
                        pre_done.add(n)
                    progressed = True
                for dr in range(2):
                    if chain[dr] is None and chain_i[dr] < NT and (2 * chain_i[dr] + dr) in pre_done:
                        chain[dr] = step(chain_i[dr], dr)
                    if chain[dr] is not None:
                        try:
                            next(chain[dr])
                        except StopIteration:
                            chain[dr] = None
                            steps_emitted[dr] += 1
                            chain_i[dr] += 1
                        progressed = True
                if not progressed:
                    break
            assert steps_emitted == [NT, NT], steps_emitted
            S.barrier()
            if h == 0:
                self.dump("oacc0", OACC[:], "OACC:0", [128, NT, 128])
            self.dump(f"oacch{h}", OACC[:], "OACC:0", [128, NT, 128])
            st6 = self.sbt(ph, "cst6", [128, 4, 6])
            mv = self.sbt(ph, "cmv", [128, 4, 6])
            zs4 = [smallt[:, i, :] for i in range(4)]
            y14 = [smallt[:, 4 + i, :] for i in range(4)]

            def tileY(t):
                s2 = t % 4
                kst, kmv, kz, ky = f"cst6{s2}", f"cmv{s2}", f"zs{s2}", f"y1{s2}"
                pz = PS[s2]
                pk = f"ps{s2}"
                S.op("dve", lambda e: e.bn_stats(out=st6[:, s2, :], in_=OACC[:, t, :]), [f"OACC:{t}"], [kst])
                for kc in range(8):
                    self.mm(pz[:, 0:128], self.hT[:, kc, t * 128:(t + 1) * 128], wq[:, kc, 384:512], kc == 0, kc == 7,
                            r=[f"hT:{t}", "wq:3"], w=[pk])
                yield
                S.op("dve", lambda e: e.bn_aggr(out=mv[:, s2, 0:2], in_=st6[:, s2, :]), [kst], [kmv])
                self.act(zs4[s2], pz[:, 0:128], AF.Exp, r=[pk], w=[kz], scale=-1.0)
                yield
                self.ts("dve", zs4[s2], zs4[s2], 1.0, None, ALU.add, r=[kz], w=[kz])
                yield
                S.op("dve", lambda e: e.reciprocal(out=zs4[s2], in_=zs4[s2]), [kz], [kz])
                yield
                self.tt("dve", mv[:, s2, 2:3], mv[:, s2, 0:1], mv[:, s2, 0:1], ALU.mult, r=[kmv], w=[kmv])
                yield
                self.tt("dve", mv[:, s2, 2:3], mv[:, s2, 2:3], mv[:, s2, 1:2], ALU.add, r=[kmv], w=[kmv])
                yield
                self.act(mv[:, s2, 3:4], mv[:, s2, 2:3], AF.Ln, r=[kmv, "epsc"], w=[kmv], bias=self.epsc[:, 1:2], scale=1.0)
                yield
                self.act(mv[:, s2, 4:5], mv[:, s2, 3:4], AF.Exp, r=[kmv], w=[kmv], scale=-0.5)
                yield
                self.stt("dve", r_(y14[s2]), OACC[:, t, :], mv[:, s2, 4:5], self.onw[:], ALU.mult, ALU.mult,
                         r=[f"OACC:{t}", kmv, "onw"], w=[ky])
                yield
                self.tt("dve", r_(y14[s2]), y14[s2], zs4[s2], ALU.mult, r=[ky, kz], w=[ky])
                yield
                self.tt("dve", r_(y14[s2]), y14[s2], pz[:, 0:128], ALU.mult, r=[ky, pk], w=[ky])
                yield
                self.tr(pz[:, 128:256], y14[s2], r=[ky, "ident"], w=[pk])
                yield
                self.cp("act", self.yT[:, h, t * 128:(t + 1) * 128], pz[:, 128:256], r=[pk], w=[f"yT:{h}"])
                yield

            self.pipe([(lambda t=t: tileY(t)) for t in range(NT)], ni=4, skew=3)
            S.barrier()

    def phaseD(self, bi, l, src):
        nc, S, d = self.nc, self.S, self.d
        PS = self.PS
        last = (l == 1)
        NI = 4
        with ExitStack() as ph:
            wo = self.sbt(ph, "wo_bf", [128, 8, 1024], BF16)
            wr = self.sbt(ph, "wr_bf", [128, 8, 36], BF16)
            rb = self.sbt(ph, "rb_bc", [128, 36])
            gt = [self.sbt(ph, f"gt1_{i}", [128, D]) for i in range(2)]
            lng = self.sbt(ph, "lng_bc", [128, D])
            lnb = self.sbt(ph, "lnb_bc", [128, D])
            xt = [self.sbt(ph, f"xtD{i}", [128, D]) for i in range(NI)]
            rt = [self.sbt(ph, f"rtD{i}", [128, D]) for i in range(NI)]
            xn = [self.sbt(ph, f"xnD{i}", [128, D]) for i in range(NI)]
            rs = [self.sbt(ph, f"rsD{i}", [128, 128]) for i in range(NI)]
            for half in range(2):
                self.ld("pool", wo[:, :, half * 512:(half + 1) * 512],
                        d["w_out"][l, :, half * 512:(half + 1) * 512].rearrange("(kc p) n -> p kc n", p=128), f"wo{half}", w=[f"wo:{half}"])
            self.ld("pool", wr[:], d["wr"][l, :, :].rearrange("(kc p) n -> p kc n", p=128), "wr", w=["wr"])
            self.ld("sp", rb[:], d["br"][l, :].partition_broadcast(128), "rb", w=["rb"])
            self.ld("sp", gt[0][:], d["modrow"][l, bi, 2048:3072].partition_broadcast(128), "gt0", r=["modrow"], w=["gt0"])
            self.ld("sp", gt[1][:], d["modrow"][l, 2, 2048:3072].partition_broadcast(128), "gt1", r=["modrow"], w=["gt1"])
            self.ld("sp", lng[:], d["ln_g"][l, 0, :].partition_broadcast(128), "lng", w=["lng"])
            self.ld("sp", lnb[:], d["ln_b"][l, 0, :].partition_broadcast(128), "lnb", w=["lnb"])
            tiles = list(range(2 if last else 0, NT))

            def tileD(idx, t):
                j = 2 if t < 2 else bi
                g = 1 if t < 2 else 0
                s = idx % NI
                tok = slice(t * 128, (t + 1) * 128)
                bank = [PS[s * 2], PS[s * 2 + 1]]
                bkey = [f"ps{s * 2}", f"ps{s * 2 + 1}"]
                kx, kr, kn, krs = f"xtD{s}", f"rtD{s}", f"xnD{s}", f"rsD{s}"
                self.ld("sp", xt[s][:], src[bi, tok, :], kx, w=[kx])
                for half in range(2):
                    for c in range(8):
                        self.mm(bank[half][:, 0:512], self.yT[:, c, tok], wo[:, c, half * 512:(half + 1) * 512], c == 0, c == 7,
                                r=[f"yT:{c}", f"wo:{half}"], w=[bkey[half]])
                yield
                for half in range(2):
                    self.tt("dve", rt[s][:, half * 512:(half + 1) * 512], bank[half][:, 0:512], gt[g][:, half * 512:(half + 1) * 512], ALU.mult,
                            r=[bkey[half], f"gt{g}"], w=[kr])
                yield
                self.stt("dve", rt[s][:], xt[s][:], ALPHA, rt[s][:], ALU.mult, ALU.add, r=[kx, kr], w=[kr])
                yield
                st = {}
                yield from self.ln_stats_g(rt[s], kr, st)
                self.ts("dve", rt[s][:], rt[s][:], st["mean"], st["rstd"], ALU.subtract, ALU.mult, r=[kr, st["k"]], w=[kr])
                yield
                self.tt("pool", rt[s][:], rt[s][:], lng[:], ALU.mult, r=[kr, "lng"], w=[kr])
                yield
                self.tt("pool", rt[s][:], rt[s][:], lnb[:], ALU.add, r=[kr, "lnb"], w=[kr])
                yield
                self.ld("sp", d["xs1"][bi, tok, :], rt[s][:], f"x1st{s}", r=[kr], w=[f"xs1:{t}"])
                if t == 2:
                    self.dump("x1_t2", rt[s][:], kr, [128, D])
                st2 = {}
                yield from self.ln_stats_g(rt[s], kr, st2)
                self.ts("dve", xn[s][:], rt[s][:], st2["mean"], st2["rstd"], ALU.subtract, ALU.mult, r=[kr, st2["k"]], w=[kn])
                yield
                for kc in range(8):
                    q = kc % 4
                    self.tr(bank[kc // 4][:, q * 128:(q + 1) * 128], xn[s][:, kc * 128:(kc + 1) * 128], r=[kn, "ident"], w=[bkey[kc // 4]])
                yield
                for kc in range(8):
                    q = kc % 4
                    self.act(self.hT[:, kc, tok], bank[kc // 4][:, q * 128:(q + 1) * 128], AF.Identity, r=[bkey[kc // 4], "onep", "modT"], w=[f"hT:{t}"],
                             scale=self.onep[:, l, 32 + kc, j:j + 1], bias=self.modT[:, l, 24 + kc, j:j + 1])
                yield
                pr = bank[0]
                prk = bkey[0]
                for kc in range(8):
                    self.mm(pr[:, 0:36], self.hT[:, kc, tok], wr[:, kc, :], kc == 0, kc == 7, r=[f"hT:{t}", "wr"], w=[prk])
                yield
                R = rs[s]
                rk = krs
                self.tt("dve", R[:, 0:36], pr[:, 0:36], rb[:], ALU.add, r=[prk, "rb"], w=[rk])
                yield
                sc = lambda i: R[:, 120 + i:121 + i]
                S.op("dve", lambda e: e.reduce_max(out=R[:, 120:121], in_=R[:, 0:4], axis=mybir.AxisListType.X), [rk], [rk])
                yield
                self.ts("dve", R[:, 36:40], R[:, 0:4], sc(0), None, ALU.subtract, r=[rk], w=[rk])
                self.ts("dve", R[:, 40:44], R[:, 0:4], sc(0), None, ALU.is_ge, r=[rk], w=[rk])
                yield
                self.act(R[:, 36:40], R[:, 36:40], AF.Exp, r=[rk], w=[rk])
                self.ts("dve", R[:, 40:44], R[:, 40:44], 1.0, 1e30, ALU.subtract, ALU.mult, r=[rk], w=[rk])
                yield
                S.op("dve", lambda e: e.reduce_sum(out=R[:, 121:122], in_=R[:, 36:40], axis=mybir.AxisListType.X), [rk], [rk])
                for g4 in range(4):
                    self.ts("dve", R[:, 44 + g4 * 8:52 + g4 * 8], R[:, 4 + g4 * 8:12 + g4 * 8], R[:, 40 + g4:41 + g4], None, ALU.add, r=[rk], w=[rk])
                yield
                S.op("dve", lambda e: e.reciprocal(out=R[:, 122:123], in_=R[:, 121:122]), [rk], [rk])
                S.op("dve", lambda e: e.reduce_max(out=R[:, 123:124], in_=R[:, 44:76], axis=mybir.AxisListType.X), [rk], [rk])
                yield
                self.ts("dve", R[:, 76:108], R[:, 44:76], sc(3), None, ALU.is_ge, r=[rk], w=[rk])
                yield
                self.stt("dve", R[:, 44:76], R[:, 76:108], -1e30, R[:, 44:76], ALU.mult, ALU.add, r=[rk], w=[rk])
                yield
                S.op("dve", lambda e: e.reduce_max(out=R[:, 124:125], in_=R[:, 44:76], axis=mybir.AxisListType.X), [rk], [rk])
                yield
                self.ts("dve", R[:, 44:76], R[:, 44:76], sc(4), None, ALU.is_ge, r=[rk], w=[rk])
                self.tt("dve", R[:, 125:126], R[:, 124:125], R[:, 123:124], ALU.subtract, r=[rk], w=[rk])
                yield
                self.act(R[:, 125:126], R[:, 125:126], AF.Exp, r=[rk], w=[rk])
                yield
                self.ts("dve", R[:, 125:126], R[:, 125:126], 1.0, None, ALU.add, r=[rk], w=[rk])
                yield
                S.op("dve", lambda e: e.reciprocal(out=R[:, 126:127], in_=R[:, 125:126]), [rk], [rk])
                yield
                self.tt("dve", R[:, 126:127], R[:, 126:127], R[:, 122:123], ALU.mult, r=[rk], w=[rk])
                yield
                self.tt("dve", R[:, 127:128], R[:, 122:123], R[:, 126:127], ALU.subtract, r=[rk], w=[rk])
                self.ts("dve", R[:, 76:108], R[:, 76:108], R[:, 126:127], None, ALU.mult, r=[rk], w=[rk])
                yield
                self.stt("dve", R[:, 76:108], R[:, 44:76], R[:, 127:128], R[:, 76:108], ALU.mult, ALU.add, r=[rk], w=[rk])
                yield
                if t == 2:
                    self.dump("comb_t2", R[:, 76:108], rk, [128, 32])
                self.tr(pr[0:32, 128:256], R[:, 76:108], r=[rk, "ident"], w=[prk])
                yield
                self.cp("act", self.combT[0:32, tok], pr[0:32, 128:256], r=[prk], w=[f"combT:{t}"])
                yield

            self.pipe([(lambda i=i, t=t: tileD(i, t)) for i, t in enumerate(tiles)], ni=NI, skew=8)
            c0 = tiles[0] * 128
            self.ld("sp", d["combD"][:, c0:T], self.combT[0:32, c0:T], "combst", r=[f"combT:{t}" for t in tiles], w=["combD"])
            S.barrier()

    def phaseE(self, bi, l):
        nc, S, d = self.nc, self.S, self.d
        PS = self.PS
        last = (l == 1)
        tok0 = NCTX if last else 0
        blocks = []
        b = tok0
        while b < T:
            n = min(512, T - b)
            blocks.append((b, n))
            b += n
        ne = self.cfg.get("nexp", 32)
        with ExitStack() as ph:
            Y = self.sbt(ph, "Yacc", [128, NT, D])
            with ExitStack() as ex:
                WG = [self.sbt(ex, f"WG{i}", [128, 8, 256], BF16) for i in range(3)]
                WU = [self.sbt(ex, f"WU{i}", [128, 8, 256], BF16) for i in range(3)]
                WD = [self.sbt(ex, f"WD{i}", [128, 2, 1024], BF16) for i in range(3)]
                CB = [self.sbt(ex, f"CB{i}", [128, T], BF16) for i in range(3)]
                SA = [self.sbt(ex, f"SA{i}", [128, 512]) for i in range(2)]
                T1 = [self.sbt(ex, f"T1{i}", [128, 512], BF16) for i in range(2)]
                AT = [[self.sbt(ex, f"AT{p}{i}", [128, 512], BF16) for i in range(2)] for p in range(2)]
                ycnt = [0]
                work = []
                bcnt = 0
                for e in range(ne):
                    for (b0, n) in blocks:
                        work.append((e, e % 3, b0, n, bcnt % 2))
                        bcnt += 1

                def load_w(e):
                    s = e % 3
                    self.ld("pool", WG[s][:], d["weg"][l, e, :, :].rearrange("(kc p) f -> p kc f", p=128), f"WG{s}", w=[f"WG{s}"])
                    self.ld("pool", WU[s][:], d["weu"][l, e, :, :].rearrange("(kc p) f -> p kc f", p=128), f"WU{s}", w=[f"WU{s}"])
                    self.ld("pool", WD[s][:], d["wed"][l, e, :, :].rearrange("(fc p) n -> p fc n", p=128), f"WD{s}", w=[f"WD{s}"])
                    self.ld("sp", CB[s][:], d["combD"][e, :].partition_broadcast(128), f"CB{s}", r=["combD"], w=[f"CB{s}"])

                def gu(wi, fc):
                    e, s, b0, n, par = work[wi]
                    tiles = list(range(b0 // 128, (b0 + n) // 128))
                    hk = [f"hT:{t}" for t in tiles]
                    pa, pb = PS[fc * 2], PS[fc * 2 + 1]
                    ka, kb = f"ps{fc * 2}", f"ps{fc * 2 + 1}"
                    for kc in range(8):
                        self.mm(pa[:, 0:n], WG[s][:, kc, fc * 128:(fc + 1) * 128], self.hT[:, kc, b0:b0 + n], kc == 0, kc == 7,
                                r=[f"WG{s}"] + hk, w=[ka])
                    for kc in range(8):
                        self.mm(pb[:, 0:n], WU[s][:, kc, fc * 128:(fc + 1) * 128], self.hT[:, kc, b0:b0 + n], kc == 0, kc == 7,
                                r=[f"WU{s}"] + hk, w=[kb])
                    self.act(SA[fc][:, 0:n], pa[:, 0:n], AF.Silu, r=[ka], w=[f"SA{fc}"])
                    self.tt("dve", T1[fc][:, 0:n], pb[:, 0:n], SA[fc][:, 0:n], ALU.mult, r=[kb, f"SA{fc}"], w=[f"T1{fc}"])
                    self.tt("pool", AT[par][fc][:, 0:n], T1[fc][:, 0:n], CB[s][:, b0:b0 + n], ALU.mult, r=[f"CB{s}", f"T1{fc}"], w=[f"AT{par}{fc}"])

                def down(wi):
                    e, s, b0, n, par = work[wi]
                    tiles = list(range(b0 // 128, (b0 + n) // 128))
                    for ti, t in enumerate(tiles):
                        for half in range(2):
                            yb = 4 + ycnt[0] % 4
                            ycnt[0] += 1
                            py = PS[yb]
                            yk = f"ps{yb}"
                            for fc in range(2):
                                self.mm(py[:, 0:512], AT[par][fc][:, ti * 128:(ti + 1) * 128], WD[s][:, fc, half * 512:(half + 1) * 512],
                                        fc == 0, fc == 1, r=[f"AT{par}{fc}", f"WD{s}"], w=[yk])
                            ysl = Y[:, t, half * 512:(half + 1) * 512]
                            ykey = f"Y:{t}:{half}"
                            if e == 0:
                                self.cp("act", ysl, py[:, 0:512], r=[yk], w=[ykey])
                            else:
                                self.tt("dve", ysl, py[:, 0:512], ysl, ALU.add, r=[yk, ykey], w=[ykey])

                loaded = set()
                for e in range(min(2, ne)):
                    load_w(e); loaded.add(e)
                nw = len(work)
                gu(0, 0)
                for wi in range(nw):
                    e = work[wi][0]
                    if wi == 0 or work[wi - 1][0] != e:
                        if e + 2 < ne and (e + 2) not in loaded:
                            load_w(e + 2); loaded.add(e + 2)
                    gu(wi, 1)
                    if wi + 1 < nw:
                        gu(wi + 1, 0)
                    down(wi)
                S.barrier()
                S.emit()

            gt = [self.sbt(ph, f"gt2_{i}", [128, D]) for i in range(2)]
            lng = self.sbt(ph, "lng2_bc", [128, D])
            lnb = self.sbt(ph, "lnb2_bc", [128, D])
            NF = 3
            xt = [self.sbt(ph, f"xtF{i}", [128, D]) for i in range(NF)]
            ot = [self.sbt(ph, f"otF{i}", [128, D]) for i in range(NF)]
            self.ld("sp", gt[0][:], d["modrow"][l, bi, 5120:6144].partition_broadcast(128), "gt20", r=["modrow"], w=["gt20"])
            self.ld("sp", gt[1][:], d["modrow"][l, 2, 5120:6144].partition_broadcast(128), "gt21", r=["modrow"], w=["gt21"])
            self.ld("sp", lng[:], d["ln_g"][l, 1, :].partition_broadcast(128), "lng2", w=["lng2"])
            self.ld("sp", lnb[:], d["ln_b"][l, 1, :].partition_broadcast(128), "lnb2", w=["lnb2"])
            tiles = list(range(2 if last else 0, NT))

            def tileF(idx, t):
                g = 1 if t < 2 else 0
                s2 = idx % NF
                tok = slice(t * 128, (t + 1) * 128)
                kx, ko = f"xtF{s2}", f"otF{s2}"
                self.ld("sp", xt[s2][:], d["xs1"][bi, tok, :], kx, r=[f"xs1:{t}"], w=[kx])
                self.tt("pool", ot[s2][:], Y[:, t, :], gt[g][:], ALU.mult, r=[f"Y:{t}:0", f"Y:{t}:1", f"gt2{g}"], w=[ko])
                yield
                self.stt("dve", ot[s2][:], xt[s2][:], ALPHA, ot[s2][:], ALU.mult, ALU.add, r=[kx, ko], w=[ko])
                yield
                st = {}
                yield from self.ln_stats_g(ot[s2], ko, st)
                self.ts("dve", ot[s2][:], ot[s2][:], st["mean"], st["rstd"], ALU.subtract, ALU.mult, r=[ko, st["k"]], w=[ko])
                yield
                self.tt("pool", ot[s2][:], ot[s2][:], lng[:], ALU.mult, r=[ko, "lng2"], w=[ko])
                yield
                self.tt("dve", ot[s2][:], ot[s2][:], lnb[:], ALU.add, r=[ko, "lnb2"], w=[ko])
                yield
                if t == 2:
                    self.dump(f"x2_t2_l{l}", ot[s2][:], ko, [128, D])
                    self.dump(f"f_t2_l{l}", Y[:, t, :], f"Y:{t}:0", [128, D])
                if last:
                    self.ld("sp", d["out"][bi, (t - 2) * 128:(t - 1) * 128, :], ot[s2][:], f"ost{s2}", r=[ko], w=[f"out:{t}"])
                else:
                    self.ld("sp", d["xs2"][bi, tok, :], ot[s2][:], f"ost{s2}", r=[ko], w=[f"xs2:{t}"])
                yield

            self.pipe([(lambda i=i, t=t: tileF(i, t)) for i, t in enumerate(tiles)], ni=NF, skew=3)
            S.barrier()


def _consts():
    k = np.arange(128)[:, None]
    i = np.arange(128)[None, :]
    cm = np.zeros((10, 128, 128), np.float32)
    cm[C_ID] = np.eye(128)
    cm[C_TRIF] = (k <= i)
    cm[C_TRIB] = (k >= i)
    cm[C_NTRIF] = -cm[C_TRIF]
    cm[C_NTRIB] = -cm[C_TRIB]
    cm[C_MSF] = np.where(i < k, 0.0, NEG)
    cm[C_MSB] = np.where(i > k, 0.0, NEG)
    cm[C_MITF] = np.where(k <= i, 0.0, NEG)
    cm[C_MITB] = np.where(k >= i, 0.0, NEG)
    cm[C_ONES] = 1.0
    sel = np.zeros((32, 32, 128), np.float32)
    for e in range(32):
        sel[e, e, :] = 1.0
    return np.ascontiguousarray(cm.transpose(1, 0, 2)), sel.reshape(32, 4096)


def prep_shared(inp):
    f = lambda a: np.ascontiguousarray(np.asarray(a, dtype=np.float32))
    sh = {}
    sh["w_ada"] = f(inp["w_ada"])
    sh["b_ada"] = f(inp["b_ada"])
    sh["b_adaT"] = f(np.asarray(inp["b_ada"]).reshape(2, 48, 128).transpose(0, 2, 1))
    sh["w_in"] = f(inp["w_in"])
    sh["cqw"] = f(np.asarray(inp["conv_qkv_w"]).reshape(2, 4, 12, 128).transpose(0, 3, 2, 1))
    sh["rgcw"] = f(np.asarray(inp["rg_conv_w"]).reshape(2, 4, 4, 128).transpose(0, 3, 2, 1))
    sh["rgcb"] = f(np.asarray(inp["rg_conv_b"]).reshape(2, 4, 128).transpose(0, 2, 1))
    sh["alog"] = f(np.asarray(inp["dn_a_log"]).reshape(2, 8))
    sh["dtb"] = f(np.asarray(inp["dn_dt_bias"]).reshape(2, 8))
    sh["onw"] = f(inp["dn_onorm_w"])
    wbd = np.zeros((2, 2, 2, 4, 128, 128), np.float32)
    for gate, nm in ((0, "rg_wa"), (1, "rg_wi")):
        w = np.asarray(inp[nm])
        for ch in range(4):
            for sub in range(2):
                wbd[:, :, gate, ch, sub * 64:(sub + 1) * 64, sub * 64:(sub + 1) * 64] = w[:, :, ch * 2 + sub]
    sh["wbd"] = f(wbd.reshape(2, 16, 128, 128).transpose(0, 2, 1, 3))
    rgb = np.zeros((2, 2, 2, 4, 128), np.float32)
    rgb[:, :, 0] = np.asarray(inp["rg_ba"]).reshape(2, 2, 4, 128)
    rgb[:, :, 1] = np.asarray(inp["rg_bi"]).reshape(2, 2, 4, 128)
    sh["rgb"] = f(rgb.reshape(2, 16, 128).transpose(0, 2, 1))
    sh["lam"] = f(np.asarray(inp["rg_lambda"]).reshape(2, 8, 128).transpose(0, 2, 1))
    sh["w_out"] = f(inp["w_out"])
    sh["ln_g"] = f(inp["ln_g"])
    sh["ln_b"] = f(inp["ln_b"])
    sh["wr"] = f(np.concatenate([np.asarray(inp["router_wg"]), np.asarray(inp["router_we"])], axis=-1))
    sh["br"] = f(np.concatenate([np.asarray(inp["router_bg"]), np.asarray(inp["router_be"])], axis=-1))
    sh["weg"] = f(inp["w_e_gate"])
    sh["weu"] = f(inp["w_e_up"])
    sh["wed"] = f(inp["w_e_down"])
    cm, sel = _consts()
    sh["cmat"] = cm
    return sh


def prep_core(inp, core):
    x = np.asarray(inp["x"], dtype=np.float32)
    ctx = np.asarray(inp["ctx"], dtype=np.float32)
    c = np.asarray(inp["c"], dtype=np.float32)
    cc = np.asarray(inp["c_ctx"], dtype=np.float32)
    b0 = 2 * core
    xin = np.concatenate([ctx[b0:b0 + 2], x[b0:b0 + 2]], axis=1)
    cols = np.stack([c[b0], c[b0 + 1], cc], axis=-1)
    cT = cols.reshape(8, 128, 3).transpose(1, 0, 2)
    return {"xin": np.ascontiguousarray(xin), "cT": np.ascontiguousarray(cT)}


def build_nc(cfg=None):
    nc = bass.Bass("TRN2", target_bir_lowering=False)
    kb = KB(nc, cfg or {})
    kb.build()
    return nc, kb


def kernel(**inputs):
    nc, kb = build_nc({})
    sh = prep_shared(inputs)
    in_maps = []
    for core in range(8):
        m = dict(sh)
        m.update(prep_core(inputs, core))
        in_maps.append(m)
    res = run_bass_kernel_spmd(nc, in_maps, core_ids=list(range(8)))
    outs = [np.asarray(r["out"]) for r in res.results]
    return np.concatenate(outs, axis=0).astype(np.float32)
```

```python
import numpy as np
from contextlib import ExitStack
import concourse.bass as bass
import concourse.mybir as mybir
from concourse.bass_utils import run_bass_kernel_spmd

F32 = mybir.dt.float32
BF16 = mybir.dt.bfloat16
AF = mybir.ActivationFunctionType
ALU = mybir.AluOpType

T = 2304
NT = 18
D = 1024
DIN = 3088
NCTX = 256
ALPHA = float((2 * 2) ** 0.25)
LN_EPS = 1e-5
NORM_EPS = 1e-6
NEG = -30000.0
ENGS = ["pe", "act", "dve", "pool", "sp"]
(C_ID, C_TRIF, C_TRIB, C_NTRIF, C_NTRIB, C_MSF, C_MSB, C_MITF, C_MITB, C_ONES) = range(10)


class Sched:
    def __init__(self, nc, stack):
        self.nc = nc
        self.stack = stack
        self.q = {e: [] for e in ENGS}
        self.sems = {}
        self.semval = {}
        self.waited = {}
        self.last_w = {}
        self.readers = {}
        self.ninstr = 0
        for e in ENGS:
            if e != "sp":
                self._sem("eng_" + e)

    def _sem(self, name):
        if name not in self.sems:
            self.sems[name] = self.stack.enter_context(self.nc.semaphore(name))
            self.semval[name] = 0
        return self.sems[name]

    @staticmethod
    def _norm(reads, writes):
        r2 = []
        w2 = []
        for k in reads:
            if k.startswith("ps"):
                w2.append(k.split(":")[0])
            else:
                r2.append(k)
        for k in writes:
            w2.append(k.split(":")[0] if k.startswith("ps") else k)
        return r2, w2

    def _deps(self, eng, reads, writes):
        toks = []
        for k in reads:
            t = self.last_w.get(k)
            if t is not None:
                toks.append(t)
        for k in writes:
            t = self.last_w.get(k)
            if t is not None:
                toks.append(t)
            toks.extend(self.readers.get(k, ()))
        best = {}
        for (s, v) in toks:
            if eng == "pe" and s == "eng_pe":
                continue
            if self.waited.get((eng, s), 0) < v and best.get(s, 0) < v:
                best[s] = v
        for s, v in best.items():
            self.waited[(eng, s)] = v
        return list(best.items())

    def _commit(self, tok, reads, writes):
        for k in reads:
            self.readers.setdefault(k, []).append(tok)
        for k in writes:
            self.last_w[k] = tok
            self.readers[k] = []

    def op(self, eng, fn, reads=(), writes=()):
        reads, writes = self._norm(reads, writes)
        waits = self._deps(eng, reads, writes)
        s = "eng_" + eng
        self.semval[s] += 1
        tok = (s, self.semval[s])
        self.q[eng].append((waits, fn, (s, 1)))
        self._commit(tok, reads, writes)
        self.ninstr += 1
        return tok

    def dma(self, eng, fn, slot, reads=(), writes=()):
        s = "dma_" + slot
        self._sem(s)
        reads, writes = self._norm(reads, writes)
        waits = self._deps(eng, reads, writes)
        self.semval[s] += 16
        tok = (s, self.semval[s])
        self.q[eng].append((waits, fn, (s, 16)))
        self._commit(tok, reads, writes)
        self.ninstr += 1
        return tok

    def barrier(self):
        allt = [(s, v) for s, v in self.semval.items() if v > 0]
        for e in ENGS:
            waits = []
            for (s, v) in allt:
                if self.waited.get((e, s), 0) < v:
                    self.waited[(e, s)] = v
                    waits.append((s, v))
            if waits:
                self.q[e].append((waits, None, None))

    def emit(self):
        self.barrier()
        nc = self.nc
        sems = self.sems
        q = self.q
        self.q = {e: [] for e in ENGS}

        def run(engobj, lst):
            for waits, fn, inc in lst:
                for (s, v) in waits:
                    engobj.wait_ge(sems[s], v)
                if fn is not None:
                    ins = fn(engobj)
                    ins.then_inc(sems[inc[0]], inc[1])

        with nc.Block() as block:
            @block.sync
            def _(e):
                run(e, q["sp"])

            @block.tensor
            def _(e):
                run(e, q["pe"])

            @block.scalar
            def _(e):
                run(e, q["act"])

            @block.vector
            def _(e):
                run(e, q["dve"])

            @block.gpsimd
            def _(e):
                run(e, q["pool"])


def rr(gens):
    gens = list(gens)
    while gens:
        nxt = []
        for g in gens:
            try:
                next(g)
                nxt.append(g)
            except StopIteration:
                pass
        gens = nxt


class KB:
    def __init__(self, nc, cfg):
        self.nc = nc
        self.cfg = cfg
        self.dbg = set(cfg.get("dbg", ()))
        self.d = {}
        self.dbg_out = {}

    def sbt(self, st, name, shape, dt=F32):
        self.uid = getattr(self, "uid", 0) + 1
        return st.enter_context(self.nc.sbuf_tensor(f"{name}_u{self.uid}", shape, dt))

    def mm(self, out, lhsT, rhs, start=True, stop=True, r=(), w=()):
        return self.S.op("pe", lambda e: e.matmul(out, lhsT=lhsT, rhs=rhs, start=start, stop=stop), r, w)

    def tr(self, out, in_, r=(), w=()):
        ident = self.cm[C_ID]
        return self.S.op("pe", lambda e: e.transpose(out, in_, ident), list(r), w)

    def act(self, out, in_, func, r=(), w=(), **kw):
        return self.S.op("act", lambda e: e.activation(out=out, in_=in_, func=func, **kw), r, w)

    def ts(self, eng, out, in0, s1, s2, op0, op1=None, r=(), w=()):
        if op1 is None:
            return self.S.op(eng, lambda e: e.tensor_scalar(out=out, in0=in0, scalar1=s1, scalar2=None, op0=op0), r, w)
        return self.S.op(eng, lambda e: e.tensor_scalar(out=out, in0=in0, scalar1=s1, scalar2=s2, op0=op0, op1=op1), r, w)

    def tt(self, eng, out, in0, in1, op, r=(), w=()):
        return self.S.op(eng, lambda e: e.tensor_tensor(out=out, in0=in0, in1=in1, op=op), r, w)

    def stt(self, eng, out, in0, scalar, in1, op0, op1, r=(), w=()):
        return self.S.op(eng, lambda e: e.scalar_tensor_tensor(out=out, in0=in0, scalar=scalar, in1=in1, op0=op0, op1=op1), r, w)

    def cp(self, eng, out, in_, r=(), w=()):
        if eng == "act":
            return self.S.op("act", lambda e: e.activation(out=out, in_=in_, func=AF.Copy), r, w)
        return self.S.op(eng, lambda e: e.tensor_copy(out=out, in_=in_), r, w)

    def memset(self, eng, ap, val, w=()):
        return self.S.op(eng, lambda e: e.memset(ap, val), (), w)

    def ld(self, q, out, in_, slot, r=(), w=()):
        return self.S.dma(q, lambda e: e.dma_start(out=out, in_=in_), slot, r, w)

    def dump(self, name, ap, key, shape, dt=F32):
        if name not in self.dbg:
            return
        o = self.nc.dram_tensor("d_" + name, shape, dt, kind="ExternalOutput").ap()
        self.dbg_out[name] = o
        self.S.dma("sp", lambda e: e.dma_start(out=o, in_=ap), "dbg_" + name, [key], ["dbgdram_" + name])

    @staticmethod
    def pipe(makers, ni=2, skew=4):
        makers = list(makers)
        active = []
        nxt = 0
        since = skew
        while nxt < len(makers) or active:
            if nxt < len(makers) and len(active) < ni and (since >= skew or not active):
                active.append(makers[nxt]())
                nxt += 1
                since = 0
            for g in list(active):
                try:
                    next(g)
                except StopIteration:
                    active.remove(g)
            since += 1

    def ln_stats_g(self, x_ap, xkey, out):
        i = self.stat_i % 8
        self.stat_i += 1
        st6 = self.stat6[:, i, :, :]
        mv = self.statmv[:, i, :]
        k6 = f"st6_{i}"
        kmv = f"stmv_{i}"
        for g in range(2):
            self.S.op("dve", lambda e, g=g: e.bn_stats(out=st6[:, g, :], in_=x_ap[:, g * 512:(g + 1) * 512]), [xkey], [k6])
        self.S.op("dve", lambda e: e.bn_aggr(out=mv[:, 0:2], in_=st6.rearrange("p a b -> p (a b)")), [k6], [kmv])
        yield
        self.act(mv[:, 2:3], mv[:, 1:2], AF.Ln, [kmv, "epsc"], [kmv], bias=self.epsc[:, 0:1], scale=1.0)
        yield
        self.act(mv[:, 3:4], mv[:, 2:3], AF.Exp, [kmv], [kmv], scale=-0.5)
        out["mean"], out["rstd"], out["k"] = mv[:, 0:1], mv[:, 3:4], kmv

    def ln_stats(self, x_ap, xkey, tag, eps=LN_EPS):
        i = self.stat_i % 8
        self.stat_i += 1
        st6 = self.stat6[:, i, :, :]
        mv = self.statmv[:, i, :]
        k6 = f"st6_{i}"
        kmv = f"stmv_{i}"
        for g in range(2):
            self.S.op("dve", lambda e, g=g: e.bn_stats(out=st6[:, g, :], in_=x_ap[:, g * 512:(g + 1) * 512]), [xkey], [k6])
        self.S.op("dve", lambda e: e.bn_aggr(out=mv[:, 0:2], in_=st6.rearrange("p a b -> p (a b)")), [k6], [kmv])
        self.act(mv[:, 2:3], mv[:, 1:2], AF.Sqrt, [kmv], [kmv], bias=self.epsc[:, 0:1] if eps == LN_EPS else self.epsc[:, 1:2], scale=1.0)
        self.S.op("dve", lambda e: e.reciprocal(out=mv[:, 3:4], in_=mv[:, 2:3]), [kmv], [kmv])
        return mv[:, 0:1], mv[:, 3:4], kmv

    def conv(self, eng, out_t, in_t, w4, bias, rk, wk, wv=None):
        wv = wv or (lambda a: a)
        if bias is not None:
            self.ts(eng, wv(out_t[:, :]), in_t[:, :], w4[:, 2:3], bias, ALU.mult, ALU.add, r=rk, w=[wk])
        else:
            self.ts(eng, wv(out_t[:, :]), in_t[:, :], w4[:, 2:3], None, ALU.mult, r=rk, w=[wk])
        o3 = out_t[:, NCTX:].rearrange("p (r c) -> p r c", c=64)
        i3 = in_t[:, NCTX:].rearrange("p (r c) -> p r c", c=64)
        for (j, o) in ((0, -2), (1, -1), (3, 1)):
            lo = max(0, -o)
            hi = NCTX - max(0, o)
            self.stt(eng, wv(out_t[:, lo:hi]), in_t[:, lo + o:hi + o], w4[:, j:j + 1], out_t[:, lo:hi], ALU.mult, ALU.add,
                     r=list(rk) + [wk], w=[wk])
            hi = 64 - max(0, o)
            self.stt(eng, wv(o3[:, :, lo:hi]), i3[:, :, lo + o:hi + o], w4[:, j:j + 1], o3[:, :, lo:hi], ALU.mult, ALU.add,
                     r=list(rk) + [wk], w=[wk])

    def conv_g(self, eng, out_t, in_t, w4, bias, rk, wk, wv=None):
        wv = wv or (lambda a: a)
        if bias is not None:
            self.ts(eng, wv(out_t[:, :]), in_t[:, :], w4[:, 2:3], bias, ALU.mult, ALU.add, r=rk, w=[wk])
        else:
            self.ts(eng, wv(out_t[:, :]), in_t[:, :], w4[:, 2:3], None, ALU.mult, r=rk, w=[wk])
        yield
        o3 = out_t[:, NCTX:].rearrange("p (r c) -> p r c", c=64)
        i3 = in_t[:, NCTX:].rearrange("p (r c) -> p r c", c=64)
        for (j, o) in ((0, -2), (1, -1), (3, 1)):
            lo = max(0, -o)
            hi = NCTX - max(0, o)
            self.stt(eng, wv(out_t[:, lo:hi]), in_t[:, lo + o:hi + o], w4[:, j:j + 1], out_t[:, lo:hi], ALU.mult, ALU.add,
                     r=list(rk) + [wk], w=[wk])
            hi = 64 - max(0, o)
            self.stt(eng, wv(o3[:, :, lo:hi]), i3[:, :, lo + o:hi + o], w4[:, j:j + 1], o3[:, :, lo:hi], ALU.mult, ALU.add,
                     r=list(rk) + [wk], w=[wk])
            yield

    def proj_fm_g(self, dst, dkey, wt, wkey, c0, wv=None):
        wv_ = wv or (lambda a: a)
        for blk in range(5):
            b0 = blk * 512
            n = min(512, T - b0)
            pb = self.PS[self.ps_rot % 4]
            pk = f"ps{self.ps_rot % 4}"
            self.ps_rot += 1
            for kc in range(8):
                self.mm(pb[:, 0:n], wt[:, kc, c0:c0 + 128], self.hT[:, kc, b0:b0 + n], kc == 0, kc == 7,
                        r=[wkey] + [f"hT:{t}" for t in range(b0 // 128, (b0 + n) // 128)], w=[pk])
            self.cp("act", wv_(dst[:, b0:b0 + n]), pb[:, 0:n], r=[pk], w=[dkey])
            yield

    def proj_fm(self, dst, dkey, wt, wkey, c0, r_extra=(), evac="act", func=None, wv=None):
        for blk in range(5):
            b0 = blk * 512
            n = min(512, T - b0)
            pb = self.PS[self.ps_rot % 4]
            pk = f"ps{self.ps_rot % 4}"
            self.ps_rot += 1
            for kc in range(8):
                self.mm(pb[:, 0:n], wt[:, kc, c0:c0 + 128], self.hT[:, kc, b0:b0 + n], kc == 0, kc == 7,
                        r=[wkey] + [f"hT:{t}" for t in range(b0 // 128, (b0 + n) // 128)] + list(r_extra), w=[pk])
            wv_ = wv or (lambda a: a)
            if func is None:
                self.cp("act", wv_(dst[:, b0:b0 + n]), pb[:, 0:n], r=[pk], w=[dkey])
            else:
                self.act(wv_(dst[:, b0:b0 + n]), pb[:, 0:n], func, r=[pk], w=[dkey])

    def build(self):
        nc = self.nc
        cfg = self.cfg
        d = self.d

        def din(name, shape):
            d[name] = nc.dram_tensor(name, shape, F32, kind="ExternalInput").ap()

        din("xin", [2, T, D]); din("cT", [128, 8, 3]); din("w_ada", [2, 1024, 6144]); din("b_ada", [2, 6144])
        din("b_adaT", [2, 128, 48]); din("w_in", [2, 1024, DIN]); din("cqw", [2, 128, 12, 4]); din("rgcw", [2, 128, 4, 4])
        din("rgcb", [2, 128, 4]); din("alog", [2, 8]); din("dtb", [2, 8]); din("onw", [2, 128])
        din("wbd", [2, 128, 16, 128]); din("rgb", [2, 128, 16]); din("lam", [2, 128, 8])
        din("w_out", [2, 1024, 1024]); din("ln_g", [2, 2, 1024]); din("ln_b", [2, 2, 1024]); din("wr", [2, 1024, 36])
        din("br", [2, 36]); din("weg", [2, 32, 1024, 256]); din("weu", [2, 32, 1024, 256]); din("wed", [2, 32, 256, 1024])
        din("cmat", [128, 10, 128])
        d["out"] = nc.dram_tensor("out", [2, 2048, D], F32, kind="ExternalOutput").ap()
        d["xs1"] = nc.dram_tensor("xs1", [2, T, D], F32, kind="Internal").ap()
        d["xs2"] = nc.dram_tensor("xs2", [2, T, D], F32, kind="Internal").ap()
        d["modrow"] = nc.dram_tensor("modrow", [2, 3, 6144], F32, kind="Internal").ap()
        d["combD"] = nc.dram_tensor("combD", [32, T], BF16, kind="Internal").ap()

        with ExitStack() as outer:
            self.S = Sched(nc, outer)
            S = self.S
            self.cmat = self.sbt(outer, "cmat_sb", [128, 10, 128])
            self.cm = [self.cmat[:, i, :] for i in range(10)]
            self.modT = self.sbt(outer, "modT", [128, 2, 48, 3])
            self.onep = self.sbt(outer, "onep", [128, 2, 48, 3])
            self.stat6 = self.sbt(outer, "stat6", [128, 8, 2, 6])
            self.statmv = self.sbt(outer, "statmv", [128, 8, 4])
            self.epsc = self.sbt(outer, "epsc", [128, 2])
            self.stat_i = 0
            self.ps_rot = 0
            self.PS = [outer.enter_context(nc.psum_tensor(f"psb{i}", [128, 512], F32)) for i in range(8)]
            self.ld("sp", self.cmat[:], d["cmat"][:, :, :], "cmat", w=["ident", "cmat"])
            self.cmatr = self.sbt(outer, "cmatr_sb", [128, 10, 128])
            self.S.op("dve", lambda e: e.tensor_copy(out=self.cmatr[:].bitcast(mybir.dt.float32r), in_=self.cmat[:]), ["cmat"], ["cmatr"])
            self.cmr = [self.cmatr[:, i, :].bitcast(mybir.dt.float32r) for i in range(10)]
            self.memset("pool", self.epsc[:, 0:1], LN_EPS, w=["epsc"])
            self.memset("pool", self.epsc[:, 1:2], NORM_EPS, w=["epsc"])
            self.stage0()
            S.emit()
            if cfg.get("stop") == "stage0":
                return
            for bi in range(cfg.get("nb", 2)):
                for l in range(cfg.get("nl", 2)):
                    self.layer(bi, l)
            S.emit()

    def stage0(self):
        nc, S, d = self.nc, self.S, self.d
        with ExitStack() as ph:
            cT = self.sbt(ph, "cT_sb", [128, 8, 3])
            scT = self.sbt(ph, "scT", [128, 8, 3])
            brow = self.sbt(ph, "brow", [3, 6144])
            bT = self.sbt(ph, "bT", [128, 48])
            wa = [self.sbt(ph, f"wa{i}", [128, 8, 512]) for i in range(2)]
            rowsb = [self.sbt(ph, f"rowsb{i}", [3, 512]) for i in range(2)]
            self.ld("sp", cT[:], d["cT"][:, :, :], "cT", w=["cT"])
            self.act(scT[:], cT[:], AF.Silu, r=["cT"], w=["scT"])
            for l in range(2):
                self.ld("sp", brow[:], d["b_ada"][l, :].partition_broadcast(3), "brow", w=["brow"])
                self.ld("sp", bT[:], d["b_adaT"][l, :, :], "bT", w=["bT"])
                psM = self.PS[l]
                for piece in range(12):
                    s = piece % 2
                    self.ld("sp", wa[s][:], d["w_ada"][l, :, piece * 512:(piece + 1) * 512].rearrange("(kc p) n -> p kc n", p=128),
                            f"wa{s}", w=[f"wa{s}"])
                    psR = self.PS[2 + s]
                    for kc in range(8):
                        self.mm(psR[0:3, 0:512], scT[:, kc, :], wa[s][:, kc, :], kc == 0, kc == 7, r=[f"wa{s}", "scT"], w=[f"psR{s}"])
                    self.tt("dve", rowsb[s][:], psR[0:3, 0:512], brow[:, piece * 512:(piece + 1) * 512], ALU.add,
                            r=[f"psR{s}", "brow"], w=[f"rowsb{s}"])
                    self.ld("sp", d["modrow"][l, :, piece * 512:(piece + 1) * 512], rowsb[s][:], f"rowst{s}", r=[f"rowsb{s}"], w=["modrow"])
                    for fc in range(4):
                        ch = piece * 4 + fc
                        self.S.op("pe", lambda e, ch=ch, fc=fc, s=s, psM=psM: e.transpose(psM[:, ch * 3:(ch + 1) * 3], rowsb[s][0:3, fc * 128:(fc + 1) * 128],
                                                                              self.cm[C_ID][0:3, 0:3]),
                                  [f"rowsb{s}", "ident"], [f"psM{l}"])
                self.cp("dve", self.modT[:, l, :, :], psM[:, 0:144].rearrange("p (c j) -> p c j", j=3), r=[f"psM{l}"], w=["modT"])
                self.ts("dve", self.onep[:, l, :, :], self.modT[:, l, :, :], 1.0, None, ALU.add, r=["modT"], w=["onep"])
            self.dump("modT", self.modT[:], "modT", [128, 2, 48, 3])
            S.barrier()

    def layer(self, bi, l):
        nc, S, d, cfg = self.nc, self.S, self.d, self.cfg
        last = (l == 1)
        src = d["xin"] if l == 0 else d["xs2"]
        stop = cfg.get("stop")
        with ExitStack() as bl:
            self.hT = self.sbt(bl, "hT", [128, 8, T], BF16)
            self.combT = self.sbt(bl, "combT", [32, T], BF16)
            with ExitStack() as ml:
                self.yT = self.sbt(ml, "yT", [128, 8, T], BF16)
                self.sm = {nm: self.sbt(ml, "sm_" + nm, [128, NT, 8]) for nm in ("BETA", "NBETA", "EGC", "NEGC", "EDEC", "GLB", "G")}
                self.cqw = self.sbt(ml, "cqw_sb", [128, 12, 4])
                self.rgcw = self.sbt(ml, "rgcw_sb", [128, 4, 4])
                self.rgcb = self.sbt(ml, "rgcb_sb", [128, 4])
                self.dtb = self.sbt(ml, "dtb_bc", [128, 8])
                self.nA = self.sbt(ml, "nA_bc", [128, 8])
                self.onw = self.sbt(ml, "onw_bc", [128, 128])
                self.rgb = self.sbt(ml, "rgb_sb", [128, 16])
                self.c1 = self.sbt(ml, "c1_sb", [128, 8])
                self.wbd = self.sbt(ml, "wbd_bf", [128, 16, 128], BF16)
                self.wsm = self.sbt(ml, "wsm_bf", [128, 8, 16], BF16)
                self.phaseA(bi, l, src)
                S.emit()
                self.dump_seq("hT", self.hT, BF16)
                if stop == "A":
                    self.dump_sm(); S.emit(); return
                self.phaseB(bi, l)
                S.emit()
                if stop == "B":
                    self.dump_seq("yT", self.yT, BF16); S.emit(); return
                for h in cfg.get("heads", range(4)):
                    self.phaseC(bi, l, h)
                    S.emit()
                if stop == "C":
                    self.dump_seq("yT", self.yT, BF16); S.emit(); return
                self.phaseD(bi, l, src)
                S.emit()
            if stop == "D":
                self.dump_seq("hT", self.hT, BF16)
                if "combT" in self.dbg:
                    self.dump("combT", self.combT[:], "combT", [32, T], BF16)
                S.emit(); return
            self.phaseE(bi, l)
            S.emit()

    def dump_seq(self, name, tile, dt):
        if name in self.dbg:
            o = self.nc.dram_tensor("d_" + name, [128, 8, T], dt, kind="ExternalOutput").ap()
            self.dbg_out[name] = o
            self.dbg.discard(name)
            self.S.dma("sp", lambda e: e.dma_start(out=o, in_=tile[:]), "dbg_" + name, [], ["dbgdram_" + name])
            self.S.barrier()

    def dump_sm(self):
        for nm, t in self.sm.items():
            self.dump("sm_" + nm, t[:], "sm", [128, NT, 8])

    def phaseA(self, bi, l, src):
        nc, S, d = self.nc, self.S, self.d
        sm = self.sm
        with ExitStack() as ph:
            xt = [self.sbt(ph, f"xt{i}", [128, D]) for i in range(4)]
            xn = [self.sbt(ph, f"xn{i}", [128, D]) for i in range(4)]
            tmp = self.sbt(ph, "smtmp", [128, 4, 5, 8])
            alog = self.sbt(ph, "alog_bc", [128, 8])
            lam = self.sbt(ph, "lam_sb", [128, 8])
            self.ld("sp", self.cqw[:], d["cqw"][l, :, :, :], "cqw", w=["cqw"])
            self.ld("sp", self.rgcw[:], d["rgcw"][l, :, :, :], "rgcw", w=["rgcw"])
            self.ld("sp", self.rgcb[:], d["rgcb"][l, :, :], "rgcb", w=["rgcb"])
            self.ld("sp", self.dtb[:], d["dtb"][l, :].partition_broadcast(128), "dtb", w=["dtb"])
            self.ld("sp", alog[:], d["alog"][l, :].partition_broadcast(128), "alog", w=["alog"])
            self.ld("sp", self.onw[:], d["onw"][l, :].partition_broadcast(128), "onw", w=["onw"])
            self.ld("sp", self.rgb[:], d["rgb"][l, :, :], "rgb", w=["rgb"])
            self.ld("sp", lam[:], d["lam"][l, :, :], "lam", w=["lam"])
            self.ld("pool", self.wbd[:], d["wbd"][l, :, :, :], "wbd", w=["wbd"])
            self.ld("pool", self.wsm[:], d["w_in"][l, :, 2048:2064].rearrange("(kc p) n -> p kc n", p=128), "wsm", w=["wsm"])
            self.act(self.nA[:], alog[:], AF.Exp, r=["alog"], w=["nA"])
            self.ts("dve", self.nA[:], self.nA[:], -1.0, None, ALU.mult, r=["nA"], w=["nA"])
            self.act(self.c1[:], lam[:], AF.Exp, r=["lam"], w=["c1"], scale=-1.0)
            self.act(self.c1[:], self.c1[:], AF.Ln, r=["c1"], w=["c1"], bias=1.0)
            self.ts("dve", self.c1[:], self.c1[:], -8.0, None, ALU.mult, r=["c1"], w=["c1"])
            def tileA(t):
                j = 2 if t < 2 else bi
                s3 = t % 4
                s2 = t % 4
                self.ld("sp", xt[s3][:], src[bi, t * 128:(t + 1) * 128, :], f"xt{s3}", w=[f"xt{s3}"])
                st = {}
                yield from self.ln_stats_g(xt[s3], f"xt{s3}", st)
                self.ts("dve", xn[s2][:], xt[s3][:], st["mean"], st["rstd"], ALU.subtract, ALU.mult, r=[f"xt{s3}", st["k"]], w=[f"xn{s2}"])
                yield
                for kc in range(8):
                    pb = self.PS[s2 * 2 + kc // 4]
                    pk = f"ps{s2 * 2 + kc // 4}:{kc % 4}"
                    self.tr(pb[:, (kc % 4) * 128:(kc % 4 + 1) * 128], xn[s2][:, kc * 128:(kc + 1) * 128], r=[f"xn{s2}", "ident"], w=[pk])
                yield
                for kc in range(8):
                    pb = self.PS[s2 * 2 + kc // 4]
                    pk = f"ps{s2 * 2 + kc // 4}:{kc % 4}"
                    self.act(self.hT[:, kc, t * 128:(t + 1) * 128], pb[:, (kc % 4) * 128:(kc % 4 + 1) * 128], AF.Identity,
                             r=[pk, "onep", "modT"], w=[f"hT:{t}"], scale=self.onep[:, l, 8 + kc, j:j + 1], bias=self.modT[:, l, kc, j:j + 1])
                yield
                p16 = self.PS[s2 * 2]
                k16 = f"ps{s2 * 2}"
                for kc in range(8):
                    self.mm(p16[:, 0:16], self.hT[:, kc, t * 128:(t + 1) * 128], self.wsm[:, kc, :], kc == 0, kc == 7,
                            r=[f"hT:{t}", "wsm"], w=[k16])
                yield
                smk = f"sm:{t}"
                tk = f"smtmp{s2}"
                self.act(tmp[:, s2, 4, :], p16[:, 0:8], AF.Exp, r=[k16], w=[tk + "b"], scale=-1.0)
                self.tt("dve", tmp[:, s2, 0, :], p16[:, 8:16], self.dtb[:], ALU.add, r=[k16, "dtb"], w=[tk])
                yield
                self.ts("dve", tmp[:, s2, 4, :], tmp[:, s2, 4, :], 1.0, None, ALU.add, r=[tk + "b"], w=[tk + "b"])
                self.act(tmp[:, s2, 1, :], tmp[:, s2, 0, :], AF.Exp, r=[tk], w=[tk])
                yield
                S.op("dve", lambda e: e.reciprocal(out=sm["BETA"][:, t, :], in_=tmp[:, s2, 4, :]), [tk + "b"], [smk])
                yield
                self.ts("dve", sm["NBETA"][:, t, :], sm["BETA"][:, t, :], -1.0, None, ALU.mult, r=[smk], w=[smk])
                yield
                self.act(tmp[:, s2, 2, :], tmp[:, s2, 1, :], AF.Ln, r=[tk], w=[tk], bias=1.0)
                yield
                self.tt("dve", sm["G"][:, t, :], tmp[:, s2, 2, :], self.nA[:], ALU.mult, r=[tk, "nA"], w=[smk])
                yield
                self.mm(p16[:, 16:20], self.cm[C_TRIF], sm["G"][:, t, 0:4], r=[smk, "cmat"], w=[k16])
                self.mm(p16[:, 20:24], self.cm[C_TRIB], sm["G"][:, t, 4:8], r=[smk, "cmat"], w=[k16])
                self.mm(p16[:, 32:40], self.cm[C_ONES], sm["G"][:, t, :], r=[smk, "cmat"], w=[k16])
                yield
                self.act(sm["EGC"][:, t, :], p16[:, 16:24], AF.Exp, r=[k16], w=[smk])
                self.act(sm["GLB"][:, t, :], p16[:, 32:40], AF.Exp, r=[k16], w=[smk])
                self.cp("act", tmp[:, s2, 3, :], p16[:, 16:24], r=[k16], w=[tk])
                yield
                self.ts("dve", sm["NEGC"][:, t, :], sm["EGC"][:, t, :], -1.0, None, ALU.mult, r=[smk], w=[smk])
                self.tt("dve", tmp[:, s2, 3, :], p16[:, 32:40], tmp[:, s2, 3, :], ALU.subtract, r=[k16, tk], w=[tk])
                yield
                self.act(sm["EDEC"][:, t, :], tmp[:, s2, 3, :], AF.Exp, r=[tk], w=[smk])
                yield

            self.pipe([(lambda t=t: tileA(t)) for t in range(NT)], ni=4, skew=4)
            S.barrier()

    def phaseB(self, bi, l):
        nc, S, d = self.nc, self.S, self.d
        with ExitStack() as ph:
            wrg = [self.sbt(ph, f"wrg{i}", [128, 8, 256], BF16) for i in range(2)]
            RAW = self.sbt(ph, "RAW", [128, T])
            XC = self.sbt(ph, "XC", [128, T])
            XCB = self.sbt(ph, "XCB", [128, T], BF16)
            Rt = self.sbt(ph, "Rt", [128, T])
            It = self.sbt(ph, "It", [128, T])
            A2 = self.sbt(ph, "A2", [128, T])
            H = [self.sbt(ph, f"H{i}", [128, T]) for i in range(2)]
            for ch in range(4):
                s = ch % 2
                wk = f"wrg{s}"
                for part, c0 in ((0, 2064 + ch * 128), (1, 2576 + ch * 128)):
                    self.ld("pool", wrg[s][:, :, part * 128:(part + 1) * 128],
                            d["w_in"][l, :, c0:c0 + 128].rearrange("(kc p) n -> p kc n", p=128), f"wrg{s}_{part}", w=[f"{wk}:{part}"])
                self.proj_fm(RAW, "RAW", wrg[s], f"{wk}:0", 0)
                self.conv("dve", XC, RAW, self.rgcw[:, ch, :], self.rgcb[:, ch:ch + 1], ["RAW", "rgcw", "rgcb"], "XC")
                self.cp("act", XCB[:], XC[:], r=["XC"], w=["XCB"])
                if ch == 0:
                    self.dump("xc0", XC[:], "XC", [128, T])
                for dr in range(2):
                    for gate, dst, dk in ((0, Rt, "Rt"), (1, It, "It")):
                        idx = (dr * 2 + gate) * 4 + ch
                        for blk in range(5):
                            b0 = blk * 512
                            n = min(512, T - b0)
                            pb = self.PS[self.ps_rot % 4]
                            pk = f"ps{self.ps_rot % 4}"
                            self.ps_rot += 1
                            self.mm(pb[:, 0:n], self.wbd[:, idx, :], XCB[:, b0:b0 + n], r=["wbd", "XCB"], w=[pk])
                            self.act(dst[:, b0:b0 + n], pb[:, 0:n], AF.Sigmoid, r=[pk, "rgb"], w=[dk], bias=self.rgb[:, idx:idx + 1])
                    self.act(Rt[:], Rt[:], AF.Exp, r=["Rt", "c1"], w=["Rt"], scale=self.c1[:, dr * 4 + ch:dr * 4 + ch + 1])
                    self.act(A2[:], Rt[:], AF.Square, r=["Rt"], w=["A2"])
                    self.act(A2[:], A2[:], AF.Sqrt, r=["A2"], w=["A2"], scale=-1.0, bias=1.0)
                    self.tt("dve", It[:], It[:], XC[:], ALU.mult, r=["It", "XC"], w=["It"])
                    self.tt("dve", It[:], It[:], A2[:], ALU.mult, r=["It", "A2"], w=["It"])
                    hk = f"H{dr}"
                    if dr == 0:
                        S.op("dve", lambda e: e.tensor_tensor_scan(out=H[0][:, :], data0=Rt[:, :], data1=It[:, :], initial=0.0,
                                                                   op0=ALU.mult, op1=ALU.add), ["Rt", "It"], [hk])
                    else:
                        S.op("dve", lambda e: e.tensor_tensor_scan(out=H[1][:, 0:NCTX][:, ::-1], data0=Rt[:, 0:NCTX][:, ::-1],
                                                                   data1=It[:, 0:NCTX][:, ::-1], initial=0.0,
                                                                   op0=ALU.mult, op1=ALU.add), ["Rt", "It"], [hk])
                        S.op("dve", lambda e: e.tensor_tensor_scan(out=H[1][:, NCTX:T][:, ::-1], data0=Rt[:, NCTX:T][:, ::-1],
                                                                   data1=It[:, NCTX:T][:, ::-1], initial=H[1][:, 0:1],
                                                                   op0=ALU.mult, op1=ALU.add), ["Rt", "It", hk], [hk])
                self.tt("dve", H[0][:], H[0][:], H[1][:], ALU.add, r=["H0", "H1"], w=["H0"])
                if ch == 0:
                    self.dump("hr0", H[0][:], "H0", [128, T])
                self.proj_fm(RAW, "RAW", wrg[s], f"{wk}:1", 128, func=AF.Gelu_apprx_tanh)
                self.tt("dve", self.yT[:, 4 + ch, :], RAW[:], H[0][:], ALU.mult, r=["RAW", "H0"], w=[f"yT:{4 + ch}"])
            S.barrier()

    def phaseC(self, bi, l, h):
        nc, S, d = self.nc, self.S, self.d
        sm = self.sm
        PS = self.PS
        cm = self.cm
        cmr = self.cmr
        r_ = lambda ap: ap
        cmr = cm
        F32R = mybir.dt.float32r
        rr_ = lambda ap: ap.bitcast(F32R)
        with ExitStack() as ph:
            wq = self.sbt(ph, "wq", [128, 8, 512], BF16)
            RAW = self.sbt(ph, "RAWc", [128, T])
            CV = self.sbt(ph, "CVc", [128, T])
            QT = self.sbt(ph, "QT", [128, T])
            KT = self.sbt(ph, "KT", [128, T])
            KTOK = self.sbt(ph, "KTOK", [128, NT, 128])
            VTOK = self.sbt(ph, "VTOK", [128, NT, 128])
            QTOK = self.sbt(ph, "QTOK", [128, NT, 128])
            OACC = self.sbt(ph, "OACC", [128, NT, 128])
            Sst = [self.sbt(ph, f"Sst{i}", [128, 128]) for i in range(2)]
            RING = [[self.sbt(ph, f"RING_{dr}_{i}", [128, 640]) for i in range(3)] for dr in range(2)]
            for part, c0 in enumerate((h * 128, 512 + h * 128, 1024 + h * 128, 1536 + h * 128)):
                self.ld("pool", wq[:, :, part * 128:(part + 1) * 128],
                        d["w_in"][l, :, c0:c0 + 128].rearrange("(kc p) n -> p kc n", p=128), f"wq_{part}", w=[f"wq:{part}"])
            flat = lambda tl: tl[:].rearrange("p a b -> p (a b)")
            KTOKf, VTOKf, QTOKf, OACCf = flat(KTOK), flat(VTOK), flat(QTOK), flat(OACC)

            def prep_chain(which, raw, kraw, cvt, kcv, dst, kdst, wv_s, wv_d):
                yield from self.proj_fm_g(raw, kraw, wq, f"wq:{which}", which * 128, wv=wv_s)
                yield from self.conv_g("dve", cvt, raw, self.cqw[:, which * 4 + h, :], None, [kraw, "cqw"], kcv, wv=wv_s)
                self.act(wv_d(dst[:, :]), cvt[:, :], AF.Silu, r=[kcv], w=[kdst])
                yield
                if which < 2:
                    self.tt("pool", wv_s(cvt[:, :]), dst[:, :], dst[:, :], ALU.mult, r=[kdst], w=[kcv])
                    yield
                    for blk in range(5):
                        b0 = blk * 512
                        n = min(512, T - b0)
                        pb = PS[self.ps_rot % 4]
                        pk = f"ps{self.ps_rot % 4}"
                        self.ps_rot += 1
                        self.mm(pb[:, 0:n], cm[C_ONES], cvt[:, b0:b0 + n], r=["cmat", kcv], w=[pk])
                        self.act(wv_s(raw[:, b0:b0 + n]), pb[:, 0:n], AF.Ln, r=[pk, "epsc"], w=[kraw], bias=self.epsc[:, 1:2], scale=1.0)
                        yield
                    self.act(wv_s(raw[:, :]), raw[:, :], AF.Exp, r=[kraw], w=[kraw], scale=-0.5)
                    yield
                    self.stt("dve", wv_d(dst[:, :]), dst[:, :], (128.0 ** -0.5) if which == 0 else 1.0, raw[:, :], ALU.mult, ALU.mult,
                             r=[kdst, kraw], w=[kdst])
                    yield

            self.pipe([
                lambda: prep_chain(1, RAW, "RAWc", CV, "CVc", KT, "KT", r_, rr_),
                lambda: prep_chain(0, KTOKf, "KTOKs", VTOKf, "VTOKs", QT, "QT", rr_, rr_),
                lambda: prep_chain(2, QTOKf, "QTOKs", OACCf, "OACCs", OACCf, "OACCs", r_, r_),
            ], ni=3, skew=1)
            S.barrier()
            if h == 0:
                self.dump("q0", QT[:], "QT", [128, T]); self.dump("k0", KT[:], "KT", [128, T]); self.dump("v0", OACCf, "OACCs", [128, T])
            cnt = 0
            for t in range(NT):
                for srcT, sk, dstT, dk2 in ((KT, "KT", KTOK, "KTOK"), (OACCf, "OACCs", VTOK, "VTOK"), (QT, "QT", QTOK, "QTOK")):
                    bank = 4 + cnt % 4
                    pb = PS[bank]
                    pk = f"ps{bank}"
                    self.tr(pb[:, 0:128], srcT[:, t * 128:(t + 1) * 128], r=[sk, "ident"], w=[pk])
                    self.cp("act" if cnt % 2 == 0 else "dve", (rr_ if dk2 != "QTOK" else r_)(dstT[:, t, :]), pb[:, 0:128], r=[pk], w=[f"{dk2}:{t}"])
                    cnt += 1
            S.barrier()
            carve_state = {"i": 0}

            def carve(n):
                i = carve_state["i"]
                if i < T and i + n > T:
                    i = T
                assert i + n <= 2 * T
                carve_state["i"] = i + n
                return (RAW if i < T else CV)[:, (i % T):(i % T) + n]

            NI = 4
            GB = [carve(128) for j in range(NI)]
            DD = [carve(256) for j in range(NI)]
            WA = [carve(384) for j in range(NI)]
            WB = [carve(384) for j in range(NI)]
            smallt = self.sbt(ph, "csmall", [128, 8, 128])
            vnt = self.sbt(ph, "cvn", [128, 2, 128])
            ttft = self.sbt(ph, "cttf", [128, NI, 128])
            ket = self.sbt(ph, "cke", [128, NI, 128])
            VN = [vnt[:, dr, :] for dr in range(2)]
            O1 = [smallt[:, 2 + dr, :] for dr in range(2)]
            zs = [smallt[:, 4 + i, :] for i in range(2)]
            y1 = [smallt[:, 6 + i, :] for i in range(2)]
            self.ts("dve", rr_(Sst[0][:]), cm[C_ONES], 0.0, None, ALU.mult, r=["cmat"], w=["S0"])
            self.ts("dve", rr_(Sst[1][:]), cm[C_ONES], 0.0, None, ALU.mult, r=["cmat"], w=["S1"])
            order = [list(range(NT)), [1, 0] + list(range(NT - 1, 1, -1))]
            oacc_written = set()

            def evac_eng(n, k):
                return "act" if (n + k) % 2 == 0 else "dve"

            def pre(n):
                i, dr = n // 2, n % 2
                t = order[dr][i]
                col = dr * 4 + h
                j = n % NI
                sl = i % 3
                bank = PS[4 + n % 4]
                bk = f"ps{4 + n % 4}"
                gb, dd, wa, wb = GB[j], DD[j], WA[j], WB[j]
                kgb, kdd, kwa, kwb = f"GB{j}", f"DD{j}", f"WA{j}", f"WB{j}"
                ring = RING[dr][sl]
                rk = f"RING_{dr}_{sl}"
                tri, ntri, ms, mit = (cmr[C_TRIF], cmr[C_NTRIF], cmr[C_MSF], cmr[C_MITF]) if dr == 0 else (cmr[C_TRIB], cmr[C_NTRIB], cmr[C_MSB], cmr[C_MITB])
                idr = cmr[C_ID]
                tok = slice(t * 128, (t + 1) * 128)
                smk = f"sm:{t}"
                self.act(r_(gb), cm[C_ONES], AF.Identity, r=["cmat", smk], w=[kgb], scale=sm["G"][:, t, col:col + 1])
                self.mm(bank[:, 0:128], rr_(KT[:, tok]), rr_(KT[:, tok]), r=["KT"], w=[bk])
                self.mm(bank[:, 128:256], rr_(KT[:, tok]), rr_(QT[:, tok]), r=["KT", "QT"], w=[bk])
                self.mm(bank[:, 256:384], tri, r_(gb), True, False, r=["cmatr", kgb], w=[bk])
                self.mm(bank[:, 256:384], r_(gb), ntri, False, False, r=["cmatr", kgb], w=[bk])
                self.mm(bank[:, 256:384], idr, ms, False, True, r=["cmatr"], w=[bk])
                self.mm(bank[:, 384:512], r_(gb), tri, True, False, r=["cmatr", kgb], w=[bk])
                self.mm(bank[:, 384:512], ntri, r_(gb), False, False, r=["cmatr", kgb], w=[bk])
                self.mm(bank[:, 384:512], idr, mit, False, True, r=["cmatr"], w=[bk])
                yield
                self.act(r_(dd[:, 0:256]), bank[:, 256:512], AF.Exp, r=[bk], w=[kdd])
                self.stt("dve", r_(wa[:, 256:384]), bank[:, 0:128], sm["NBETA"][:, t, col:col + 1], dd[:, 0:128], ALU.mult, ALU.mult,
                         r=[bk, smk, kdd], w=[kwa])
                self.tt("dve", rr_(ring[:, 384:512]), bank[:, 128:256], dd[:, 128:256], ALU.mult, r=[bk, kdd], w=[rk])
                yield
                self.tr(bank[:, 0:128], wa[:, 256:384], r=[kwa, "ident"], w=[bk])
                yield
                self.cp(evac_eng(n, 0), r_(wa[:, 0:128]), bank[:, 0:128], r=[bk], w=[kwa])
                yield
                self.mm(bank[:, 0:128], r_(wa[:, 256:384]), r_(wa[:, 0:128]), r=[kwa], w=[bk])
                self.mm(bank[:, 256:384], r_(wa[:, 0:128]), r_(wa[:, 256:384]), r=[kwa], w=[bk])
                yield
                self.cp("act", wb[:, 0:384].rearrange("p (a b) -> p a b", b=128)[:, ::2, :],
                        bank[:, 0:384].rearrange("p (a b) -> p a b", b=128)[:, ::2, :], r=[bk], w=[kwb])
                self.tt("dve", wb[:, 128:256], wa[:, 0:128], cm[C_ID], ALU.add, r=[kwa, "cmat"], w=[kwb])
                yield
                cur, kcur, nxt, knxt = wb, kwb, wa, kwa
                for k in range(1, 6):
                    self.mm(bank[:, 0:256], r_(cur[:, 256:384]), r_(cur[:, 0:256]), r=[kcur], w=[bk])
                    self.mm(bank[:, 256:384], r_(cur[:, 0:128]), r_(cur[:, 256:384]), r=[kcur], w=[bk])
                    yield
                    self.cp("act", nxt[:, 0:384].rearrange("p (a b) -> p a b", b=128)[:, ::2, :],
                            bank[:, 0:384].rearrange("p (a b) -> p a b", b=128)[:, ::2, :], r=[bk], w=[knxt])
                    self.tt("dve", nxt[:, 128:256], bank[:, 128:256], cur[:, 128:256], ALU.add, r=[bk, kcur], w=[knxt])
                    cur, kcur, nxt, knxt = nxt, knxt, cur, kcur
                    yield
                self.mm(bank[:, 0:128], r_(cur[:, 256:384]), r_(cur[:, 128:256]), True, False, r=[kcur], w=[bk])
                self.mm(bank[:, 0:128], idr, r_(cur[:, 128:256]), False, True, r=[kcur, "cmatr"], w=[bk])
                yield
                self.act(rr_(ttft[:, j, :]), bank[:, 0:128], AF.Identity, r=[bk, smk], w=[f"TTF{j}"], scale=sm["BETA"][:, t, col:col + 1])
                self.act(rr_(ket[:, j, :]), KTOK[:, t, :], AF.Identity, r=[f"KTOK:{t}", smk], w=[f"KE{j}"], scale=sm["EGC"][:, t, col:col + 1])
                self.ts("dve", r_(nxt[:, 256:384]), QTOK[:, t, :], sm["EGC"][:, t, col:col + 1], None, ALU.mult, r=[f"QTOK:{t}", smk], w=[knxt])
                self.ts("dve", rr_(ring[:, 512:640]), KTOK[:, t, :], sm["EDEC"][:, t, col:col + 1], None, ALU.mult, r=[f"KTOK:{t}", smk], w=[rk])
                yield
                self.mm(bank[:, 0:128], rr_(ttft[:, j, :]), rr_(VTOK[:, t, :]), r=[f"TTF{j}", f"VTOK:{t}"], w=[bk])
                self.mm(bank[:, 128:256], rr_(ket[:, j, :]), rr_(ttft[:, j, :]), r=[f"KE{j}", f"TTF{j}"], w=[bk])
                self.tr(bank[:, 256:384], nxt[:, 256:384], r=[knxt, "ident"], w=[bk])
                yield
                self.cp(evac_eng(n, 0), rr_(ring[:, 0:384]), bank[:, 0:384], r=[bk], w=[rk])
                yield

            def step(i, dr):
                t = order[dr][i]
                col = dr * 4 + h
                sl = i % 3
                ring = RING[dr][sl]
                rk = f"RING_{dr}_{sl}"
                A = PS[dr * 2]
                B = PS[dr * 2 + 1]
                ka = f"ps{dr * 2}"
                kb = f"ps{dr * 2 + 1}"
                Sd = Sst[dr]
                skey = f"S{dr}"
                vn, o1 = VN[dr], O1[dr]
                kvn, ko1 = f"VN{dr}", f"O1{dr}"
                smk = f"sm:{t}"
                self.mm(A[:, 0:128], rr_(ring[:, 128:256]), rr_(Sd[:]), r=[rk, skey], w=[ka])
                yield
                self.tt("dve", rr_(vn), ring[:, 0:128], A[:, 0:128], ALU.subtract, r=[rk, ka], w=[kvn])
                yield
                self.mm(B[:, 0:128], rr_(ring[:, 256:384]), rr_(Sd[:]), True, False, r=[rk, skey], w=[kb])
                self.mm(B[:, 0:128], rr_(ring[:, 384:512]), rr_(vn), False, True, r=[rk, kvn], w=[kb])
                self.mm(A[:, 128:256], rr_(ring[:, 512:640]), rr_(vn), r=[rk, kvn], w=[ka])
                yield
                self.stt("dve", rr_(Sd[:]), Sd[:], sm["GLB"][:, t, col:col + 1], A[:, 128:256], ALU.mult, ALU.add,
                         r=[skey, smk, ka], w=[skey])
                if t not in oacc_written:
                    oacc_written.add(t)
                    self.cp("act", OACC[:, t, :], B[:, 0:128], r=[kb], w=[f"OACC:{t}"])
                else:
                    self.cp("act", r_(o1), B[:, 0:128], r=[kb], w=[ko1])
                    self.tt("pool", OACC[:, t, :], OACC[:, t, :], o1, ALU.add, r=[f"OACC:{t}", ko1], w=[f"OACC:{t}"])
                yield

            nitems = 2 * NT
            next_item = 0
            active = []
            pre_done = set()
            steps_emitted = [0, 0]
            chain = [None, None]
            chain_i = [0, 0]
            while True:
                progressed = False
                while len(active) < NI and next_item < nitems:
                    n = next_item
                    i, dr = n // 2, n % 2
                    if i >= 3 and steps_emitted[dr] < i - 2:
                        break
                    assert all(a[0] % NI != n % NI for a in active)
                    active.append((n, pre(n)))
                    next_item += 1
                for (n, g) in list(active):
                    try:
                        next(g)
                    except StopIteration:
                        active.remove((n, g))
                        pre_done.add(n)
                    progressed = True
                for dr in range(2):
                    if chain[dr] is None and chain_i[dr] < NT and (2 * chain_i[dr] + dr) in pre_done:
                        chain[dr] = step(chain_i[dr], dr)
                    if chain[dr] is not None:
                        try:
                            next(chain[dr])
                        except StopIteration:
                            chain[dr] = None
                            steps_emitted[dr] += 1
                            chain_i[dr] += 1
                        progressed = True
                if not progressed:
                    break
            assert steps_emitted == [NT, NT], steps_emitted
            S.barrier()
            if h == 0:
                self.dump("oacc0", OACC[:], "OACC:0", [128, NT, 128])
            self.dump(f"oacch{h}", OACC[:], "OACC:0", [128, NT, 128])
            st6 = self.sbt(ph, "cst6", [128, 4, 6])
            mv = self.sbt(ph, "cmv", [128, 4, 6])
            zs4 = [smallt[:, i, :] for i in range(4)]
            y14 = [smallt[:, 4 + i, :] for i in range(4)]

            def tileY(t):
                s2 = t % 4
                kst, kmv, kz, ky = f"cst6{s2}", f"cmv{s2}", f"zs{s2}", f"y1{s2}"
                pz = PS[s2]
                pk = f"ps{s2}"
                S.op("dve", lambda e: e.bn_stats(out=st6[:, s2, :], in_=OACC[:, t, :]), [f"OACC:{t}"], [kst])
                for kc in range(8):
                    self.mm(pz[:, 0:128], self.hT[:, kc, t * 128:(t + 1) * 128], wq[:, kc, 384:512], kc == 0, kc == 7,
                            r=[f"hT:{t}", "wq:3"], w=[pk])
                yield
                S.op("dve", lambda e: e.bn_aggr(out=mv[:, s2, 0:2], in_=st6[:, s2, :]), [kst], [kmv])
                self.act(zs4[s2], pz[:, 0:128], AF.Exp, r=[pk], w=[kz], scale=-1.0)
                yield
                self.ts("dve", zs4[s2], zs4[s2], 1.0, None, ALU.add, r=[kz], w=[kz])
                yield
                S.op("dve", lambda e: e.reciprocal(out=zs4[s2], in_=zs4[s2]), [kz], [kz])
                yield
                self.tt("dve", mv[:, s2, 2:3], mv[:, s2, 0:1], mv[:, s2, 0:1], ALU.mult, r=[kmv], w=[kmv])
                yield
                self.tt("dve", mv[:, s2, 2:3], mv[:, s2, 2:3], mv[:, s2, 1:2], ALU.add, r=[kmv], w=[kmv])
                yield
                self.act(mv[:, s2, 3:4], mv[:, s2, 2:3], AF.Ln, r=[kmv, "epsc"], w=[kmv], bias=self.epsc[:, 1:2], scale=1.0)
                yield
                self.act(mv[:, s2, 4:5], mv[:, s2, 3:4], AF.Exp, r=[kmv], w=[kmv], scale=-0.5)
                yield
                self.stt("dve", r_(y14[s2]), OACC[:, t, :], mv[:, s2, 4:5], self.onw[:], ALU.mult, ALU.mult,
                         r=[f"OACC:{t}", kmv, "onw"], w=[ky])
                yield
                self.tt("dve", r_(y14[s2]), y14[s2], zs4[s2], ALU.mult, r=[ky, kz], w=[ky])
                yield
                self.tt("dve", r_(y14[s2]), y14[s2], pz[:, 0:128], ALU.mult, r=[ky, pk], w=[ky])
                yield
                self.tr(pz[:, 128:256], y14[s2], r=[ky, "ident"], w=[pk])
                yield
                self.cp("act", self.yT[:, h, t * 128:(t + 1) * 128], pz[:, 128:256], r=[pk], w=[f"yT:{h}"])
                yield

            self.pipe([(lambda t=t: tileY(t)) for t in range(NT)], ni=4, skew=3)
            S.barrier()

    def phaseD(self, bi, l, src):
        nc, S, d = self.nc, self.S, self.d
        PS = self.PS
        last = (l == 1)
        NI = 4
        with ExitStack() as ph:
            wo = self.sbt(ph, "wo_bf", [128, 8, 1024], BF16)
            wr = self.sbt(ph, "wr_bf", [128, 8, 36], BF16)
            rb = self.sbt(ph, "rb_bc", [128, 36])
            gt = [self.sbt(ph, f"gt1_{i}", [128, D]) for i in range(2)]
            lng = self.sbt(ph, "lng_bc", [128, D])
            lnb = self.sbt(ph, "lnb_bc", [128, D])
            xt = [self.sbt(ph, f"xtD{i}", [128, D]) for i in range(NI)]
            rt = [self.sbt(ph, f"rtD{i}", [128, D]) for i in range(NI)]
            xn = [self.sbt(ph, f"xnD{i}", [128, D]) for i in range(NI)]
            rs = [self.sbt(ph, f"rsD{i}", [128, 128]) for i in range(NI)]
            for half in range(2):
                self.ld("pool", wo[:, :, half * 512:(half + 1) * 512],
                        d["w_out"][l, :, half * 512:(half + 1) * 512].rearrange("(kc p) n -> p kc n", p=128), f"wo{half}", w=[f"wo:{half}"])
            self.ld("pool", wr[:], d["wr"][l, :, :].rearrange("(kc p) n -> p kc n", p=128), "wr", w=["wr"])
            self.ld("sp", rb[:], d["br"][l, :].partition_broadcast(128), "rb", w=["rb"])
            self.ld("sp", gt[0][:], d["modrow"][l, bi, 2048:3072].partition_broadcast(128), "gt0", r=["modrow"], w=["gt0"])
            self.ld("sp", gt[1][:], d["modrow"][l, 2, 2048:3072].partition_broadcast(128), "gt1", r=["modrow"], w=["gt1"])
            self.ld("sp", lng[:], d["ln_g"][l, 0, :].partition_broadcast(128), "lng", w=["lng"])
            self.ld("sp", lnb[:], d["ln_b"][l, 0, :].partition_broadcast(128), "lnb", w=["lnb"])
            tiles = list(range(2 if last else 0, NT))

            def tileD(idx, t):
                j = 2 if t < 2 else bi
                g = 1 if t < 2 else 0
                s = idx % NI
                tok = slice(t * 128, (t + 1) * 128)
                bank = [PS[s * 2], PS[s * 2 + 1]]
                bkey = [f"ps{s * 2}", f"ps{s * 2 + 1}"]
                kx, kr, kn, krs = f"xtD{s}", f"rtD{s}", f"xnD{s}", f"rsD{s}"
                self.ld("sp", xt[s][:], src[bi, tok, :], kx, w=[kx])
                for half in range(2):
                    for c in range(8):
                        self.mm(bank[half][:, 0:512], self.yT[:, c, tok], wo[:, c, half * 512:(half + 1) * 512], c == 0, c == 7,
                                r=[f"yT:{c}", f"wo:{half}"], w=[bkey[half]])
                yield
                for half in range(2):
                    self.tt("dve", rt[s][:, half * 512:(half + 1) * 512], bank[half][:, 0:512], gt[g][:, half * 512:(half + 1) * 512], ALU.mult,
                            r=[bkey[half], f"gt{g}"], w=[kr])
                yield
                self.stt("dve", rt[s][:], xt[s][:], ALPHA, rt[s][:], ALU.mult, ALU.add, r=[kx, kr], w=[kr])
                yield
                st = {}
                yield from self.ln_stats_g(rt[s], kr, st)
                self.ts("dve", rt[s][:], rt[s][:], st["mean"], st["rstd"], ALU.subtract, ALU.mult, r=[kr, st["k"]], w=[kr])
                yield
                self.tt("pool", rt[s][:], rt[s][:], lng[:], ALU.mult, r=[kr, "lng"], w=[kr])
                yield
                self.tt("pool", rt[s][:], rt[s][:], lnb[:], ALU.add, r=[kr, "lnb"], w=[kr])
                yield
                self.ld("sp", d["xs1"][bi, tok, :], rt[s][:], f"x1st{s}", r=[kr], w=[f"xs1:{t}"])
                if t == 2:
                    self.dump("x1_t2", rt[s][:], kr, [128, D])
                st2 = {}
                yield from self.ln_stats_g(rt[s], kr, st2)
                self.ts("dve", xn[s][:], rt[s][:], st2["mean"], st2["rstd"], ALU.subtract, ALU.mult, r=[kr, st2["k"]], w=[kn])
                yield
                for kc in range(8):
                    q = kc % 4
                    self.tr(bank[kc // 4][:, q * 128:(q + 1) * 128], xn[s][:, kc * 128:(kc + 1) * 128], r=[kn, "ident"], w=[bkey[kc // 4]])
                yield
                for kc in range(8):
                    q = kc % 4
                    self.act(self.hT[:, kc, tok], bank[kc // 4][:, q * 128:(q + 1) * 128], AF.Identity, r=[bkey[kc // 4], "onep", "modT"], w=[f"hT:{t}"],
                             scale=self.onep[:, l, 32 + kc, j:j + 1], bias=self.modT[:, l, 24 + kc, j:j + 1])
                yield
                pr = bank[0]
                prk = bkey[0]
                for kc in range(8):
                    self.mm(pr[:, 0:36], self.hT[:, kc, tok], wr[:, kc, :], kc == 0, kc == 7, r=[f"hT:{t}", "wr"], w=[prk])
                yield
                R = rs[s]
                rk = krs
                self.tt("dve", R[:, 0:36], pr[:, 0:36], rb[:], ALU.add, r=[prk, "rb"], w=[rk])
                yield
                sc = lambda i: R[:, 120 + i:121 + i]
                S.op("dve", lambda e: e.reduce_max(out=R[:, 120:121], in_=R[:, 0:4], axis=mybir.AxisListType.X), [rk], [rk])
                yield
                self.ts("dve", R[:, 36:40], R[:, 0:4], sc(0), None, ALU.subtract, r=[rk], w=[rk])
                self.ts("dve", R[:, 40:44], R[:, 0:4], sc(0), None, ALU.is_ge, r=[rk], w=[rk])
                yield
                self.act(R[:, 36:40], R[:, 36:40], AF.Exp, r=[rk], w=[rk])
                self.ts("dve", R[:, 40:44], R[:, 40:44], 1.0, 1e30, ALU.subtract, ALU.mult, r=[rk], w=[rk])
                yield
                S.op("dve", lambda e: e.reduce_sum(out=R[:, 121:122], in_=R[:, 36:40], axis=mybir.AxisListType.X), [rk], [rk])
                for g4 in range(4):
                    self.ts("dve", R[:, 44 + g4 * 8:52 + g4 * 8], R[:, 4 + g4 * 8:12 + g4 * 8], R[:, 40 + g4:41 + g4], None, ALU.add, r=[rk], w=[rk])
                yield
                S.op("dve", lambda e: e.reciprocal(out=R[:, 122:123], in_=R[:, 121:122]), [rk], [rk])
                S.op("dve", lambda e: e.reduce_max(out=R[:, 123:124], in_=R[:, 44:76], axis=mybir.AxisListType.X), [rk], [rk])
                yield
                self.ts("dve", R[:, 76:108], R[:, 44:76], sc(3), None, ALU.is_ge, r=[rk], w=[rk])
                yield
                self.stt("dve", R[:, 44:76], R[:, 76:108], -1e30, R[:, 44:76], ALU.mult, ALU.add, r=[rk], w=[rk])
                yield
                S.op("dve", lambda e: e.reduce_max(out=R[:, 124:125], in_=R[:, 44:76], axis=mybir.AxisListType.X), [rk], [rk])
                yield
                self.ts("dve", R[:, 44:76], R[:, 44:76], sc(4), None, ALU.is_ge, r=[rk], w=[rk])
                self.tt("dve", R[:, 125:126], R[:, 124:125], R[:, 123:124], ALU.subtract, r=[rk], w=[rk])
                yield
                self.act(R[:, 125:126], R[:, 125:126], AF.Exp, r=[rk], w=[rk])
                yield
                self.ts("dve", R[:, 125:126], R[:, 125:126], 1.0, None, ALU.add, r=[rk], w=[rk])
                yield
                S.op("dve", lambda e: e.reciprocal(out=R[:, 126:127], in_=R[:, 125:126]), [rk], [rk])
                yield
                self.tt("dve", R[:, 126:127], R[:, 126:127], R[:, 122:123], ALU.mult, r=[rk], w=[rk])
                yield
                self.tt("dve", R[:, 127:128], R[:, 122:123], R[:, 126:127], ALU.subtract, r=[rk], w=[rk])
                self.ts("dve", R[:, 76:108], R[:, 76:108], R[:, 126:127], None, ALU.mult, r=[rk], w=[rk])
                yield
                self.stt("dve", R[:, 76:108], R[:, 44:76], R[:, 127:128], R[:, 76:108], ALU.mult, ALU.add, r=[rk], w=[rk])
                yield
                if t == 2:
                    self.dump("comb_t2", R[:, 76:108], rk, [128, 32])
                self.tr(pr[0:32, 128:256], R[:, 76:108], r=[rk, "ident"], w=[prk])
                yield
                self.cp("act", self.combT[0:32, tok], pr[0:32, 128:256], r=[prk], w=[f"combT:{t}"])
                yield

            self.pipe([(lambda i=i, t=t: tileD(i, t)) for i, t in enumerate(tiles)], ni=NI, skew=8)
            c0 = tiles[0] * 128
            self.ld("sp", d["combD"][:, c0:T], self.combT[0:32, c0:T], "combst", r=[f"combT:{t}" for t in tiles], w=["combD"])
            S.barrier()

    def phaseE(self, bi, l):
        nc, S, d = self.nc, self.S, self.d
        PS = self.PS
        last = (l == 1)
        tok0 = NCTX if last else 0
        blocks = []
        b = tok0
        while b < T:
            n = min(512, T - b)
            blocks.append((b, n))
            b += n
        ne = self.cfg.get("nexp", 32)
        with ExitStack() as ph:
            Y = self.sbt(ph, "Yacc", [128, NT, D])
            with ExitStack() as ex:
                WG = [self.sbt(ex, f"WG{i}", [128, 8, 256], BF16) for i in range(3)]
                WU = [self.sbt(ex, f"WU{i}", [128, 8, 256], BF16) for i in range(3)]
                WD = [self.sbt(ex, f"WD{i}", [128, 2, 1024], BF16) for i in range(3)]
                CB = [self.sbt(ex, f"CB{i}", [128, T], BF16) for i in range(3)]
                SA = [self.sbt(ex, f"SA{i}", [128, 512]) for i in range(2)]
                T1 = [self.sbt(ex, f"T1{i}", [128, 512], BF16) for i in range(2)]
                AT = [[self.sbt(ex, f"AT{p}{i}", [128, 512], BF16) for i in range(2)] for p in range(2)]
                ycnt = [0]
                work = []
                bcnt = 0
                for e in range(ne):
                    for (b0, n) in blocks:
                        work.append((e, e % 3, b0, n, bcnt % 2))
                        bcnt += 1

                def load_w(e):
                    s = e % 3
                    self.ld("pool", WG[s][:], d["weg"][l, e, :, :].rearrange("(kc p) f -> p kc f", p=128), f"WG{s}", w=[f"WG{s}"])
                    self.ld("pool", WU[s][:], d["weu"][l, e, :, :].rearrange("(kc p) f -> p kc f", p=128), f"WU{s}", w=[f"WU{s}"])
                    self.ld("pool", WD[s][:], d["wed"][l, e, :, :].rearrange("(fc p) n -> p fc n", p=128), f"WD{s}", w=[f"WD{s}"])
                    self.ld("sp", CB[s][:], d["combD"][e, :].partition_broadcast(128), f"CB{s}", r=["combD"], w=[f"CB{s}"])

                def gu(wi, fc):
                    e, s, b0, n, par = work[wi]
                    tiles = list(range(b0 // 128, (b0 + n) // 128))
                    hk = [f"hT:{t}" for t in tiles]
                    pa, pb = PS[fc * 2], PS[fc * 2 + 1]
                    ka, kb = f"ps{fc * 2}", f"ps{fc * 2 + 1}"
                    for kc in range(8):
                        self.mm(pa[:, 0:n], WG[s][:, kc, fc * 128:(fc + 1) * 128], self.hT[:, kc, b0:b0 + n], kc == 0, kc == 7,
                                r=[f"WG{s}"] + hk, w=[ka])
                    for kc in range(8):
                        self.mm(pb[:, 0:n], WU[s][:, kc, fc * 128:(fc + 1) * 128], self.hT[:, kc, b0:b0 + n], kc == 0, kc == 7,
                                r=[f"WU{s}"] + hk, w=[kb])
                    self.act(SA[fc][:, 0:n], pa[:, 0:n], AF.Silu, r=[ka], w=[f"SA{fc}"])
                    self.tt("dve", T1[fc][:, 0:n], pb[:, 0:n], SA[fc][:, 0:n], ALU.mult, r=[kb, f"SA{fc}"], w=[f"T1{fc}"])
                    self.tt("pool", AT[par][fc][:, 0:n], T1[fc][:, 0:n], CB[s][:, b0:b0 + n], ALU.mult, r=[f"CB{s}", f"T1{fc}"], w=[f"AT{par}{fc}"])

                def down(wi):
                    e, s, b0, n, par = work[wi]
                    tiles = list(range(b0 // 128, (b0 + n) // 128))
                    for ti, t in enumerate(tiles):
                        for half in range(2):
                            yb = 4 + ycnt[0] % 4
                            ycnt[0] += 1
                            py = PS[yb]
                            yk = f"ps{yb}"
                            for fc in range(2):
                                self.mm(py[:, 0:512], AT[par][fc][:, ti * 128:(ti + 1) * 128], WD[s][:, fc, half * 512:(half + 1) * 512],
                                        fc == 0, fc == 1, r=[f"AT{par}{fc}", f"WD{s}"], w=[yk])
                            ysl = Y[:, t, half * 512:(half + 1) * 512]
                            ykey = f"Y:{t}:{half}"
                            if e == 0:
                                self.cp("act", ysl, py[:, 0:512], r=[yk], w=[ykey])
                            else:
                                self.tt("dve", ysl, py[:, 0:512], ysl, ALU.add, r=[yk, ykey], w=[ykey])

                loaded = set()
                for e in range(min(2, ne)):
                    load_w(e); loaded.add(e)
                nw = len(work)
                gu(0, 0)
                for wi in range(nw):
                    e = work[wi][0]
                    if wi == 0 or work[wi - 1][0] != e:
                        if e + 2 < ne and (e + 2) not in loaded:
                            load_w(e + 2); loaded.add(e + 2)
                    gu(wi, 1)
                    if wi + 1 < nw:
                        gu(wi + 1, 0)
                    down(wi)
                S.barrier()
                S.emit()

            gt = [self.sbt(ph, f"gt2_{i}", [128, D]) for i in range(2)]
            lng = self.sbt(ph, "lng2_bc", [128, D])
            lnb = self.sbt(ph, "lnb2_bc", [128, D])
            NF = 4
            xt = [self.sbt(ph, f"xtF{i}", [128, D]) for i in range(NF)]
            ot = [self.sbt(ph, f"otF{i}", [128, D]) for i in range(NF)]
            self.ld("sp", gt[0][:], d["modrow"][l, bi, 5120:6144].partition_broadcast(128), "gt20", r=["modrow"], w=["gt20"])
            self.ld("sp", gt[1][:], d["modrow"][l, 2, 5120:6144].partition_broadcast(128), "gt21", r=["modrow"], w=["gt21"])
            self.ld("sp", lng[:], d["ln_g"][l, 1, :].partition_broadcast(128), "lng2", w=["lng2"])
            self.ld("sp", lnb[:], d["ln_b"][l, 1, :].partition_broadcast(128), "lnb2", w=["lnb2"])
            tiles = list(range(2 if last else 0, NT))

            def tileF(idx, t):
                g = 1 if t < 2 else 0
                s2 = idx % NF
                tok = slice(t * 128, (t + 1) * 128)
                kx, ko = f"xtF{s2}", f"otF{s2}"
                self.ld("sp", xt[s2][:], d["xs1"][bi, tok, :], kx, r=[f"xs1:{t}"], w=[kx])
                self.tt("pool", ot[s2][:], Y[:, t, :], gt[g][:], ALU.mult, r=[f"Y:{t}:0", f"Y:{t}:1", f"gt2{g}"], w=[ko])
                yield
                self.stt("dve", ot[s2][:], xt[s2][:], ALPHA, ot[s2][:], ALU.mult, ALU.add, r=[kx, ko], w=[ko])
                yield
                st = {}
                yield from self.ln_stats_g(ot[s2], ko, st)
                self.ts("dve", ot[s2][:], ot[s2][:], st["mean"], st["rstd"], ALU.subtract, ALU.mult, r=[ko, st["k"]], w=[ko])
                yield
                self.tt("pool", ot[s2][:], ot[s2][:], lng[:], ALU.mult, r=[ko, "lng2"], w=[ko])
                yield
                self.tt("dve", ot[s2][:], ot[s2][:], lnb[:], ALU.add, r=[ko, "lnb2"], w=[ko])
                yield
                if t == 2:
                    self.dump(f"x2_t2_l{l}", ot[s2][:], ko, [128, D])
                    self.dump(f"f_t2_l{l}", Y[:, t, :], f"Y:{t}:0", [128, D])
                if last:
                    self.ld("sp", d["out"][bi, (t - 2) * 128:(t - 1) * 128, :], ot[s2][:], f"ost{s2}", r=[ko], w=[f"out:{t}"])
                else:
                    self.ld("sp", d["xs2"][bi, tok, :], ot[s2][:], f"ost{s2}", r=[ko], w=[f"xs2:{t}"])
                yield

            self.pipe([(lambda i=i, t=t: tileF(i, t)) for i, t in enumerate(tiles)], ni=NF, skew=3)
            S.barrier()


def _consts():
    k = np.arange(128)[:, None]
    i = np.arange(128)[None, :]
    cm = np.zeros((10, 128, 128), np.float32)
    cm[C_ID] = np.eye(128)
    cm[C_TRIF] = (k <= i)
    cm[C_TRIB] = (k >= i)
    cm[C_NTRIF] = -cm[C_TRIF]
    cm[C_NTRIB] = -cm[C_TRIB]
    cm[C_MSF] = np.where(i < k, 0.0, NEG)
    cm[C_MSB] = np.where(i > k, 0.0, NEG)
    cm[C_MITF] = np.where(k <= i, 0.0, NEG)
    cm[C_MITB] = np.where(k >= i, 0.0, NEG)
    cm[C_ONES] = 1.0
    sel = np.zeros((32, 32, 128), np.float32)
    for e in range(32):
        sel[e, e, :] = 1.0
    return np.ascontiguousarray(cm.transpose(1, 0, 2)), sel.reshape(32, 4096)


def prep_shared(inp):
    f = lambda a: np.ascontiguousarray(np.asarray(a, dtype=np.float32))
    sh = {}
    sh["w_ada"] = f(inp["w_ada"])
    sh["b_ada"] = f(inp["b_ada"])
    sh["b_adaT"] = f(np.asarray(inp["b_ada"]).reshape(2, 48, 128).transpose(0, 2, 1))
    sh["w_in"] = f(inp["w_in"])
    sh["cqw"] = f(np.asarray(inp["conv_qkv_w"]).reshape(2, 4, 12, 128).transpose(0, 3, 2, 1))
    sh["rgcw"] = f(np.asarray(inp["rg_conv_w"]).reshape(2, 4, 4, 128).transpose(0, 3, 2, 1))
    sh["rgcb"] = f(np.asarray(inp["rg_conv_b"]).reshape(2, 4, 128).transpose(0, 2, 1))
    sh["alog"] = f(np.asarray(inp["dn_a_log"]).reshape(2, 8))
    sh["dtb"] = f(np.asarray(inp["dn_dt_bias"]).reshape(2, 8))
    sh["onw"] = f(inp["dn_onorm_w"])
    wbd = np.zeros((2, 2, 2, 4, 128, 128), np.float32)
    for gate, nm in ((0, "rg_wa"), (1, "rg_wi")):
        w = np.asarray(inp[nm])
        for ch in range(4):
            for sub in range(2):
                wbd[:, :, gate, ch, sub * 64:(sub + 1) * 64, sub * 64:(sub + 1) * 64] = w[:, :, ch * 2 + sub]
    sh["wbd"] = f(wbd.reshape(2, 16, 128, 128).transpose(0, 2, 1, 3))
    rgb = np.zeros((2, 2, 2, 4, 128), np.float32)
    rgb[:, :, 0] = np.asarray(inp["rg_ba"]).reshape(2, 2, 4, 128)
    rgb[:, :, 1] = np.asarray(inp["rg_bi"]).reshape(2, 2, 4, 128)
    sh["rgb"] = f(rgb.reshape(2, 16, 128).transpose(0, 2, 1))
    sh["lam"] = f(np.asarray(inp["rg_lambda"]).reshape(2, 8, 128).transpose(0, 2, 1))
    sh["w_out"] = f(inp["w_out"])
    sh["ln_g"] = f(inp["ln_g"])
    sh["ln_b"] = f(inp["ln_b"])
    sh["wr"] = f(np.concatenate([np.asarray(inp["router_wg"]), np.asarray(inp["router_we"])], axis=-1))
    sh["br"] = f(np.concatenate([np.asarray(inp["router_bg"]), np.asarray(inp["router_be"])], axis=-1))
    sh["weg"] = f(inp["w_e_gate"])
    sh["weu"] = f(inp["w_e_up"])
    sh["wed"] = f(inp["w_e_down"])
    cm, sel = _consts()
    sh["cmat"] = cm
    return sh


def prep_core(inp, core):
    x = np.asarray(inp["x"], dtype=np.float32)
    ctx = np.asarray(inp["ctx"], dtype=np.float32)
    c = np.asarray(inp["c"], dtype=np.float32)
    cc = np.asarray(inp["c_ctx"], dtype=np.float32)
    b0 = 2 * core
    xin = np.concatenate([ctx[b0:b0 + 2], x[b0:b0 + 2]], axis=1)
    cols = np.stack([c[b0], c[b0 + 1], cc], axis=-1)
    cT = cols.reshape(8, 128, 3).transpose(1, 0, 2)
    return {"xin": np.ascontiguousarray(xin), "cT": np.ascontiguousarray(cT)}


def build_nc(cfg=None):
    nc = bass.Bass("TRN2", target_bir_lowering=False)
    kb = KB(nc, cfg or {})
    kb.build()
    return nc, kb


def kernel(**inputs):
    nc, kb = build_nc({})
    sh = prep_shared(inputs)
    in_maps = []
    for core in range(8):
        m = dict(sh)
        m.update(prep_core(inputs, core))
        in_maps.append(m)
    res = run_bass_kernel_spmd(nc, in_maps, core_ids=list(range(8)))
    outs = [np.asarray(r["out"]) for r in res.results]
    return np.concatenate(outs, axis=0).astype(np.float32)
```
